# Optimizing a Trainium2 kernel written in Bass

```python
import math
import jax, jax.numpy as jnp
from jax import lax
import numpy as np


D_MODEL = 2048
BATCH = 4
SEQ = 8192
DEPTH = 1

CHUNK = 64
PLE_DIM = 256
EPS = 1e-6
GLA_HEADS = 8
GLA_DK = 64
GLA_DV = 128
GLA_GATE_RANK = 16
GLA_GATE_TEMP = 16.0
GLA_QK = GLA_HEADS * GLA_DK
GLA_V = GLA_HEADS * GLA_DV
DSA_HEADS = 8
DSA_HD = 128
DSA_W = DSA_HEADS * DSA_HD
IDX_HEADS = 16
IDX_DIM = 64
IDX_Q = IDX_HEADS * IDX_DIM
INDEX_TOPK = 256
Q_BLOCK = 64
REL_BUCKETS = 32
REL_MAX_DIST = 128
N_GROUPS = 8
EXPERTS_PER_GROUP = 8
N_EXPERTS = N_GROUPS * EXPERTS_PER_GROUP
EXPERT_FF = 512
TOP_K_INNER = 2
MOE_BLOCK = 128
SPLITS = (GLA_QK, GLA_QK, GLA_V, GLA_V, GLA_GATE_RANK, DSA_W, DSA_W, DSA_W, IDX_Q, IDX_DIM, IDX_HEADS)
IN_COLS = sum(SPLITS)

kernel_name = 'hybrid_gla_dsa_hmoe_streaming_block'


def rms_norm(t, g):
    tf = t.astype(jnp.float32)
    tf = tf * lax.rsqrt(jnp.mean(tf * tf, axis=-1, keepdims=True) + EPS)
    return (tf * g.astype(jnp.float32)).astype(t.dtype)


def split_cols(z):
    offs, acc = [], 0
    for w in SPLITS[:-1]:
        acc += w
        offs.append(acc)
    return jnp.split(z, offs, axis=-1)


def t5_bucket(rel):
    half = REL_BUCKETS // 2
    exact = half // 2
    sign = jnp.where(rel > 0, half, 0)
    n = jnp.abs(rel)
    nf = jnp.maximum(n, 1).astype(jnp.float32)
    large = exact + (jnp.log(nf / exact) / math.log(REL_MAX_DIST / exact) * (half - exact)).astype(jnp.int32)
    large = jnp.minimum(large, half - 1)
    return sign + jnp.where(n < exact, n, large)


def gla_mixer(q, k, v, g):
    B, S = q.shape[:2]
    n = S // CHUNK

    def to_chunks(t):
        return t.reshape(B, n, CHUNK, *t.shape[2:]).swapaxes(0, 1)

    G = jnp.cumsum(g.reshape(B, n, CHUNK, GLA_HEADS, GLA_DK), axis=2).swapaxes(0, 1)
    causal = jnp.tril(jnp.ones((CHUNK, CHUNK), dtype=bool))

    def step(state, inp):
        qb, kb, vb, Gb = inp
        o_inter = jnp.einsum('bihk,bhkv->bihv', qb * jnp.exp(Gb), state)
        diff = jnp.where(causal[None, :, :, None, None], Gb[:, :, None] - Gb[:, None, :], -jnp.inf)
        scores = jnp.einsum('bihk,bjhk,bijhk->bhij', qb, kb, jnp.exp(diff))
        o_intra = jnp.einsum('bhij,bjhv->bihv', scores, vb)
        G_last = Gb[:, -1]
        k_dec = kb * jnp.exp(G_last[:, None] - Gb)
        state = state * jnp.exp(G_last)[..., None] + jnp.einsum('bjhk,bjhv->bhkv', k_dec, vb)
        return state, o_inter + o_intra

    state0 = jnp.zeros((B, GLA_HEADS, GLA_DK, GLA_DV), q.dtype)
    _, o = lax.scan(step, state0, (to_chunks(q), to_chunks(k), to_chunks(v), G))
    return o.swapaxes(0, 1).reshape(B, S, GLA_HEADS, GLA_DV)


def dsa_mixer(q, k, v, qi, ki, wi, rel_bias):
    B, S = q.shape[:2]
    topk = min(INDEX_TOPK, S // 4)
    nblk = S // Q_BLOCK
    key_chunk = jnp.arange(S) // CHUNK

    def blocks(t):
        return t.reshape(B, nblk, Q_BLOCK, *t.shape[2:]).swapaxes(0, 1)

    def one_block(args):
        blk, qb, qib, wib = args
        qpos = blk * Q_BLOCK + jnp.arange(Q_BLOCK)
        dots = jnp.einsum('bqjd,bsd->bqjs', qib, ki)
        score = jnp.einsum('bqj,bqjs->bqs', wib, jax.nn.relu(dots)).astype(jnp.float32)
        adm = key_chunk[None, :] <= (qpos // CHUNK)[:, None]
        score = jnp.where(adm[None], score, -jnp.inf)
        top_val, top_idx = lax.top_k(score, topk)
        valid = top_val > -jnp.inf
        k_sel = jax.vmap(lambda kk, ii: kk[ii])(k, top_idx)
        v_sel = jax.vmap(lambda vv, ii: vv[ii])(v, top_idx)
        logits = jnp.einsum('bqhd,bqkhd->bhqk', qb, k_sel).astype(jnp.float32) * (DSA_HD ** -0.5)
        bias = rel_bias[t5_bucket(top_idx - qpos[None, :, None])]
        logits = logits + jnp.moveaxis(bias, -1, 1).astype(jnp.float32)
        logits = jnp.where(valid[:, None], logits, -jnp.inf)
        probs = jax.nn.softmax(logits, axis=-1).astype(v.dtype)
        return jnp.einsum('bhqk,bqkhd->bqhd', probs, v_sel)

    out = lax.map(one_block, (jnp.arange(nblk), blocks(q), blocks(qi), blocks(wi)))
    return out.swapaxes(0, 1).reshape(B, S, DSA_W)


def hier_moe(h, w_group_router, b_group_router, w_expert_router, b_expert_router, w_gate, w_up, w_down):
    B, S, D = h.shape
    T = B * S
    hf = h.reshape(T, D)
    grp_logits = (hf @ w_group_router + b_group_router).astype(jnp.float32)
    grp_prob = jax.nn.softmax(grp_logits, axis=-1)
    grp = jnp.argmax(grp_logits, axis=-1)
    grp_w = jnp.take_along_axis(grp_prob, grp[:, None], axis=1)
    exp_logits = (hf @ w_expert_router + b_expert_router).astype(jnp.float32)
    exp_logits = exp_logits.reshape(T, N_GROUPS, EXPERTS_PER_GROUP)
    in_grp = jnp.take_along_axis(exp_logits, grp[:, None, None], axis=1)[:, 0]
    top_val, top_loc = lax.top_k(in_grp, TOP_K_INNER)
    wts = (grp_w * jax.nn.softmax(top_val, axis=-1)).astype(h.dtype)
    eid = grp[:, None] * EXPERTS_PER_GROUP + top_loc

    A = T * TOP_K_INNER
    flat_e = eid.reshape(A)
    flat_tok = jnp.repeat(jnp.arange(T), TOP_K_INNER)
    flat_w = wts.reshape(A)
    order = jnp.argsort(flat_e)
    se, stok, sw = flat_e[order], flat_tok[order], flat_w[order]
    counts = jnp.bincount(flat_e, length=N_EXPERTS)
    padded = ((counts + MOE_BLOCK - 1) // MOE_BLOCK) * MOE_BLOCK
    start = jnp.cumsum(counts) - counts
    pend = jnp.cumsum(padded)
    pstart = pend - padded
    dest = pstart[se] + (jnp.arange(A) - start[se])
    P = A + N_EXPERTS * MOE_BLOCK
    nblk = P // MOE_BLOCK
    row_tok = jnp.zeros((P,), jnp.int32).at[dest].set(stok)
    row_w = jnp.zeros((P,), h.dtype).at[dest].set(sw)
    blk_expert = jnp.clip(jnp.searchsorted(pend, jnp.arange(nblk) * MOE_BLOCK, side='right'), 0, N_EXPERTS - 1)

    def expert_block(args):
        e, tok = args
        xb = hf[tok]
        return (jax.nn.silu(xb @ w_gate[e]) * (xb @ w_up[e])) @ w_down[e]

    y = lax.map(expert_block, (blk_expert, row_tok.reshape(nblk, MOE_BLOCK))).reshape(P, D)
    out = jax.ops.segment_sum(y * row_w[:, None], row_tok, num_segments=T)
    return out.reshape(B, S, D)


def setup_inputs(seed: int = 0) -> dict:
    key = jax.random.key(seed)
    ks = iter(jax.random.split(key, 32))
    L, D = DEPTH, D_MODEL

    def nrm(shape, scale):
        return jax.random.normal(next(ks), shape, jnp.float32) * scale

    return {
        'x': nrm((BATCH, SEQ, D), 1.0),
        'p': nrm((DEPTH, BATCH, SEQ, PLE_DIM), 1.0),
        'norm_mix_g': 1.0 + nrm((L, D), 0.02),
        'w_in': nrm((L, D, IN_COLS), D ** -0.5),
        'gla_w_alpha': nrm((L, GLA_GATE_RANK, GLA_QK), GLA_GATE_RANK ** -0.5),
        'gla_b_alpha': nrm((L, GLA_QK), 0.1),
        'gla_norm_g': 1.0 + nrm((L, GLA_DV), 0.02),
        'w_out_gla': nrm((L, GLA_V, D), GLA_V ** -0.5),
        'q_norm_g': 1.0 + nrm((L, DSA_HD), 0.02),
        'k_norm_g': 1.0 + nrm((L, DSA_HD), 0.02),
        'rel_bias': nrm((REL_BUCKETS, DSA_HEADS), 0.5),
        'w_out_dsa': nrm((L, DSA_W, D), DSA_W ** -0.5),
        'w_branch_gate': nrm((L, D, 2 * D), D ** -0.5),
        'b_branch_gate': nrm((L, 2 * D), 0.01),
        'w_out': nrm((L, D, D), D ** -0.5),
        'norm_ffn_g': 1.0 + nrm((L, D), 0.02),
        'w_group_router': nrm((L, D, N_GROUPS), D ** -0.5),
        'b_group_router': nrm((L, N_GROUPS), 0.01),
        'w_expert_router': nrm((L, D, N_EXPERTS), D ** -0.5),
        'b_expert_router': nrm((L, N_EXPERTS), 0.01),
        'w_exp_gate': nrm((L, N_EXPERTS, D, EXPERT_FF), D ** -0.5),
        'w_exp_up': nrm((L, N_EXPERTS, D, EXPERT_FF), D ** -0.5),
        'w_exp_down': nrm((L, N_EXPERTS, EXPERT_FF, D), EXPERT_FF ** -0.5),
        'norm_ple_g': 1.0 + nrm((L, D), 0.02),
        'w_ple_gate': nrm((L, D, D), D ** -0.5),
        'b_ple_gate': nrm((L, D), 0.01),
        'w_ple_proj': nrm((L, PLE_DIM, D), PLE_DIM ** -0.5),
    }


def reference(x, p, norm_mix_g, w_in, gla_w_alpha, gla_b_alpha, gla_norm_g, w_out_gla, q_norm_g, k_norm_g, rel_bias, w_out_dsa, w_branch_gate, b_branch_gate, w_out, norm_ffn_g, w_group_router, b_group_router, w_expert_router, b_expert_router, w_exp_gate, w_exp_up, w_exp_down, norm_ple_g, w_ple_gate, b_ple_gate, w_ple_proj):
    B, S, D = x.shape
    for i in range(DEPTH):
        h = rms_norm(x, norm_mix_g[i])
        z = h @ w_in[i]
        gq, gk, gv, gr, glr, dq, dk, dv, iq, ik, iw = split_cols(z)
        q_a = gq.reshape(B, S, GLA_HEADS, GLA_DK) * (GLA_DK ** -0.5)
        k_a = gk.reshape(B, S, GLA_HEADS, GLA_DK)
        v_a = gv.reshape(B, S, GLA_HEADS, GLA_DV)
        g_a = (jax.nn.log_sigmoid(glr @ gla_w_alpha[i] + gla_b_alpha[i]) / GLA_GATE_TEMP).reshape(B, S, GLA_HEADS, GLA_DK)
        o_a = gla_mixer(q_a, k_a, v_a, g_a)
        o_a = rms_norm(o_a, gla_norm_g[i]).reshape(B, S, GLA_V) * jax.nn.silu(gr)
        y_a = o_a @ w_out_gla[i]
        q_b = rms_norm(dq.reshape(B, S, DSA_HEADS, DSA_HD), q_norm_g[i])
        k_b = rms_norm(dk.reshape(B, S, DSA_HEADS, DSA_HD), k_norm_g[i])
        v_b = dv.reshape(B, S, DSA_HEADS, DSA_HD)
        q_i = iq.reshape(B, S, IDX_HEADS, IDX_DIM)
        w_i = iw * ((IDX_HEADS ** -0.5) * (IDX_DIM ** -0.5))
        o_b = dsa_mixer(q_b, k_b, v_b, q_i, ik, w_i, rel_bias)
        y_b = o_b @ w_out_dsa[i]
        gates = jax.nn.sigmoid(h @ w_branch_gate[i] + b_branch_gate[i])
        gate_a, gate_b = jnp.split(gates, 2, axis=-1)
        x = x + (gate_a * y_a + gate_b * y_b) @ w_out[i]
        x = x + hier_moe(rms_norm(x, norm_ffn_g[i]), w_group_router[i], b_group_router[i], w_expert_router[i], b_expert_router[i], w_exp_gate[i], w_exp_up[i], w_exp_down[i])
        ple = p[i] @ w_ple_proj[i]
        ple_gate = jax.nn.sigmoid(rms_norm(x, norm_ple_g[i]) @ w_ple_gate[i] + b_ple_gate[i])
        x = x + ple_gate * ple
    return x
```

```python
import numpy as np
from contextlib import ExitStack
import concourse.bass as bass
import concourse.mybir as mybir
from concourse.bass_utils import run_bass_kernel_spmd

F32 = mybir.dt.float32
BF16 = mybir.dt.bfloat16
I32 = mybir.dt.int32
U32 = mybir.dt.uint32
AF = mybir.ActivationFunctionType
ALU = mybir.AluOpType
AX = mybir.AxisListType

D = 2048
EPS = 1e-6


class Buf:
    def __init__(self, name):
        self.name = name
        self.w = {}
        self.r = {}
        self.sem = None


class T:
    def __init__(self, t, b):
        self.t = t
        self.b = b

    def __getitem__(self, key):
        return self.t[key]


class KB:
    def __init__(self, nc, es):
        self.nc = nc
        self.es = es
        self.eng = {"pe": nc.tensor, "act": nc.scalar, "dve": nc.vector, "pool": nc.gpsimd, "sp": nc.sync}
        self.esem = {}
        for k in self.eng:
            self.esem[k] = es.enter_context(nc.semaphore("sem_" + k))
        self.ecnt = {k: 0 for k in self.eng}
        self.gen = {k: 0 for k in self.eng}
        self.rebase_at = 20000
        self.ptiles = [[]]
        self.waited = {}
        self.dsems = []
        self.free_dsems = []
        self.stack = [es]
        self.nops = 0
        self.nwaits = 0

    def push(self, es):
        self.stack.append(es)
        self.ptiles.append([])

    def pop(self):
        self.stack.pop()
        tl = self.ptiles.pop()
        for t in tl:
            if t.b.sem is not None:
                if t.b.sem[1] < 20000:
                    self.free_dsems.append(t.b.sem)
                t.b.sem = None

    def sb(self, name, shape, dtype):
        t = self.stack[-1].enter_context(self.nc.sbuf_tensor("sb_" + name, list(shape), dtype))
        tt = T(t, Buf(name))
        self.ptiles[-1].append(tt)
        return tt

    def dram(self, name, shape, dtype, kind="Internal"):
        t = self.nc.dram_tensor(name, list(shape), dtype, kind=kind).ap()
        return T(t, Buf(name))

    def _dsem(self, b):
        if b.sem is None:
            if self.free_dsems:
                b.sem = self.free_dsems.pop()
            else:
                s = self.es.enter_context(self.nc.semaphore("dsem%d" % len(self.dsems)))
                b.sem = [s, 0]
                self.dsems.append(b.sem)
        return b.sem

    def release(self, tiles):
        for t in tiles:
            if t.b.sem is not None:
                self.free_dsems.append(t.b.sem)
                t.b.sem = None

    def _wait(self, e, deps):
        for key, (sem, val) in deps.items():
            if e == "pe" and isinstance(key, tuple) and key[0] == "pe":
                continue
            wk = (e, key)
            if self.waited.get(wk, 0) >= val:
                continue
            self.waited[wk] = val
            self.eng[e].wait_ge(sem, val)
            self.nwaits += 1

    @staticmethod
    def _merge(d, key, sem, val):
        if key not in d or d[key][1] < val:
            d[key] = (sem, val)

    def _deps(self, r, w):
        deps = {}
        for t in r:
            for key, (sem, val) in t.b.w.items():
                self._merge(deps, key, sem, val)
        for t in w:
            for key, (sem, val) in t.b.w.items():
                self._merge(deps, key, sem, val)
            for key, (sem, val) in t.b.r.items():
                self._merge(deps, key, sem, val)
        return deps

    def op(self, e, fn, r=(), w=()):
        self._wait(e, self._deps(r, w))
        if self.ecnt[e] >= self.rebase_at:
            self.gen[e] += 1
            self.esem[e] = self.es.enter_context(self.nc.semaphore("sem_%s_g%d" % (e, self.gen[e])))
            self.ecnt[e] = 0
        inst = fn(self.eng[e])
        self.ecnt[e] += 1
        inst.then_inc(self.esem[e], 1)
        self.nops += 1
        key = (e, self.gen[e])
        for t in r:
            self._merge(t.b.r, key, self.esem[e], self.ecnt[e])
        for t in w:
            self._merge(t.b.w, key, self.esem[e], self.ecnt[e])
        return inst

    def dma(self, q, out, in_, r=(), w=(), indirect=None, **kw):
        self._wait(q, self._deps(r, w))
        ds = self._dsem(w[0].b)
        if indirect is not None:
            inst = self.eng[q].indirect_dma_start(out=out, in_=in_, **indirect)
        else:
            inst = self.eng[q].dma_start(out=out, in_=in_, **kw)
        ds[1] += 16
        inst.then_inc(ds[0], 16)
        self.nops += 1
        key = id(ds)
        for t in r:
            self._merge(t.b.r, key, ds[0], ds[1])
        for t in w:
            self._merge(t.b.w, key, ds[0], ds[1])
        return inst

    def barrier(self, engines=("pe", "act", "dve", "pool", "sp")):
        deps = {}
        for k in self.eng:
            if self.ecnt[k] > 0:
                deps[(k, self.gen[k])] = (self.esem[k], self.ecnt[k])
        for ds in self.dsems:
            if ds[1] > 0:
                deps[id(ds)] = (ds[0], ds[1])
        for e in engines:
            d2 = {kk: v for kk, v in deps.items() if not (isinstance(kk, tuple) and kk[0] == e)}
            self._wait(e, d2)
        for k in self.eng:
            if self.ecnt[k] > self.rebase_at:
                self.gen[k] += 1
                self.esem[k] = self.es.enter_context(self.nc.semaphore("sem_%s_g%d" % (k, self.gen[k])))
                self.ecnt[k] = 0


class CFG:
    def __init__(self, NT=32, G=8, debug=False, phases=99):
        self.NT = NT
        self.G = G
        self.debug = debug
        self.phases = phases
        self.nit = 22
        self.cap = 256
        self.nexp = 64
        self.rebase_at = 20000


O_GQ, O_GK, O_GV, O_GR, O_GLR, O_DQ, O_DK, O_DV, O_IQ, O_IK, O_IW = (
    0, 512, 1024, 2048, 3072, 3088, 4112, 5136, 6160, 7184, 7248)
IN_COLS = 7264

C_GMIX, C_GFFN, C_GPLE, C_BBG, C_GGLA, C_GQ, C_GK, C_PREV, C_PREB, C_NCOL = 0, 16, 32, 48, 80, 81, 82, 83, 84, 96


def build(cfg):
    NT, G = cfg.NT, cfg.G
    TO = NT * 128
    TV = 2 * TO
    GT = G * 128
    NG = NT // G
    NH = GT // 512
    nc = bass.Bass("TRN2", target_bir_lowering=False)
    es = ExitStack()
    k = KB(nc, es)
    k.rebase_at = cfg.rebase_at
    okind = "ExternalOutput" if cfg.debug else "Internal"

    def din(name, shape, dt=F32):
        return k.dram(name, shape, dt, kind="ExternalInput")

    x_pre = din("x_pre", [TO, D])
    x_own = din("x_own", [TO, D])
    p_own = din("p_own", [TO, 256])
    cstd = din("cst", [128, C_NCOL])
    identd = din("ident", [128, 128])
    w_in = din("w_in", [D, IN_COLS])
    w_bg = din("w_bg", [D, 2 * D])
    w_alpha = din("w_alpha", [16, 512])
    b_alpha = din("b_alpha", [1, 512])
    trid = din("tri", [128, 128])
    out_d = k.dram("out", [TO, D], F32, kind="ExternalOutput")
    oaT_d = k.dram("oaT_d", [1024, TO], BF16, okind)
    tbd = din("tb", [128, 8, 2, 128])
    cbd = din("cb", [128, 8])
    obT_d = k.dram("obT_d", [1024, TO], BF16, okind)
    w_oa = din("w_oa", [1024, D])
    w_ob = din("w_ob", [1024, D])
    w_o = din("w_o", [D, D])
    w_r = din("w_r", [D, 72])
    b_r = din("b_r", [1, 72])
    gffn_row = din("gffn_row", [1, D])
    ebased = din("ebase", [128, 64])
    CAP = cfg.cap
    mT_d = k.dram("mT_d", [D, TO], BF16, okind)
    x1_d = k.dram("x1_d", [TO, D], F32, okind)
    Xd = k.dram("Xd", [64 * CAP, D], BF16, "Internal")
    slots_o = k.dram("slots_o", [128, NT, 2], I32, okind)
    wts_o = k.dram("wts_o", [128, NT, 2], F32, okind)
    if cfg.phases >= 5:
        w_eg = din("w_eg", [64, D, 512])
        w_eu = din("w_eu", [64, D, 512])
        w_ed = din("w_ed", [64, 512, D])
    w_pg = din("w_pg", [D, D])
    w_pp = din("w_pp", [256, D])
    b_pg = din("b_pg", [1, D])
    gple_row = din("gple_row", [1, D])
    Yd = k.dram("Yd", [64 * CAP, D], F32, "Internal")
    slots_i = k.sb("slots_i", [128, NT, 2], I32)
    wts = k.sb("wts", [128, NT, 2], F32)

    grT_d = k.dram("grT_d", [1024, TO], BF16, okind)
    qT_d = k.dram("qT_d", [8, 128, TO], BF16, okind)
    KT_d = k.dram("KT_d", [8, 128, TV], BF16, okind)
    iqT_d = k.dram("iqT_d", [1024, TO], BF16, okind)
    gateT_d = k.dram("gateT_d", [4096, TO], BF16, okind)
    ikT_d = k.dram("ikT_d", [64, TV], BF16, okind)
    glrT_d = k.dram("glrT_d", [16, TV], F32, okind)
    q_d = k.dram("q_d", [TO, 512], BF16, okind)
    k_d = k.dram("k_d", [TV, 512], BF16, okind)
    v_d = k.dram("v_d", [TV, 1024], BF16, okind)
    iw_d = k.dram("iw_d", [TO, 16], F32, okind)
    Vd = k.dram("Vd", [8, 128, 2 * NT, 129], BF16, okind)

    cst = k.sb("cst", [128, C_NCOL], F32)
    identf = k.sb("identf", [128, 128], F32)
    identb = k.sb("identb", [128, 128], BF16)
    onesb = k.sb("onesb", [128, 128], BF16)
    epsc = k.sb("epsc", [128, 1], F32)
    k.dma("sp", cst[:], cstd[:], w=[cst])
    k.dma("sp", identf[:], identd[:], w=[identf])
    k.op("dve", lambda e: e.tensor_copy(out=identb[:], in_=identf[:]), r=[identf], w=[identb])
    k.op("dve", lambda e: e.memset(onesb[:], 1.0 / 128.0), w=[onesb])
    k.op("dve", lambda e: e.memset(epsc[:], EPS), w=[epsc])

    psum = []
    for i in range(8):
        t = es.enter_context(nc.psum_tensor("ps%d" % i, [128, 512], F32))
        psum.append(T(t, Buf("ps%d" % i)))
    psi = [0]

    def nps():
        p = psum[psi[0] % 8]
        psi[0] += 1
        return p

    def phase1():
        ph = ExitStack()
        k.push(ph)
        hT = k.sb("hT", [128, 16, GT], BF16)
        wt = [k.sb("wt%d" % i, [128, 16, 512], BF16) for i in range(2)]
        wsm = k.sb("wsm", [128, 16, 96], BF16)
        xt = [k.sb("xt%d" % i, [128, D], F32) for i in range(2)]
        xn = [k.sb("xn%d" % i, [128, D], BF16) for i in range(2)]
        junk = k.sb("junk", [128, D], BF16)
        ss = [k.sb("ss%d" % i, [128, 4], F32) for i in range(2)]
        stg = [k.sb("stg%d" % i, [128, 512], BF16) for i in range(4)]
        stgf = [k.sb("stgf%d" % i, [128, 512], F32) for i in range(2)]
        sq = [k.sb("sq%d" % i, [128, 512], BF16) for i in range(2)]
        rr = [k.sb("rr%d" % i, [128, 512], F32) for i in range(2)]
        cnt = {"stg": 0, "stgf": 0, "sq": 0, "wt": 0, "x": 0, "stv": 0}
        stv = [k.sb("stv%d" % i, [128, 4, 129], BF16) for i in range(2)]
        for i_ in range(2):
            k.op("dve", lambda e: e.memset(stv[i_][:], 1.0), w=[stv[i_]])

        w_in_v = w_in.t.rearrange("(c p) n -> p c n", p=128)
        w_bg_v = w_bg.t.rearrange("(c p) n -> p c n", p=128)
        k.dma("pool", wsm[:, :, 0:64], w_in_v[:, :, O_IK:O_IK + 64], r=[w_in], w=[wsm])
        k.dma("pool", wsm[:, :, 64:80], w_in_v[:, :, O_GLR:O_GLR + 16], r=[w_in], w=[wsm])
        k.dma("pool", wsm[:, :, 80:96], w_in_v[:, :, O_IW:O_IW + 16], r=[w_in], w=[wsm])

        def load_w(src_v, col0):
            t = wt[cnt["wt"] % 2]
            cnt["wt"] += 1
            k.dma("pool", t[:], src_v[:, :, col0:col0 + 512], w=[t])
            return t

        def stage():
            t = stg[cnt["stg"] % 4]
            cnt["stg"] += 1
            return t

        def fm_mm(ps, wtile, c0, ncol, hf):
            for c in range(16):
                k.op("pe", lambda e: e.matmul(ps[0:ncol, :], lhsT=wtile[:, c, c0:c0 + ncol],
                                              rhs=hT[:, c, hf * 512:(hf + 1) * 512],
                                              start=(c == 0), stop=(c == 15)),
                     r=[wtile, hT], w=[ps])

        def tm_mm(ps, wtile, c0, ncol, j):
            for c in range(16):
                k.op("pe", lambda e: e.matmul(ps[:, 0:ncol], lhsT=hT[:, c, j * 128:(j + 1) * 128],
                                              rhs=wtile[:, c, c0:c0 + ncol],
                                              start=(c == 0), stop=(c == 15)),
                     r=[wtile, hT], w=[ps])

        def headnorm(ps, gcol, scale, dst_ap, dst_t):
            s = sq[cnt["sq"] % 2]
            r_ = rr[cnt["sq"] % 2]
            cnt["sq"] += 1
            k.op("act", lambda e: e.activation(out=s[:], in_=ps[:], func=AF.Square), r=[ps], w=[s])
            ps2 = nps()
            k.op("pe", lambda e: e.matmul(ps2[:], lhsT=onesb[:], rhs=s[:], start=True, stop=True),
                 r=[onesb, s], w=[ps2])
            k.op("act", lambda e: e.activation(out=r_[:], in_=ps2[:], func=AF.Sqrt, bias=epsc[:, 0:1], scale=1.0),
                 r=[ps2, epsc], w=[r_])
            k.op("dve", lambda e: e.reciprocal(out=r_[:], in_=r_[:]), r=[r_], w=[r_])
            st = stage()
            if scale != 1.0:
                k.op("dve", lambda e: e.tensor_scalar(out=r_[:], in0=r_[:], scalar1=cst[:, gcol:gcol + 1], scalar2=float(scale),
                                                      op0=ALU.mult, op1=ALU.mult), r=[r_, cst], w=[r_])
            else:
                k.op("dve", lambda e: e.tensor_scalar(out=r_[:], in0=r_[:], scalar1=cst[:, gcol:gcol + 1], scalar2=None,
                                                      op0=ALU.mult), r=[r_, cst], w=[r_])
            k.op("dve", lambda e: e.tensor_tensor(out=st[:], in0=ps[:], in1=r_[:], op=ALU.mult), r=[ps, r_], w=[st])
            k.dma("sp", dst_ap, st[:], r=[st], w=[dst_t])

        for half in range(2):
            own = half == 1
            xsrc = x_own if own else x_pre
            for g in range(NG):
                tok_o = g * GT
                tok_v = half * TO + tok_o
                for j in range(G):
                    i = cnt["x"] % 2
                    cnt["x"] += 1
                    r0 = tok_o + j * 128
                    k.dma("sp", xt[i][:], xsrc[r0:r0 + 128, :], r=[xsrc], w=[xt[i]])
                    k.op("act", lambda e: e.activation(out=junk[:], in_=xt[i][:], func=AF.Square, accum_out=ss[i][:, 0:1]),
                         r=[xt[i]], w=[junk, ss[i]])
                    k.op("act", lambda e: e.activation(out=ss[i][:, 1:2], in_=ss[i][:, 0:1], func=AF.Sqrt, bias=epsc[:, 0:1],
                                                       scale=1.0 / D), r=[ss[i], epsc], w=[ss[i]])
                    k.op("dve", lambda e: e.reciprocal(out=ss[i][:, 2:3], in_=ss[i][:, 1:2]), r=[ss[i]], w=[ss[i]])
                    k.op("dve", lambda e: e.tensor_scalar(out=xn[i][:], in0=xt[i][:], scalar1=ss[i][:, 2:3], scalar2=None,
                                                          op0=ALU.mult), r=[xt[i], ss[i]], w=[xn[i]])
                    for hh in range(2):
                        ps = nps()
                        psb = ps.t[:].bitcast(BF16).rearrange("p (c n) -> p c n", n=128)
                        for c in range(8):
                            cc = hh * 8 + c
                            k.op("pe", lambda e: e.transpose(out=psb[:, c, :], in_=xn[i][:, cc * 128:(cc + 1) * 128],
                                                             identity=identb[:]), r=[xn[i], identb], w=[ps])
                        gb = cst[:, C_GMIX + hh * 8:C_GMIX + hh * 8 + 8].unsqueeze(2).to_broadcast([128, 8, 128])
                        k.op("dve", lambda e: e.tensor_tensor(out=hT[:, hh * 8:hh * 8 + 8, j * 128:(j + 1) * 128], in0=psb,
                                                              in1=gb, op=ALU.mult), r=[ps, cst], w=[hT])

                for hf in range(NH):
                    ps = nps()
                    fm_mm(ps, wsm, 0, 80, hf)
                    st = stage()
                    k.op("act", lambda e: e.activation(out=st[0:64, :], in_=ps[0:64, :], func=AF.Copy), r=[ps], w=[st])
                    c0 = tok_v + hf * 512
                    k.dma("sp", ikT_d[:, c0:c0 + 512], st[0:64, :], r=[st], w=[ikT_d])
                    sf = stgf[cnt["stgf"] % 2]
                    cnt["stgf"] += 1
                    k.op("dve", lambda e: e.tensor_copy(out=sf[64:80, :], in_=ps[64:80, :]), r=[ps], w=[sf])
                    k.dma("sp", glrT_d[:, c0:c0 + 512], sf[64:80, :], r=[sf], w=[glrT_d])
                if own:
                    for j in range(G):
                        ps = nps()
                        tm_mm(ps, wsm, 80, 16, j)
                        sf = stgf[cnt["stgf"] % 2]
                        cnt["stgf"] += 1
                        k.op("dve", lambda e: e.tensor_copy(out=sf[:, 0:16], in_=ps[:, 0:16]), r=[ps], w=[sf])
                        r0 = tok_o + j * 128
                        k.dma("sp", iw_d[r0:r0 + 128, :], sf[:, 0:16], r=[sf], w=[iw_d])

                blocks = []
                if own:
                    blocks.append(("tm", w_in_v, O_GQ, ("q", 0)))
                blocks.append(("tm", w_in_v, O_GK, ("k", 0)))
                blocks.append(("tm", w_in_v, O_GV, ("v", 0)))
                blocks.append(("tm", w_in_v, O_GV + 512, ("v", 512)))
                if own:
                    blocks.append(("fm", w_in_v, O_GR, ("gr", 0)))
                    blocks.append(("fm", w_in_v, O_GR + 512, ("gr", 4)))
                    blocks.append(("fm", w_in_v, O_DQ, ("dq", 0)))
                    blocks.append(("fm", w_in_v, O_DQ + 512, ("dq", 4)))
                blocks.append(("fm", w_in_v, O_DK, ("dk", 0)))
                blocks.append(("fm", w_in_v, O_DK + 512, ("dk", 4)))
                blocks.append(("tm", w_in_v, O_DV, ("dv", 0)))
                blocks.append(("tm", w_in_v, O_DV + 512, ("dv", 4)))
                if own:
                    blocks.append(("fm", w_in_v, O_IQ, ("iq", 0)))
                    blocks.append(("fm", w_in_v, O_IQ + 512, ("iq", 4)))
                    for bgi in range(8):
                        blocks.append(("fm", w_bg_v, bgi * 512, ("bg", bgi * 4)))

                nxt = load_w(blocks[0][1], blocks[0][2])
                for bi, (kind, src, col0, (nm, off)) in enumerate(blocks):
                    wtile = nxt
                    if bi + 1 < len(blocks):
                        nxt = load_w(blocks[bi + 1][1], blocks[bi + 1][2])
                    if kind == "tm":
                        for j in range(G):
                            ps = nps()
                            tm_mm(ps, wtile, 0, 512, j)
                            if nm == "dv":
                                sv = stv[cnt["stv"] % 2]
                                cnt["stv"] += 1
                                k.op("dve", lambda e: e.tensor_copy(out=sv[:, :, 0:128], in_=ps[:].rearrange("p (h d) -> p h d", d=128)),
                                     r=[ps], w=[sv])
                                vt = half * NT + g * G + j
                                dst = Vd[off:off + 4, :, vt, :].rearrange("h p d -> p h d")
                                k.dma("sp", dst, sv[:], r=[sv], w=[Vd])
                                continue
                            st = stage()
                            if (j % 2) == 0:
                                k.op("act", lambda e: e.activation(out=st[:], in_=ps[:], func=AF.Copy), r=[ps], w=[st])
                            else:
                                k.op("dve", lambda e: e.tensor_copy(out=st[:], in_=ps[:]), r=[ps], w=[st])
                            ro = tok_o + j * 128
                            rv = tok_v + j * 128
                            if nm == "q":
                                k.dma("sp", q_d[ro:ro + 128, :], st[:], r=[st], w=[q_d])
                            elif nm == "k":
                                k.dma("sp", k_d[rv:rv + 128, :], st[:], r=[st], w=[k_d])
                            elif nm == "v":
                                k.dma("sp", v_d[rv:rv + 128, off:off + 512], st[:], r=[st], w=[v_d])
                    else:
                        for sbi in range(4):
                            for hf in range(NH):
                                ps = nps()
                                fm_mm(ps, wtile, sbi * 128, 128, hf)
                                blk = off + sbi
                                co = tok_o + hf * 512
                                cv = tok_v + hf * 512
                                if nm == "gr":
                                    st = stage()
                                    k.op("act", lambda e: e.activation(out=st[:], in_=ps[:], func=AF.Silu), r=[ps], w=[st])
                                    k.dma("sp", grT_d[blk * 128:(blk + 1) * 128, co:co + 512], st[:], r=[st], w=[grT_d])
                                elif nm == "iq":
                                    st = stage()
                                    k.op("dve", lambda e: e.tensor_copy(out=st[:], in_=ps[:]), r=[ps], w=[st])
                                    k.dma("sp", iqT_d[blk * 128:(blk + 1) * 128, co:co + 512], st[:], r=[st], w=[iqT_d])
                                elif nm == "bg":
                                    st = stage()
                                    k.op("act", lambda e: e.activation(out=st[:], in_=ps[:], func=AF.Sigmoid,
                                                                       bias=cst[:, C_BBG + blk:C_BBG + blk + 1], scale=1.0),
                                         r=[ps, cst], w=[st])
                                    k.dma("sp", gateT_d[blk * 128:(blk + 1) * 128, co:co + 512], st[:], r=[st], w=[gateT_d])
                                elif nm == "dq":
                                    headnorm(ps, C_GQ, 128.0 ** -0.5, qT_d[blk, :, co:co + 512], qT_d)
                                elif nm == "dk":
                                    headnorm(ps, C_GK, 1.0, KT_d[blk, :, cv:cv + 512], KT_d)
                k.barrier()
        k.barrier()
        k.pop()
        ph.close()

    def phase2():
        ph = ExitStack()
        k.push(ph)
        wal = k.sb("wal", [16, 512], F32)
        bal = k.sb("bal", [1, 512], F32)
        onesrow = k.sb("onesrow", [1, 128], F32)
        onescol = k.sb("onescol", [128, 1], F32)
        onec = k.sb("onec", [128, 1], F32)
        trif = k.sb("trif", [128, 128], F32)
        trib = k.sb("trib", [128, 4, 128], BF16)
        S = k.sb("S", [64, 8, 128], F32)
        Sb = k.sb("Sb", [64, 8, 128], BF16)
        k.dma("sp", wal[:], w_alpha[:], w=[wal])
        k.dma("sp", bal[:], b_alpha[:], w=[bal])
        k.dma("sp", trif[:], trid[:], w=[trif])
        k.op("dve", lambda e: e.memset(onesrow[:], 1.0), w=[onesrow])
        k.op("dve", lambda e: e.memset(onescol[:], 1.0), w=[onescol])
        k.op("dve", lambda e: e.memset(onec[:], 1.0), w=[onec])
        k.op("dve", lambda e: e.memset(S[:], 0.0), w=[S])
        k.op("dve", lambda e: e.memset(Sb[:], 0.0), w=[Sb])
        for u in range(4):
            k.op("dve", lambda e: e.tensor_copy(out=trib[:, u, :], in_=trif[:]), r=[trif], w=[trib])
        NB = 2
        kt_ = [k.sb("kt%d" % i, [128, 512], BF16) for i in range(NB)]
        vt_ = [k.sb("vt%d" % i, [128, 1024], BF16) for i in range(NB)]
        qt_ = [k.sb("qt%d" % i, [128, 512], BF16) for i in range(NB)]
        gl_ = [k.sb("gl%d" % i, [16, 128], F32) for i in range(NB)]
        gr_ = [k.sb("gr%d" % i, [128, 8, 128], BF16) for i in range(NB)]
        e1 = k.sb("e1", [128, 512], F32)
        spt = k.sb("spt", [128, 512], F32)
        Ep = k.sb("Ep", [128, 512], F32)
        Em = k.sb("Em", [128, 512], F32)
        ktl = k.sb("ktl", [128, 512], BF16)
        qtl = k.sb("qtl", [128, 512], BF16)
        kTt = k.sb("kTt", [64, 8, 128], BF16)
        qTt = k.sb("qTt", [64, 8, 128], BF16)
        egl = k.sb("egl", [64, 8], F32)
        ATs = k.sb("ATs", [128, 8, 128], BF16)
        sq2 = k.sb("sq2", [128, 512], BF16)
        rr2 = k.sb("rr2", [128, 512], F32)
        ost = [k.sb("ost%d" % i, [128, 4, 128], BF16) for i in range(2)]
        grv = grT_d.t.rearrange("(h p) t -> p h t", p=128)
        oav = oaT_d.t.rearrange("(h p) t -> p h t", p=128)

        def loads(vt):
            i = vt % NB
            own = vt >= NT
            r0 = vt * 128
            k.dma("sp", kt_[i][:], k_d[r0:r0 + 128, :], r=[k_d], w=[kt_[i]])
            k.dma("sp", vt_[i][:], v_d[r0:r0 + 128, :], r=[v_d], w=[vt_[i]])
            k.dma("sp", gl_[i][:], glrT_d[:, r0:r0 + 128], r=[glrT_d], w=[gl_[i]])
            if own:
                ro = r0 - TO
                k.dma("sp", qt_[i][:], q_d[ro:ro + 128, :], r=[q_d], w=[qt_[i]])
                k.dma("sp", gr_[i][:], grv[:, :, ro:ro + 128], r=[grT_d], w=[gr_[i]])

        loads(0)
        for vt in range(2 * NT):
            i = vt % NB
            own = vt >= NT
            ro = vt * 128 - TO
            if vt + 1 < 2 * NT:
                loads(vt + 1)
            if vt == NT:
                k.op("dve", lambda e: e.tensor_scalar(out=S[:], in0=S[:], scalar1=cst[0:64, C_PREV:C_PREV + 1], scalar2=None,
                                                      op0=ALU.mult), r=[S, cst], w=[S])
                k.op("dve", lambda e: e.tensor_copy(out=Sb[:], in_=S[:]), r=[S], w=[Sb])
            ps = nps()
            k.op("pe", lambda e: e.matmul(ps[:], lhsT=gl_[i][:], rhs=wal[:], start=True, stop=False), r=[gl_[i], wal], w=[ps])
            k.op("pe", lambda e: e.matmul(ps[:], lhsT=onesrow[:], rhs=bal[:], start=False, stop=True), r=[onesrow, bal], w=[ps])
            k.op("act", lambda e: e.activation(out=e1[:], in_=ps[:], func=AF.Exp, scale=-1.0), r=[ps], w=[e1])
            k.op("act", lambda e: e.activation(out=spt[:], in_=e1[:], func=AF.Ln, bias=onec[:, 0:1], scale=1.0), r=[e1, onec], w=[spt])
            psG = nps()
            k.op("pe", lambda e: e.matmul(psG[:], lhsT=trif[:], rhs=spt[:], start=True, stop=True), r=[trif, spt], w=[psG])
            k.op("act", lambda e: e.activation(out=Em[:], in_=psG[:], func=AF.Exp, scale=1.0 / 16.0), r=[psG], w=[Em])
            if own:
                k.op("act", lambda e: e.activation(out=Ep[:], in_=psG[:], func=AF.Exp, scale=-1.0 / 16.0), r=[psG], w=[Ep])
            psE = nps()
            for h in range(8):
                k.op("pe", lambda e: e.matmul(psE[0:64, h:h + 1], lhsT=spt[:, h * 64:(h + 1) * 64], rhs=onescol[:], start=True, stop=True),
                     r=[spt, onescol], w=[psE])
            k.op("act", lambda e: e.activation(out=egl[:], in_=psE[0:64, 0:8], func=AF.Exp, scale=-1.0 / 16.0), r=[psE], w=[egl])
            k.op("dve", lambda e: e.tensor_tensor(out=ktl[:], in0=kt_[i][:], in1=Em[:], op=ALU.mult), r=[kt_[i], Em], w=[ktl])
            if own:
                k.op("dve", lambda e: e.scalar_tensor_tensor(out=qtl[:], in0=qt_[i][:], scalar=0.125, in1=Ep[:], op0=ALU.mult, op1=ALU.mult),
                     r=[qt_[i], Ep], w=[qtl])
                for (src, dst) in ((ktl, kTt), (qtl, qTt)):
                    pst = nps()
                    pstb = pst.t[:].bitcast(BF16).rearrange("p (c n) -> p c n", n=128)
                    for h in range(8):
                        k.op("pe", lambda e: e.transpose(out=pstb[0:64, h, :], in_=src[:, h * 64:(h + 1) * 64], identity=identb[:]),
                             r=[src, identb], w=[pst])
                    k.op("dve", lambda e: e.tensor_copy(out=dst[:], in_=pstb[0:64, :, :]), r=[pst], w=[dst])
                for hg in range(2):
                    psA = nps()
                    for u in range(4):
                        h = hg * 4 + u
                        k.op("pe", lambda e: e.matmul(psA[:, u * 128:(u + 1) * 128], lhsT=kTt[:, h, :], rhs=qTt[:, h, :], start=True, stop=True),
                             r=[kTt, qTt], w=[psA])
                    k.op("dve", lambda e: e.tensor_tensor(out=ATs[:, hg * 4:hg * 4 + 4, :], in0=psA[:].rearrange("p (u n) -> p u n", n=128),
                                                          in1=trib[:], op=ALU.mult), r=[psA, trib], w=[ATs])
                for hg in range(2):
                    psO = nps()
                    for u in range(4):
                        h = hg * 4 + u
                        k.op("pe", lambda e: e.matmul(psO[:, u * 128:(u + 1) * 128], lhsT=vt_[i][:, h * 128:(h + 1) * 128], rhs=ATs[:, h, :],
                                                      start=True, stop=False), r=[vt_[i], ATs], w=[psO])
                        k.op("pe", lambda e: e.matmul(psO[:, u * 128:(u + 1) * 128], lhsT=Sb[:, h, :], rhs=qTt[:, h, :],
                                                      start=False, stop=True), r=[Sb, qTt], w=[psO])
                    k.op("act", lambda e: e.activation(out=sq2[:], in_=psO[:], func=AF.Square), r=[psO], w=[sq2])
                    psM = nps()
                    k.op("pe", lambda e: e.matmul(psM[:], lhsT=onesb[:], rhs=sq2[:], start=True, stop=True), r=[onesb, sq2], w=[psM])
                    k.op("act", lambda e: e.activation(out=rr2[:], in_=psM[:], func=AF.Sqrt, bias=epsc[:, 0:1], scale=1.0), r=[psM, epsc], w=[rr2])
                    k.op("dve", lambda e: e.reciprocal(out=rr2[:], in_=rr2[:]), r=[rr2], w=[rr2])
                    k.op("dve", lambda e: e.scalar_tensor_tensor(out=rr2[:], in0=rr2[:], scalar=cst[:, C_GGLA:C_GGLA + 1],
                                                                 in1=gr_[i][:, hg * 4:hg * 4 + 4, :].rearrange("p u n -> p (u n)"),
                                                                 op0=ALU.mult, op1=ALU.mult), r=[rr2, cst, gr_[i]], w=[rr2])
                    o_ = ost[hg]
                    k.op("dve", lambda e: e.tensor_tensor(out=o_[:].rearrange("p u n -> p (u n)"), in0=psO[:], in1=rr2[:], op=ALU.mult),
                         r=[psO, rr2], w=[o_])
                    k.dma("sp", oav[:, hg * 4:hg * 4 + 4, ro:ro + 128], o_[:], r=[o_], w=[oaT_d])
            for hg in range(2):
                psU = nps()
                for u in range(4):
                    h = hg * 4 + u
                    k.op("pe", lambda e: e.matmul(psU[0:64, u * 128:(u + 1) * 128], lhsT=ktl[:, h * 64:(h + 1) * 64], rhs=vt_[i][:, h * 128:(h + 1) * 128],
                                                  start=True, stop=True), r=[ktl, vt_[i]], w=[psU])
                k.op("dve", lambda e: e.tensor_tensor(out=S[:, hg * 4:hg * 4 + 4, :], in0=psU[0:64, :].rearrange("p (u n) -> p u n", n=128),
                                                      in1=S[:, hg * 4:hg * 4 + 4, :], op=ALU.add), r=[psU, S], w=[S])
            k.op("dve", lambda e: e.tensor_tensor(out=S[:], in0=S[:], in1=egl[:].unsqueeze(2).to_broadcast([64, 8, 128]), op=ALU.mult),
                 r=[S, egl], w=[S])
            k.op("dve", lambda e: e.tensor_copy(out=Sb[:], in_=S[:]), r=[S], w=[Sb])
        k.barrier()
        k.pop()
        ph.close()

    def phase3():
        ph = ExitStack()
        k.push(ph)
        NKM = TV
        NKP = NKM + 128
        NIT = cfg.nit
        CK = 16
        ik2 = k.sb("ik2", [128, TV], BF16)
        k.dma("sp", ik2[0:64, :], ikT_d[:, :], r=[ikT_d], w=[ik2])
        k.dma("sp", ik2[64:128, :], ikT_d[:, :], r=[ikT_d], w=[ik2])
        tbf = k.sb("tbf", [128, 8, 2, 128], F32)
        cb = k.sb("cb", [128, 8], F32)
        tbb = k.sb("tbb", [128, 8, 2, 128], BF16)
        k.dma("sp", tbf[:], tbd[:], w=[tbf])
        k.dma("sp", cb[:], cbd[:], w=[cb])
        k.op("dve", lambda e: e.tensor_tensor(out=tbb[:].rearrange("p h a q -> p h (a q)"), in0=tbf[:].rearrange("p h a q -> p h (a q)"),
                                              in1=cb[:].unsqueeze(2).to_broadcast([128, 8, 256]), op=ALU.subtract), r=[tbf, cb], w=[tbb])
        sc = [k.sb("sc%d" % i, [128, NKP], F32) for i in range(2)]
        mk = k.sb("mk", [128, NKP], BF16)
        mkT = k.sb("mkT", [128, 2 * NT, 128], BF16)
        rl = [k.sb("rl%d" % i, [128, 512], BF16) for i in range(4)]
        iq = [k.sb("iq%d" % i, [128, 8, 128], BF16) for i in range(3)]
        iwt = [k.sb("iwt%d" % i, [128, 16], F32) for i in range(3)]
        qTt = [k.sb("qTd%d" % i, [128, 8, 128], BF16) for i in range(3)]
        aw = k.sb("aw", [128, 16], F32)
        sg = k.sb("sg", [128, 16], F32)
        dg = k.sb("dg", [128, 16, 128], BF16)
        gm = k.sb("gm", [128, 256], F32)
        bs = k.sb("bs", [128, 8], F32)
        NCB = 4
        kth = [k.sb("kth%d" % i, [128, CK * 128], BF16) for i in range(NCB)]
        vh = [k.sb("vh%d" % i, [128, CK, 129], BF16) for i in range(NCB)]
        pe_ = [k.sb("pe%d" % i, [128, 4, 128], BF16) for i in range(3)]
        pT = [k.sb("pT%d" % i, [128, 4, 128], BF16) for i in range(3)]
        obr = k.sb("obr", [128, 8, 129], F32)
        ob = k.sb("ob", [128, 8, 128], BF16)
        obT = k.sb("obT", [128, 8, 128], BF16)
        rden = k.sb("rden", [128, 8], F32)
        iqv = iqT_d.t.rearrange("(c p) t -> p c t", p=128)
        qTv = qT_d.t.rearrange("h p t -> p h t")
        obv = obT_d.t.rearrange("(h p) t -> p h t", p=128)
        p4c = [0]

        def nps4():
            p = psum[p4c[0] % 4]
            p4c[0] += 1
            return p
        cn = {"rl": 0, "pp": 0, "cb": 0, "ss": 0}

        def geo(t):
            vt = NT + t
            nkt = vt + 1
            return vt, nkt, nkt * 128

        def tloads(t):
            i = t % 3
            ro = t * 128
            k.dma("sp", iq[i][:], iqv[:, :, ro:ro + 128], r=[iqT_d], w=[iq[i]])
            k.dma("sp", iwt[i][:], iw_d[ro:ro + 128, :], r=[iw_d], w=[iwt[i]])
            k.dma("sp", qTt[i][:], qTv[:, :, ro:ro + 128], r=[qT_d], w=[qTt[i]])

        def stageA(t):
            i = t % 3
            vt, nkt, NK = geo(t)
            scb = sc[t % 2]
            nkb = (NK + 511) // 512
            k.op("act", lambda e: e.activation(out=aw[:], in_=iwt[i][:], func=AF.Abs, scale=1.0 / 32.0), r=[iwt[i]], w=[aw])
            k.op("act", lambda e: e.activation(out=sg[:], in_=iwt[i][:], func=AF.Sign), r=[iwt[i]], w=[sg])
            for j in range(16):
                k.op("pool", lambda e: e.tensor_scalar(out=dg[:, j, :], in0=identb[:], scalar1=sg[:, j:j + 1], scalar2=None, op0=ALU.mult),
                     r=[identb, sg], w=[dg])
            for kb in range(nkb):
                wd = min(512, NK - kb * 512)
                c0 = kb * 512
                psS = psum[4 + (cn["ss"] % 2)]
                cn["ss"] += 1
                pend = []

                def acc(jj, r__):
                    k.op("pe", lambda e: e.matmul(psS[:, 0:wd], lhsT=dg[:, jj, :], rhs=r__[:, 0:wd], start=(jj == 0), stop=(jj == 15)),
                         r=[dg, r__], w=[psS])
                for j in range(16):
                    c = j // 2
                    po = (j % 2) * 64
                    ps = nps4()
                    k.op("pe", lambda e: e.matmul(ps[:, 0:wd], lhsT=iq[i][po:po + 64, c, :], rhs=ik2[po:po + 64, c0:c0 + wd],
                                                  start=True, stop=True), r=[iq[i], ik2], w=[ps])
                    r_ = rl[cn["rl"] % 4]
                    cn["rl"] += 1
                    k.op("act", lambda e: e.activation(out=r_[:, 0:wd], in_=ps[:, 0:wd], func=AF.Relu, scale=aw[:, j:j + 1]),
                         r=[ps, aw], w=[r_])
                    pend.append((j, r_))
                    if len(pend) > 2:
                        acc(*pend.pop(0))
                while pend:
                    acc(*pend.pop(0))
                if c0 < TO:
                    k.op("act", lambda e: e.activation(out=scb[:, c0:c0 + wd], in_=psS[:, 0:wd], func=AF.Identity,
                                                       bias=cst[:, C_PREB:C_PREB + 1], scale=1.0), r=[psS, cst], w=[scb])
                else:
                    k.op("act", lambda e: e.activation(out=scb[:, c0:c0 + wd], in_=psS[:, 0:wd], func=AF.Copy), r=[psS], w=[scb])

        def stageB(t):
            vt, nkt, NK = geo(t)
            scb = sc[t % 2]
            k.op("dve", lambda e: e.memset(scb[0:64, NK - 64:NK], -1e30), w=[scb])
            NKp = NK
            if nkt % 2 == 1:
                k.op("dve", lambda e: e.memset(scb[:, NK:NK + 128], -1e30), w=[scb])
                NKp = NK + 128
            k.op("dve", lambda e: e.tensor_reduce(out=gm[:], in_=scb[:, 0:NKp].rearrange("p (a g) -> p g a", g=256), axis=AX.X, op=ALU.max),
                 r=[scb], w=[gm])
            k.op("dve", lambda e: e.tensor_reduce(out=bs[:, 0:1], in_=gm[:], axis=AX.X, op=ALU.min), r=[gm], w=[bs])
            k.op("dve", lambda e: e.tensor_reduce(out=bs[:, 1:2], in_=gm[:], axis=AX.X, op=ALU.max), r=[gm], w=[bs])
            k.op("dve", lambda e: e.tensor_tensor(out=bs[:, 2:3], in0=bs[:, 1:2], in1=bs[:, 0:1], op=ALU.subtract), r=[bs], w=[bs])
            for it in range(NIT):
                ck = 2.0 ** -(it + 1)
                k.op("dve", lambda e: e.scalar_tensor_tensor(out=bs[:, 3:4], in0=bs[:, 2:3], scalar=ck, in1=bs[:, 0:1], op0=ALU.mult, op1=ALU.add),
                     r=[bs], w=[bs])
                k.op("dve", lambda e: e.tensor_scalar(out=mk[:, 0:NK], in0=scb[:, 0:NK], scalar1=bs[:, 3:4], scalar2=0.0, op0=ALU.is_ge, op1=ALU.add,
                                                      accum_out=bs[:, 4:5]), r=[scb, bs], w=[mk, bs])
                k.op("dve", lambda e: e.tensor_scalar(out=bs[:, 5:6], in0=bs[:, 4:5], scalar1=255.5, scalar2=ck, op0=ALU.is_ge, op1=ALU.mult),
                     r=[bs], w=[bs])
                k.op("dve", lambda e: e.scalar_tensor_tensor(out=bs[:, 0:1], in0=bs[:, 5:6], scalar=bs[:, 2:3], in1=bs[:, 0:1], op0=ALU.mult, op1=ALU.add),
                     r=[bs], w=[bs])
            k.op("dve", lambda e: e.tensor_scalar(out=bs[:, 6:7], in0=bs[:, 0:1], scalar1=-1e29, scalar2=None, op0=ALU.max), r=[bs], w=[bs])
            k.op("dve", lambda e: e.tensor_scalar(out=mk[:, 0:NK], in0=scb[:, 0:NK], scalar1=bs[:, 6:7], scalar2=None, op0=ALU.is_ge),
                 r=[scb, bs], w=[mk])

        def maskT(t):
            vt, nkt, NK = geo(t)
            for g8 in range((nkt + 7) // 8):
                n = min(8, nkt - g8 * 8)
                ps = nps4()
                psb = ps.t[:].bitcast(BF16).rearrange("p (c n) -> p c n", n=128)
                for u in range(n):
                    kt = g8 * 8 + u
                    k.op("pe", lambda e: e.transpose(out=psb[:, u, :], in_=mk[:, kt * 128:(kt + 1) * 128], identity=identb[:]),
                         r=[mk, identb], w=[ps])
                k.op("act", lambda e: e.activation(out=mkT[:, g8 * 8:g8 * 8 + n, :], in_=psb[:, 0:n, :], func=AF.Copy), r=[ps], w=[mkT])

        def attn(t):
            i = t % 3
            ro = t * 128
            vt, nkt, NK = geo(t)
            nck = (nkt + CK - 1) // CK
            seq = [(h, c_) for h in range(8) for c_ in range(nck)]
            bufs = {}

            def cload(idx):
                h, c_ = seq[idx]
                bi = cn["cb"] % NCB
                cn["cb"] += 1
                k0 = c_ * CK
                n_ = min(CK, nkt - k0)
                k.dma("sp", kth[bi][:, 0:n_ * 128], KT_d[h, :, k0 * 128:(k0 + n_) * 128], r=[KT_d], w=[kth[bi]])
                k.dma("sp", vh[bi][:, 0:n_, :], Vd[h, :, k0:k0 + n_, :], r=[Vd], w=[vh[bi]])
                bufs[idx] = bi
            for idx in range(min(2, len(seq))):
                cload(idx)
            prev = None
            state = {}

            def pv(st):
                hh, kts, p_, bi, psO = st
                for u, kt in enumerate(kts):
                    k.op("pe", lambda e: e.matmul(psO[:, 0:129], lhsT=p_[:, u, :], rhs=vh[bi][:, kt % CK, :],
                                                  start=(kt == 0), stop=(kt == nkt - 1)), r=[p_, vh[bi]], w=[psO])
                if kts[-1] == nkt - 1:
                    k.op("act", lambda e: e.activation(out=obr[:, hh, :], in_=psO[:, 0:129], func=AF.Copy), r=[psO], w=[obr])
            for idx, (h, c_) in enumerate(seq):
                if idx + 2 < len(seq):
                    cload(idx + 2)
                bi = bufs[idx]
                psO = psum[6 + (h % 2)]
                k0 = c_ * CK
                n_ = min(CK, nkt - k0)
                for kg in range((n_ + 3) // 4):
                    kts = [k0 + kg * 4 + u for u in range(min(4, n_ - kg * 4))]
                    n = len(kts)
                    ps = nps4()
                    for u, kt in enumerate(kts):
                        near = kt >= vt - 1
                        kl = kt - k0
                        k.op("pe", lambda e: e.matmul(ps[:, u * 128:(u + 1) * 128], lhsT=kth[bi][:, kl * 128:(kl + 1) * 128], rhs=qTt[i][:, h, :],
                                                      start=True, stop=not near), r=[kth[bi], qTt[i]], w=[ps])
                        if near:
                            a = vt - kt
                            k.op("pe", lambda e: e.matmul(ps[:, u * 128:(u + 1) * 128], lhsT=identb[:], rhs=tbb[:, h, a, :],
                                                          start=False, stop=True), r=[identb, tbb], w=[ps])
                    e_ = pe_[cn["pp"] % 3]
                    p_ = pT[cn["pp"] % 3]
                    cn["pp"] += 1
                    k.op("act", lambda e: e.activation(out=e_[:, 0:n, :].rearrange("p u n -> p (u n)"), in_=ps[:, 0:n * 128], func=AF.Exp,
                                                       bias=cb[:, h:h + 1], scale=1.0), r=[ps, cb], w=[e_])
                    k.op("pool", lambda e: e.tensor_tensor(out=p_[:, 0:n, :], in0=e_[:, 0:n, :], in1=mkT[:, kts[0]:kts[0] + n, :], op=ALU.mult),
                         r=[e_, mkT], w=[p_])
                    cur = (h, kts, p_, bi, psO)
                    if prev is not None:
                        pv(prev)
                    prev = cur
            pv(prev)
            k.op("dve", lambda e: e.reciprocal(out=rden[:], in_=obr[:, :, 128]), r=[obr], w=[rden])
            k.op("dve", lambda e: e.tensor_tensor(out=ob[:], in0=obr[:, :, 0:128], in1=rden[:].unsqueeze(2).to_broadcast([128, 8, 128]), op=ALU.mult),
                 r=[obr, rden], w=[ob])
            ps = nps4()
            psb = ps.t[:].bitcast(BF16).rearrange("p (c n) -> p c n", n=128)
            for h in range(8):
                k.op("pe", lambda e: e.transpose(out=psb[:, h, :], in_=ob[:, h, :], identity=identb[:]), r=[ob, identb], w=[ps])
            k.op("act", lambda e: e.activation(out=obT[:], in_=psb, func=AF.Copy), r=[ps], w=[obT])
            k.dma("sp", obv[:, :, ro:ro + 128], obT[:], r=[obT], w=[obT_d])

        tloads(0)
        if NT > 1:
            tloads(1)
        stageA(0)
        stageB(0)
        if NT > 1:
            stageA(1)
        for t in range(NT):
            if t + 2 < NT:
                tloads(t + 2)
            maskT(t)
            if t + 1 < NT:
                stageB(t + 1)
            attn(t)
            if t + 2 < NT:
                stageA(t + 2)
        k.barrier()
        k.pop()
        ph.close()

    def phase4a():
        ph = ExitStack()
        k.push(ph)
        wog = k.sb("wog", [128, 8, D], BF16)
        wod = k.sb("wod", [128, 8, D], BF16)
        for c in range(8):
            k.dma("pool", wog[:, c, :], w_oa[c * 128:(c + 1) * 128, :], r=[w_oa], w=[wog])
            k.dma("pool", wod[:, c, :], w_ob[c * 128:(c + 1) * 128, :], r=[w_ob], w=[wod])
        oag = [k.sb("oag%d" % i, [128, 8, 512], BF16) for i in range(2)]
        obg = [k.sb("obg%d" % i, [128, 8, 512], BF16) for i in range(2)]
        gt = [k.sb("gt%d" % i, [128, 2, 512], BF16) for i in range(3)]
        t1 = [k.sb("t1_%d" % i, [128, 512], F32) for i in range(2)]
        t2 = [k.sb("t2_%d" % i, [128, 512], F32) for i in range(2)]
        mst = [k.sb("mst%d" % i, [128, 512], BF16) for i in range(3)]
        oav = oaT_d.t.rearrange("(h p) t -> p h t", p=128)
        obv = obT_d.t.rearrange("(h p) t -> p h t", p=128)
        gtv = gateT_d.t.rearrange("(a c p) t -> p a c t", a=2, p=128)
        n = 0
        for g4 in range(NT // 4):
            t0 = g4 * 512
            i = g4 % 2
            k.dma("sp", oag[i][:], oav[:, :, t0:t0 + 512], r=[oaT_d], w=[oag[i]])
            k.dma("sp", obg[i][:], obv[:, :, t0:t0 + 512], r=[obT_d], w=[obg[i]])
            for cbk in range(16):
                g_ = gt[n % 3]
                k.dma("sp", g_[:], gtv[:, :, cbk, t0:t0 + 512], r=[gateT_d], w=[g_])
                psA = nps()
                for kc in range(8):
                    k.op("pe", lambda e: e.matmul(psA[:], lhsT=wog[:, kc, cbk * 128:(cbk + 1) * 128], rhs=oag[i][:, kc, :],
                                                  start=(kc == 0), stop=(kc == 7)), r=[wog, oag[i]], w=[psA])
                psB = nps()
                for kc in range(8):
                    k.op("pe", lambda e: e.matmul(psB[:], lhsT=wod[:, kc, cbk * 128:(cbk + 1) * 128], rhs=obg[i][:, kc, :],
                                                  start=(kc == 0), stop=(kc == 7)), r=[wod, obg[i]], w=[psB])
                a_ = t1[n % 2]
                b_ = t2[n % 2]
                m_ = mst[n % 3]
                k.op("dve", lambda e: e.tensor_tensor(out=a_[:], in0=psA[:], in1=g_[:, 0, :], op=ALU.mult), r=[psA, g_], w=[a_])
                k.op("dve", lambda e: e.tensor_tensor(out=b_[:], in0=psB[:], in1=g_[:, 1, :], op=ALU.mult), r=[psB, g_], w=[b_])
                k.op("pool", lambda e: e.tensor_tensor(out=m_[:], in0=a_[:], in1=b_[:], op=ALU.add), r=[a_, b_], w=[m_])
                k.dma("sp", mT_d[cbk * 128:(cbk + 1) * 128, t0:t0 + 512], m_[:], r=[m_], w=[mT_d])
                n += 1
        k.barrier()
        k.pop()
        ph.close()

    def phase4b():
        ph = ExitStack()
        k.push(ph)
        wo = k.sb("wo", [128, 16, D], BF16)
        for c in range(16):
            k.dma("pool", wo[:, c, :], w_o[c * 128:(c + 1) * 128, :], r=[w_o], w=[wo])
        wr = k.sb("wr", [128, 16, 72], BF16)
        k.dma("pool", wr[:], w_r.t.rearrange("(c p) n -> p c n", p=128), r=[w_r], w=[wr])
        brb = k.sb("brb", [1, 72], BF16)
        k.dma("pool", brb[:], b_r[:], r=[b_r], w=[brb])
        onesrb = k.sb("onesrb", [1, 128], BF16)
        k.op("dve", lambda e: e.memset(onesrb[:], 1.0), w=[onesrb])
        gffn = k.sb("gffn", [128, D], F32)
        k.dma("sp", gffn[:], gffn_row.t.partition_broadcast(128), r=[gffn_row], w=[gffn])
        trif = k.sb("trif4", [128, 128], F32)
        k.dma("sp", trif[:], trid[:], w=[trif])
        stri = k.sb("stri", [128, 128], BF16)
        k.op("dve", lambda e: e.tensor_tensor(out=stri[:], in0=trif[:], in1=identf[:], op=ALU.subtract), r=[trif, identf], w=[stri])
        ones1 = k.sb("ones1", [128, 128], BF16)
        k.op("dve", lambda e: e.memset(ones1[:], 1.0), w=[ones1])
        ebase = k.sb("ebase", [128, 64], F32)
        k.dma("sp", ebase[:], ebased[:], w=[ebase])
        run = k.sb("run", [128, 64], F32)
        k.op("dve", lambda e: e.memset(run[:], 0.0), w=[run])
        mt = [k.sb("mt%d" % i, [128, 16, 128], BF16) for i in range(2)]
        xt = [k.sb("x4_%d" % i, [128, D], F32) for i in range(2)]
        junk = k.sb("junk4", [128, D], BF16)
        hf = [k.sb("hf%d" % i, [128, D], BF16) for i in range(2)]
        hfT = k.sb("hfT", [128, 16, 128], BF16)
        rs = k.sb("rs4", [128, 16], F32)
        lgt = k.sb("lgt", [128, 72], F32)
        ohg = k.sb("ohg", [128, 8], F32)
        pen = k.sb("pen", [128, 8], F32)
        ml = k.sb("ml", [128, 64], F32)
        ml2 = k.sb("ml2", [128, 64], F32)
        s1 = k.sb("s1", [128, 64], F32)
        s2 = k.sb("s2", [128, 64], F32)
        selb = k.sb("selb", [128, 64], BF16)
        pos = k.sb("pos", [128, 64], F32)
        junk64 = k.sb("junk64", [128, 64], F32)
        slf = k.sb("slf", [128, 2], F32)
        mtv = mT_d.t.rearrange("(c p) t -> p c t", p=128)

        def loads(t):
            i = t % 2
            ro = t * 128
            k.dma("sp", mt[i][:], mtv[:, :, ro:ro + 128], r=[mT_d], w=[mt[i]])
            k.dma("sp", xt[i][:], x_own[ro:ro + 128, :], r=[x_own], w=[xt[i]])
        loads(0)
        for t in range(NT):
            i = t % 2
            ro = t * 128
            if t + 1 < NT:
                loads(t + 1)
            for nb in range(4):
                ps = nps()
                for c in range(16):
                    k.op("pe", lambda e: e.matmul(ps[:], lhsT=mt[i][:, c, :], rhs=wo[:, c, nb * 512:(nb + 1) * 512],
                                                  start=(c == 0), stop=(c == 15)), r=[mt[i], wo], w=[ps])
                k.op("dve", lambda e: e.tensor_tensor(out=xt[i][:, nb * 512:(nb + 1) * 512], in0=ps[:], in1=xt[i][:, nb * 512:(nb + 1) * 512],
                                                      op=ALU.add), r=[ps, xt[i]], w=[xt[i]])
            k.dma("sp", x1_d[ro:ro + 128, :], xt[i][:], r=[xt[i]], w=[x1_d])
            k.op("act", lambda e: e.activation(out=junk[:], in_=xt[i][:], func=AF.Square, accum_out=rs[:, 0:1]), r=[xt[i]], w=[junk, rs])
            k.op("act", lambda e: e.activation(out=rs[:, 1:2], in_=rs[:, 0:1], func=AF.Sqrt, bias=epsc[:, 0:1], scale=1.0 / D), r=[rs, epsc], w=[rs])
            k.op("dve", lambda e: e.reciprocal(out=rs[:, 2:3], in_=rs[:, 1:2]), r=[rs], w=[rs])
            k.op("dve", lambda e: e.scalar_tensor_tensor(out=hf[i][:], in0=xt[i][:], scalar=rs[:, 2:3], in1=gffn[:], op0=ALU.mult, op1=ALU.mult),
                 r=[xt[i], rs, gffn], w=[hf[i]])
            for hh in range(2):
                ps = nps()
                psb = ps.t[:].bitcast(BF16).rearrange("p (c n) -> p c n", n=128)
                for c in range(8):
                    cc = hh * 8 + c
                    k.op("pe", lambda e: e.transpose(out=psb[:, c, :], in_=hf[i][:, cc * 128:(cc + 1) * 128], identity=identb[:]),
                         r=[hf[i], identb], w=[ps])
                if hh == 0:
                    k.op("act", lambda e: e.activation(out=hfT[:, 0:8, :], in_=psb, func=AF.Copy), r=[ps], w=[hfT])
                else:
                    k.op("dve", lambda e: e.tensor_copy(out=hfT[:, 8:16, :], in_=psb), r=[ps], w=[hfT])
            ps = nps()
            for c in range(16):
                k.op("pe", lambda e: e.matmul(ps[:, 0:72], lhsT=hfT[:, c, :], rhs=wr[:, c, :], start=(c == 0), stop=False), r=[hfT, wr], w=[ps])
            k.op("pe", lambda e: e.matmul(ps[:, 0:72], lhsT=onesrb[:], rhs=brb[:], start=False, stop=True), r=[onesrb, brb], w=[ps])
            k.op("dve", lambda e: e.tensor_copy(out=lgt[:], in_=ps[:, 0:72]), r=[ps], w=[lgt])
            k.op("dve", lambda e: e.tensor_reduce(out=rs[:, 3:4], in_=lgt[:, 0:8], axis=AX.X, op=ALU.max), r=[lgt], w=[rs])
            k.op("dve", lambda e: e.tensor_scalar(out=ohg[:], in0=lgt[:, 0:8], scalar1=rs[:, 3:4], scalar2=None, op0=ALU.is_ge), r=[lgt, rs], w=[ohg])
            k.op("dve", lambda e: e.tensor_scalar(out=rs[:, 4:5], in0=rs[:, 3:4], scalar1=-1.0, scalar2=None, op0=ALU.mult), r=[rs], w=[rs])
            k.op("act", lambda e: e.activation(out=pen[:], in_=lgt[:, 0:8], func=AF.Exp, bias=rs[:, 4:5], scale=1.0, accum_out=rs[:, 5:6]),
                 r=[lgt, rs], w=[pen, rs])
            k.op("dve", lambda e: e.reciprocal(out=rs[:, 6:7], in_=rs[:, 5:6]), r=[rs], w=[rs])
            k.op("dve", lambda e: e.tensor_scalar(out=pen[:], in0=ohg[:], scalar1=1e9, scalar2=-1e9, op0=ALU.mult, op1=ALU.add), r=[ohg], w=[pen])
            k.op("dve", lambda e: e.tensor_tensor(out=ml[:].rearrange("p (g e) -> p g e", e=8), in0=lgt[:, 8:72].rearrange("p (g e) -> p g e", e=8),
                                                  in1=pen[:].unsqueeze(2).to_broadcast([128, 8, 8]), op=ALU.add), r=[lgt, pen], w=[ml])
            k.op("dve", lambda e: e.tensor_reduce(out=rs[:, 7:8], in_=ml[:], axis=AX.X, op=ALU.max), r=[ml], w=[rs])
            k.op("dve", lambda e: e.tensor_scalar(out=s1[:], in0=ml[:], scalar1=rs[:, 7:8], scalar2=None, op0=ALU.is_ge), r=[ml, rs], w=[s1])
            k.op("dve", lambda e: e.scalar_tensor_tensor(out=ml2[:], in0=s1[:], scalar=-1e9, in1=ml[:], op0=ALU.mult, op1=ALU.add), r=[s1, ml], w=[ml2])
            k.op("dve", lambda e: e.tensor_reduce(out=rs[:, 8:9], in_=ml2[:], axis=AX.X, op=ALU.max), r=[ml2], w=[rs])
            k.op("dve", lambda e: e.tensor_scalar(out=s2[:], in0=ml2[:], scalar1=rs[:, 8:9], scalar2=None, op0=ALU.is_ge), r=[ml2, rs], w=[s2])
            k.op("dve", lambda e: e.tensor_tensor(out=rs[:, 9:10], in0=rs[:, 8:9], in1=rs[:, 7:8], op=ALU.subtract), r=[rs], w=[rs])
            k.op("act", lambda e: e.activation(out=rs[:, 10:11], in_=rs[:, 9:10], func=AF.Exp), r=[rs], w=[rs])
            k.op("dve", lambda e: e.tensor_scalar(out=rs[:, 11:12], in0=rs[:, 10:11], scalar1=1.0, scalar2=None, op0=ALU.add), r=[rs], w=[rs])
            k.op("dve", lambda e: e.reciprocal(out=rs[:, 12:13], in_=rs[:, 11:12]), r=[rs], w=[rs])
            k.op("dve", lambda e: e.tensor_tensor(out=wts[:, t, 0:1], in0=rs[:, 6:7], in1=rs[:, 12:13], op=ALU.mult), r=[rs], w=[wts])
            k.op("dve", lambda e: e.tensor_tensor(out=wts[:, t, 1:2], in0=wts[:, t, 0:1], in1=rs[:, 10:11], op=ALU.mult), r=[rs, wts], w=[wts])
            k.op("dve", lambda e: e.tensor_tensor(out=selb[:], in0=s1[:], in1=s2[:], op=ALU.add), r=[s1, s2], w=[selb])
            psP = nps()
            k.op("pe", lambda e: e.matmul(psP[:, 0:64], lhsT=stri[:], rhs=selb[:], start=True, stop=True), r=[stri, selb], w=[psP])
            psC = nps()
            k.op("pe", lambda e: e.matmul(psC[:, 0:64], lhsT=ones1[:], rhs=selb[:], start=True, stop=True), r=[ones1, selb], w=[psC])
            k.op("dve", lambda e: e.tensor_tensor(out=pos[:], in0=psP[:, 0:64], in1=run[:], op=ALU.add), r=[psP, run], w=[pos])
            k.op("dve", lambda e: e.tensor_tensor(out=run[:], in0=psC[:, 0:64], in1=run[:], op=ALU.add), r=[psC, run], w=[run])
            k.op("dve", lambda e: e.scalar_tensor_tensor(out=pos[:], in0=pos[:], scalar=float(CAP - 1), in1=ebase[:], op0=ALU.min, op1=ALU.add),
                 r=[pos, ebase], w=[pos])
            for (sx, col) in ((s1, 0), (s2, 1)):
                k.op("dve", lambda e: e.tensor_tensor(out=junk64[:], in0=sx[:], in1=pos[:], op=ALU.mult), r=[sx, pos], w=[junk64])
                k.op("dve", lambda e: e.tensor_reduce(out=slf[:, col:col + 1], in_=junk64[:], axis=AX.X, op=ALU.add), r=[junk64], w=[slf])
            k.op("dve", lambda e: e.tensor_copy(out=slots_i[:, t, :], in_=slf[:]), r=[slf], w=[slots_i])
            for j in range(2):
                k.dma("pool", Xd[:, :], hf[i][:], r=[hf[i], slots_i], w=[Xd],
                      indirect=dict(out_offset=bass.IndirectOffsetOnAxis(ap=slots_i[:, t, j:j + 1], axis=0), in_offset=None))
        if cfg.debug:
            k.dma("sp", slots_o[:], slots_i[:], r=[slots_i], w=[slots_o])
            k.dma("sp", wts_o[:], wts[:], r=[wts], w=[wts_o])
        k.barrier()
        k.pop()
        ph.close()

    def phase5():
        ph = ExitStack()
        k.push(ph)
        NE = cfg.nexp
        NRB = CAP // 128
        wg = [k.sb("wg%d" % i, [128, 16, 512], BF16) for i in range(2)]
        wu = [k.sb("wu%d" % i, [128, 16, 512], BF16) for i in range(2)]
        wd = [k.sb("wd%d" % i, [128, 4, D], BF16) for i in range(2)]
        xr = [k.sb("xr%d" % i, [128, NRB, D], BF16) for i in range(2)]
        XT = k.sb("XT", [128, 16, CAP], BF16)
        sgt = [k.sb("sgt%d" % i, [128, CAP], F32) for i in range(2)]
        aT = k.sb("aT", [128, 4, CAP], BF16)
        yst = [k.sb("yst%d" % i, [128, D], F32) for i in range(2)]

        def loads(e):
            i = e % 2
            k.dma("pool", wg[i][:], w_eg.t[e].rearrange("(c p) f -> p c f", p=128), r=[w_eg], w=[wg[i]])
            k.dma("pool", wu[i][:], w_eu.t[e].rearrange("(c p) f -> p c f", p=128), r=[w_eu], w=[wu[i]])
            k.dma("pool", wd[i][:], w_ed.t[e].rearrange("(c p) n -> p c n", p=128), r=[w_ed], w=[wd[i]])
            k.dma("sp", xr[i][:], Xd.t[e * CAP:(e + 1) * CAP, :].rearrange("(b p) d -> p b d", p=128), r=[Xd], w=[xr[i]])
        loads(0)
        ny = 0
        for e_ in range(NE):
            i = e_ % 2
            if e_ + 1 < NE:
                loads(e_ + 1)
            n = 0
            for rb in range(NRB):
                for hh in range(2):
                    ps = nps()
                    psb = ps.t[:].bitcast(BF16).rearrange("p (c n) -> p c n", n=128)
                    for c in range(8):
                        cc = hh * 8 + c
                        k.op("pe", lambda e: e.transpose(out=psb[:, c, :], in_=xr[i][:, rb, cc * 128:(cc + 1) * 128], identity=identb[:]),
                             r=[xr[i], identb], w=[ps])
                    if n % 2 == 0:
                        k.op("act", lambda e: e.activation(out=XT[:, hh * 8:hh * 8 + 8, rb * 128:(rb + 1) * 128], in_=psb, func=AF.Copy), r=[ps], w=[XT])
                    else:
                        k.op("dve", lambda e: e.tensor_copy(out=XT[:, hh * 8:hh * 8 + 8, rb * 128:(rb + 1) * 128], in_=psb), r=[ps], w=[XT])
                    n += 1
            for fb in range(4):
                psg = nps()
                for c in range(16):
                    k.op("pe", lambda e: e.matmul(psg[:, 0:CAP], lhsT=wg[i][:, c, fb * 128:(fb + 1) * 128], rhs=XT[:, c, :],
                                                  start=(c == 0), stop=(c == 15)), r=[wg[i], XT], w=[psg])
                psu = nps()
                for c in range(16):
                    k.op("pe", lambda e: e.matmul(psu[:, 0:CAP], lhsT=wu[i][:, c, fb * 128:(fb + 1) * 128], rhs=XT[:, c, :],
                                                  start=(c == 0), stop=(c == 15)), r=[wu[i], XT], w=[psu])
                s_ = sgt[fb % 2]
                k.op("act", lambda e: e.activation(out=s_[:], in_=psg[:, 0:CAP], func=AF.Silu), r=[psg], w=[s_])
                k.op("dve", lambda e: e.tensor_tensor(out=aT[:, fb, :], in0=psu[:, 0:CAP], in1=s_[:], op=ALU.mult), r=[psu, s_], w=[aT])
            for rb in range(NRB):
                y_ = yst[ny % 2]
                ny += 1
                for nb in range(4):
                    ps = nps()
                    for fc in range(4):
                        k.op("pe", lambda e: e.matmul(ps[:], lhsT=aT[:, fc, rb * 128:(rb + 1) * 128], rhs=wd[i][:, fc, nb * 512:(nb + 1) * 512],
                                                      start=(fc == 0), stop=(fc == 3)), r=[aT, wd[i]], w=[ps])
                    if nb % 2 == 0:
                        k.op("act", lambda e: e.activation(out=y_[:, nb * 512:(nb + 1) * 512], in_=ps[:], func=AF.Copy), r=[ps], w=[y_])
                    else:
                        k.op("dve", lambda e: e.tensor_copy(out=y_[:, nb * 512:(nb + 1) * 512], in_=ps[:]), r=[ps], w=[y_])
                r0 = e_ * CAP + rb * 128
                k.dma("sp", Yd[r0:r0 + 128, :], y_[:], r=[y_], w=[Yd])
            if e_ % 16 == 15:
                k.barrier()
        k.barrier()
        k.pop()
        ph.close()

    def phase6():
        ph = ExitStack()
        k.push(ph)
        wpg = k.sb("wpg", [128, 16, D], BF16)
        for c in range(16):
            k.dma("pool", wpg[:, c, :], w_pg[c * 128:(c + 1) * 128, :], r=[w_pg], w=[wpg])
        wpp = k.sb("wpp", [128, 2, D], BF16)
        k.dma("pool", wpp[:], w_pp.t.rearrange("(c p) n -> p c n", p=128), r=[w_pp], w=[wpp])
        bpg = k.sb("bpg", [1, D], BF16)
        k.dma("pool", bpg[:], b_pg[:], r=[b_pg], w=[bpg])
        onesrb = k.sb("onesrb6", [1, 128], BF16)
        k.op("dve", lambda e: e.memset(onesrb[:], 1.0), w=[onesrb])
        gple = k.sb("gple", [128, D], F32)
        k.dma("sp", gple[:], gple_row.t.partition_broadcast(128), r=[gple_row], w=[gple])
        x1t = [k.sb("x1t%d" % i, [128, D], F32) for i in range(2)]
        y1t = [k.sb("y1t%d" % i, [128, D], F32) for i in range(2)]
        y2t = [k.sb("y2t%d" % i, [128, D], F32) for i in range(2)]
        pt = [k.sb("pt%d" % i, [128, 256], F32) for i in range(2)]
        ptb = k.sb("ptb", [128, 256], BF16)
        pTt = k.sb("pTt", [128, 2, 128], BF16)
        junk = k.sb("junk6", [128, D], BF16)
        hp = k.sb("hp", [128, D], BF16)
        hpT = k.sb("hpT", [128, 16, 128], BF16)
        rs = k.sb("rs6", [128, 4], F32)
        sgm = [k.sb("sgm%d" % i, [128, 512], F32) for i in range(2)]
        ot = [k.sb("ot%d" % i, [128, D], F32) for i in range(2)]

        def loads(t):
            i = t % 2
            ro = t * 128
            k.dma("sp", x1t[i][:], x1_d[ro:ro + 128, :], r=[x1_d], w=[x1t[i]])
            k.dma("sp", pt[i][:], p_own[ro:ro + 128, :], r=[p_own], w=[pt[i]])
            k.dma("pool", y1t[i][:], Yd[:, :], r=[Yd, slots_i], w=[y1t[i]],
                  indirect=dict(out_offset=None, in_offset=bass.IndirectOffsetOnAxis(ap=slots_i[:, t, 0:1], axis=0)))
            k.dma("pool", y2t[i][:], Yd[:, :], r=[Yd, slots_i], w=[y2t[i]],
                  indirect=dict(out_offset=None, in_offset=bass.IndirectOffsetOnAxis(ap=slots_i[:, t, 1:2], axis=0)))
        loads(0)
        for t in range(NT):
            i = t % 2
            ro = t * 128
            if t + 1 < NT:
                loads(t + 1)
            x2 = x1t[i]
            k.op("dve", lambda e: e.scalar_tensor_tensor(out=x2[:], in0=y1t[i][:], scalar=wts[:, t, 0:1], in1=x2[:], op0=ALU.mult, op1=ALU.add),
                 r=[y1t[i], wts, x2], w=[x2])
            k.op("dve", lambda e: e.scalar_tensor_tensor(out=x2[:], in0=y2t[i][:], scalar=wts[:, t, 1:2], in1=x2[:], op0=ALU.mult, op1=ALU.add),
                 r=[y2t[i], wts, x2], w=[x2])
            k.op("act", lambda e: e.activation(out=junk[:], in_=x2[:], func=AF.Square, accum_out=rs[:, 0:1]), r=[x2], w=[junk, rs])
            k.op("act", lambda e: e.activation(out=rs[:, 1:2], in_=rs[:, 0:1], func=AF.Sqrt, bias=epsc[:, 0:1], scale=1.0 / D), r=[rs, epsc], w=[rs])
            k.op("dve", lambda e: e.reciprocal(out=rs[:, 2:3], in_=rs[:, 1:2]), r=[rs], w=[rs])
            k.op("dve", lambda e: e.scalar_tensor_tensor(out=hp[:], in0=x2[:], scalar=rs[:, 2:3], in1=gple[:], op0=ALU.mult, op1=ALU.mult),
                 r=[x2, rs, gple], w=[hp])
            for hh in range(2):
                ps = nps()
                psb = ps.t[:].bitcast(BF16).rearrange("p (c n) -> p c n", n=128)
                for c in range(8):
                    cc = hh * 8 + c
                    k.op("pe", lambda e: e.transpose(out=psb[:, c, :], in_=hp[:, cc * 128:(cc + 1) * 128], identity=identb[:]),
                         r=[hp, identb], w=[ps])
                if hh == 0:
                    k.op("act", lambda e: e.activation(out=hpT[:, 0:8, :], in_=psb, func=AF.Copy), r=[ps], w=[hpT])
                else:
                    k.op("dve", lambda e: e.tensor_copy(out=hpT[:, 8:16, :], in_=psb), r=[ps], w=[hpT])
            k.op("dve", lambda e: e.tensor_copy(out=ptb[:], in_=pt[i][:]), r=[pt[i]], w=[ptb])
            ps = nps()
            psb = ps.t[:].bitcast(BF16).rearrange("p (c n) -> p c n", n=128)
            for c in range(2):
                k.op("pe", lambda e: e.transpose(out=psb[:, c, :], in_=ptb[:, c * 128:(c + 1) * 128], identity=identb[:]), r=[ptb, identb], w=[ps])
            k.op("dve", lambda e: e.tensor_copy(out=pTt[:], in_=psb[:, 0:2, :]), r=[ps], w=[pTt])
            o_ = ot[i]
            for nb in range(4):
                psg = nps()
                for c in range(16):
                    k.op("pe", lambda e: e.matmul(psg[:], lhsT=hpT[:, c, :], rhs=wpg[:, c, nb * 512:(nb + 1) * 512], start=(c == 0), stop=False),
                         r=[hpT, wpg], w=[psg])
                k.op("pe", lambda e: e.matmul(psg[:], lhsT=onesrb[:], rhs=bpg[:, nb * 512:(nb + 1) * 512], start=False, stop=True),
                     r=[onesrb, bpg], w=[psg])
                psp = nps()
                for c in range(2):
                    k.op("pe", lambda e: e.matmul(psp[:], lhsT=pTt[:, c, :], rhs=wpp[:, c, nb * 512:(nb + 1) * 512], start=(c == 0), stop=(c == 1)),
                         r=[pTt, wpp], w=[psp])
                s_ = sgm[nb % 2]
                k.op("act", lambda e: e.activation(out=s_[:], in_=psg[:], func=AF.Sigmoid), r=[psg], w=[s_])
                k.op("dve", lambda e: e.tensor_tensor(out=s_[:], in0=psp[:], in1=s_[:], op=ALU.mult), r=[psp, s_], w=[s_])
                k.op("pool", lambda e: e.tensor_tensor(out=o_[:, nb * 512:(nb + 1) * 512], in0=s_[:], in1=x2[:, nb * 512:(nb + 1) * 512], op=ALU.add),
                     r=[s_, x2], w=[o_])
            k.dma("sp", out_d[ro:ro + 128, :], o_[:], r=[o_], w=[out_d])
        k.barrier()
        k.pop()
        ph.close()

    phase1()
    if cfg.phases >= 2:
        phase2()
    if cfg.phases >= 3:
        phase3()
    if cfg.phases >= 4:
        phase4a()
        phase4b()
    if cfg.phases >= 5:
        phase5()
        phase6()

    k.barrier()
    return nc, es, k


def _pack_cst(inp, s):
    c = np.zeros((128, C_NCOL), np.float32)
    c[:, C_GMIX:C_GMIX + 16] = inp["norm_mix_g"][0].reshape(16, 128).T
    c[:, C_GFFN:C_GFFN + 16] = inp["norm_ffn_g"][0].reshape(16, 128).T
    c[:, C_GPLE:C_GPLE + 16] = inp["norm_ple_g"][0].reshape(16, 128).T
    c[:, C_BBG:C_BBG + 32] = inp["b_branch_gate"][0].reshape(32, 128).T
    c[:, C_GGLA] = inp["gla_norm_g"][0]
    c[:, C_GQ] = inp["q_norm_g"][0]
    c[:, C_GK] = inp["k_norm_g"][0]
    c[:, C_PREV] = 1.0 if s == 1 else 0.0
    c[:, C_PREB] = 0.0 if s == 1 else -1e30
    return c


def _t5_bucket(rel):
    half, exact = 16, 8
    sign = np.where(rel > 0, half, 0)
    n = np.abs(rel)
    nf = np.maximum(n, 1).astype(np.float32)
    large = exact + (np.log(nf / exact) / np.log(128 / exact) * (half - exact)).astype(np.int32)
    large = np.minimum(large, half - 1)
    return sign + np.where(n < exact, n, large)


def _bias_tables(rel_bias):
    s_ = np.arange(128)[:, None]
    q_ = np.arange(128)[None, :]
    tb = np.zeros((128, 8, 2, 128), np.float32)
    for a in range(2):
        bk = _t5_bucket((s_ - q_ - 128 * a).astype(np.int32))
        tb[:, :, a, :] = np.transpose(rel_bias[bk], (0, 2, 1))
    cb = np.ascontiguousarray(np.broadcast_to(rel_bias[15][None, :], (128, 8))).astype(np.float32)
    return tb, cb


def make_in_maps(inp, cfg):
    NT = cfg.NT
    TO = NT * 128
    x = inp["x"]
    p = inp["p"][0]
    ident = np.eye(128, dtype=np.float32)
    tb, cb = _bias_tables(inp["rel_bias"])
    w_r = np.ascontiguousarray(np.concatenate([inp["w_group_router"][0], inp["w_expert_router"][0]], axis=1))
    b_r = np.concatenate([inp["b_group_router"][0], inp["b_expert_router"][0]]).reshape(1, 72).astype(np.float32)
    ebase = np.ascontiguousarray(np.broadcast_to((np.arange(64, dtype=np.float32) * cfg.cap)[None, :], (128, 64)))
    maps = []
    for c in range(8):
        b, s = c // 2, c % 2
        m = {
            "x_pre": np.ascontiguousarray(x[b, 0:TO]),
            "x_own": np.ascontiguousarray(x[b, s * TO:(s + 1) * TO]),
            "p_own": np.ascontiguousarray(p[b, s * TO:(s + 1) * TO]),
            "cst": _pack_cst(inp, s),
            "ident": ident,
            "w_in": inp["w_in"][0],
            "w_bg": inp["w_branch_gate"][0],
            "w_alpha": inp["gla_w_alpha"][0],
            "b_alpha": inp["gla_b_alpha"][0].reshape(1, 512),
            "tri": np.triu(np.ones((128, 128), np.float32)),
            "tb": tb,
            "cb": cb,
            "w_oa": inp["w_out_gla"][0],
            "w_ob": inp["w_out_dsa"][0],
            "w_o": inp["w_out"][0],
            "w_r": w_r,
            "b_r": b_r,
            "gffn_row": inp["norm_ffn_g"][0].reshape(1, D),
            "ebase": ebase,
            "w_eg": inp["w_exp_gate"][0] if cfg.phases >= 5 else None,
            "w_eu": inp["w_exp_up"][0] if cfg.phases >= 5 else None,
            "w_ed": inp["w_exp_down"][0] if cfg.phases >= 5 else None,
            "w_pg": inp["w_ple_gate"][0],
            "w_pp": inp["w_ple_proj"][0],
            "b_pg": inp["b_ple_gate"][0].reshape(1, D),
            "gple_row": inp["norm_ple_g"][0].reshape(1, D),
        }
        maps.append({kk: vv for kk, vv in m.items() if vv is not None})
    return maps


def run(inp, cfg, trace=False):
    nc, es, k = build(cfg)
    maps = make_in_maps(inp, cfg)
    res = run_bass_kernel_spmd(nc, maps, core_ids=list(range(8)), trace=trace)
    es.close()
    return res


def kernel(**inputs):
    cfg = CFG(NT=32, G=8)
    inp = {k_: np.asarray(v) for k_, v in inputs.items()}
    res = run(inp, cfg)
    B, S = inp["x"].shape[:2]
    out = np.zeros((B, S, D), np.float32)
    TO = cfg.NT * 128
    for c in range(8):
        b, s = c // 2, c % 2
        out[b, s * TO:(s + 1) * TO] = res.results[c]["out"]
    return out
```

```python
import numpy as np
from contextlib import ExitStack
import concourse.bass as bass
import concourse.mybir as mybir
from concourse.bass_utils import run_bass_kernel_spmd

F32 = mybir.dt.float32
BF16 = mybir.dt.bfloat16
I32 = mybir.dt.int32
U32 = mybir.dt.uint32
AF = mybir.ActivationFunctionType
ALU = mybir.AluOpType
AX = mybir.AxisListType

D = 2048
EPS = 1e-6


class Buf:
    def __init__(self, name):
        self.name = name
        self.w = {}
        self.r = {}
        self.sem = None


class T:
    def __init__(self, t, b):
        self.t = t
        self.b = b

    def __getitem__(self, key):
        return self.t[key]


class KB:
    def __init__(self, nc, es):
        self.nc = nc
        self.es = es
        self.eng = {"pe": nc.tensor, "act": nc.scalar, "dve": nc.vector, "pool": nc.gpsimd, "sp": nc.sync}
        self.esem = {}
        for k in self.eng:
            self.esem[k] = es.enter_context(nc.semaphore("sem_" + k))
        self.ecnt = {k: 0 for k in self.eng}
        self.gen = {k: 0 for k in self.eng}
        self.rebase_at = 20000
        self.ptiles = [[]]
        self.waited = {}
        self.dsems = []
        self.free_dsems = []
        self.stack = [es]
        self.nops = 0
        self.nwaits = 0

    def push(self, es):
        self.stack.append(es)
        self.ptiles.append([])

    def pop(self):
        self.stack.pop()
        tl = self.ptiles.pop()
        for t in tl:
            if t.b.sem is not None:
                if t.b.sem[1] < 20000:
                    self.free_dsems.append(t.b.sem)
                t.b.sem = None

    def sb(self, name, shape, dtype):
        t = self.stack[-1].enter_context(self.nc.sbuf_tensor("sb_" + name, list(shape), dtype))
        tt = T(t, Buf(name))
        self.ptiles[-1].append(tt)
        return tt

    def dram(self, name, shape, dtype, kind="Internal"):
        t = self.nc.dram_tensor(name, list(shape), dtype, kind=kind).ap()
        return T(t, Buf(name))

    def _dsem(self, b):
        if b.sem is None:
            if self.free_dsems:
                b.sem = self.free_dsems.pop()
            else:
                s = self.es.enter_context(self.nc.semaphore("dsem%d" % len(self.dsems)))
                b.sem = [s, 0]
                self.dsems.append(b.sem)
        return b.sem

    def release(self, tiles):
        for t in tiles:
            if t.b.sem is not None:
                self.free_dsems.append(t.b.sem)
                t.b.sem = None

    def _wait(self, e, deps):
        for key, (sem, val) in deps.items():
            if e == "pe" and isinstance(key, tuple) and key[0] == "pe":
                continue
            wk = (e, key)
            if self.waited.get(wk, 0) >= val:
                continue
            self.waited[wk] = val
            self.eng[e].wait_ge(sem, val)
            self.nwaits += 1

    @staticmethod
    def _merge(d, key, sem, val):
        if key not in d or d[key][1] < val:
            d[key] = (sem, val)

    def _deps(self, r, w):
        deps = {}
        for t in r:
            for key, (sem, val) in t.b.w.items():
                self._merge(deps, key, sem, val)
        for t in w:
            for key, (sem, val) in t.b.w.items():
                self._merge(deps, key, sem, val)
            for key, (sem, val) in t.b.r.items():
                self._merge(deps, key, sem, val)
        return deps

    def op(self, e, fn, r=(), w=()):
        self._wait(e, self._deps(r, w))
        if self.ecnt[e] >= self.rebase_at:
            self.gen[e] += 1
            self.esem[e] = self.es.enter_context(self.nc.semaphore("sem_%s_g%d" % (e, self.gen[e])))
            self.ecnt[e] = 0
        inst = fn(self.eng[e])
        self.ecnt[e] += 1
        inst.then_inc(self.esem[e], 1)
        self.nops += 1
        key = (e, self.gen[e])
        for t in r:
            self._merge(t.b.r, key, self.esem[e], self.ecnt[e])
        for t in w:
            self._merge(t.b.w, key, self.esem[e], self.ecnt[e])
        return inst

    def dma(self, q, out, in_, r=(), w=(), indirect=None, **kw):
        self._wait(q, self._deps(r, w))
        ds = self._dsem(w[0].b)
        if indirect is not None:
            inst = self.eng[q].indirect_dma_start(out=out, in_=in_, **indirect)
        else:
            inst = self.eng[q].dma_start(out=out, in_=in_, **kw)
        ds[1] += 16
        inst.then_inc(ds[0], 16)
        self.nops += 1
        key = id(ds)
        for t in r:
            self._merge(t.b.r, key, ds[0], ds[1])
        for t in w:
            self._merge(t.b.w, key, ds[0], ds[1])
        return inst

    def barrier(self, engines=("pe", "act", "dve", "pool", "sp")):
        deps = {}
        for k in self.eng:
            if self.ecnt[k] > 0:
                deps[(k, self.gen[k])] = (self.esem[k], self.ecnt[k])
        for ds in self.dsems:
            if ds[1] > 0:
                deps[id(ds)] = (ds[0], ds[1])
        for e in engines:
            d2 = {kk: v for kk, v in deps.items() if not (isinstance(kk, tuple) and kk[0] == e)}
            self._wait(e, d2)
        for k in self.eng:
            if self.ecnt[k] > self.rebase_at:
                self.gen[k] += 1
                self.esem[k] = self.es.enter_context(self.nc.semaphore("sem_%s_g%d" % (k, self.gen[k])))
                self.ecnt[k] = 0


class CFG:
    def __init__(self, NT=32, G=8, debug=False, phases=99):
        self.NT = NT
        self.G = G
        self.debug = debug
        self.phases = phases
        self.nit = 22
        self.cap = 256
        self.nexp = 64
        self.rebase_at = 20000


O_GQ, O_GK, O_GV, O_GR, O_GLR, O_DQ, O_DK, O_DV, O_IQ, O_IK, O_IW = (
    0, 512, 1024, 2048, 3072, 3088, 4112, 5136, 6160, 7184, 7248)
IN_COLS = 7264

C_GMIX, C_GFFN, C_GPLE, C_BBG, C_GGLA, C_GQ, C_GK, C_PREV, C_PREB, C_NCOL = 0, 16, 32, 48, 80, 81, 82, 83, 84, 96


def build(cfg):
    NT, G = cfg.NT, cfg.G
    TO = NT * 128
    TV = 2 * TO
    GT = G * 128
    NG = NT // G
    NH = GT // 512
    nc = bass.Bass("TRN2", target_bir_lowering=False)
    es = ExitStack()
    k = KB(nc, es)
    k.rebase_at = cfg.rebase_at
    okind = "ExternalOutput" if cfg.debug else "Internal"

    def din(name, shape, dt=F32):
        return k.dram(name, shape, dt, kind="ExternalInput")

    x_pre = din("x_pre", [TO, D])
    x_own = din("x_own", [TO, D])
    p_own = din("p_own", [TO, 256])
    cstd = din("cst", [128, C_NCOL])
    identd = din("ident", [128, 128])
    w_in = din("w_in", [D, IN_COLS])
    w_bg = din("w_bg", [D, 2 * D])
    w_alpha = din("w_alpha", [16, 512])
    b_alpha = din("b_alpha", [1, 512])
    trid = din("tri", [128, 128])
    out_d = k.dram("out", [TO, D], F32, kind="ExternalOutput")
    oaT_d = k.dram("oaT_d", [1024, TO], BF16, okind)
    tbd = din("tb", [128, 8, 2, 128])
    cbd = din("cb", [128, 8])
    obT_d = k.dram("obT_d", [1024, TO], BF16, okind)
    w_oa = din("w_oa", [1024, D])
    w_ob = din("w_ob", [1024, D])
    w_o = din("w_o", [D, D])
    w_r = din("w_r", [D, 72])
    b_r = din("b_r", [1, 72])
    gffn_row = din("gffn_row", [1, D])
    ebased = din("ebase", [128, 64])
    CAP = cfg.cap
    mT_d = k.dram("mT_d", [D, TO], BF16, okind)
    x1_d = k.dram("x1_d", [TO, D], F32, okind)
    Xd = k.dram("Xd", [64 * CAP, D], BF16, "Internal")
    slots_o = k.dram("slots_o", [128, NT, 2], I32, okind)
    wts_o = k.dram("wts_o", [128, NT, 2], F32, okind)
    if cfg.phases >= 5:
        w_eg = din("w_eg", [64, D, 512])
        w_eu = din("w_eu", [64, D, 512])
        w_ed = din("w_ed", [64, 512, D])
    w_pg = din("w_pg", [D, D])
    w_pp = din("w_pp", [256, D])
    b_pg = din("b_pg", [1, D])
    gple_row = din("gple_row", [1, D])
    Yd = k.dram("Yd", [64 * CAP, D], F32, "Internal")
    slots_i = k.sb("slots_i", [128, NT, 2], I32)
    wts = k.sb("wts", [128, NT, 2], F32)

    grT_d = k.dram("grT_d", [1024, TO], BF16, okind)
    qT_d = k.dram("qT_d", [8, 128, TO], BF16, okind)
    KT_d = k.dram("KT_d", [8, 128, TV], BF16, okind)
    iqT_d = k.dram("iqT_d", [1024, TO], BF16, okind)
    gateT_d = k.dram("gateT_d", [4096, TO], BF16, okind)
    ikT_d = k.dram("ikT_d", [64, TV], BF16, okind)
    glrT_d = k.dram("glrT_d", [16, TV], F32, okind)
    q_d = k.dram("q_d", [TO, 512], BF16, okind)
    k_d = k.dram("k_d", [TV, 512], BF16, okind)
    v_d = k.dram("v_d", [TV, 1024], BF16, okind)
    iw_d = k.dram("iw_d", [TO, 16], F32, okind)
    Vd = k.dram("Vd", [8, 128, 2 * NT, 129], BF16, okind)

    cst = k.sb("cst", [128, C_NCOL], F32)
    identf = k.sb("identf", [128, 128], F32)
    identb = k.sb("identb", [128, 128], BF16)
    onesb = k.sb("onesb", [128, 128], BF16)
    epsc = k.sb("epsc", [128, 1], F32)
    k.dma("sp", cst[:], cstd[:], w=[cst])
    k.dma("sp", identf[:], identd[:], w=[identf])
    k.op("dve", lambda e: e.tensor_copy(out=identb[:], in_=identf[:]), r=[identf], w=[identb])
    k.op("dve", lambda e: e.memset(onesb[:], 1.0 / 128.0), w=[onesb])
    k.op("dve", lambda e: e.memset(epsc[:], EPS), w=[epsc])

    psum = []
    for i in range(8):
        t = es.enter_context(nc.psum_tensor("ps%d" % i, [128, 512], F32))
        psum.append(T(t, Buf("ps%d" % i)))
    psi = [0]

    def nps():
        p = psum[psi[0] % 8]
        psi[0] += 1
        return p

    def phase1():
        ph = ExitStack()
        k.push(ph)
        hT = k.sb("hT", [128, 16, GT], BF16)
        wt = [k.sb("wt%d" % i, [128, 16, 512], BF16) for i in range(2)]
        wsm = k.sb("wsm", [128, 16, 96], BF16)
        xt = [k.sb("xt%d" % i, [128, D], F32) for i in range(2)]
        xn = [k.sb("xn%d" % i, [128, D], BF16) for i in range(2)]
        junk = k.sb("junk", [128, D], BF16)
        ss = [k.sb("ss%d" % i, [128, 4], F32) for i in range(2)]
        stg = [k.sb("stg%d" % i, [128, 512], BF16) for i in range(4)]
        stgf = [k.sb("stgf%d" % i, [128, 512], F32) for i in range(2)]
        sq = [k.sb("sq%d" % i, [128, 512], BF16) for i in range(2)]
        rr = [k.sb("rr%d" % i, [128, 512], F32) for i in range(2)]
        cnt = {"stg": 0, "stgf": 0, "sq": 0, "wt": 0, "x": 0, "stv": 0}
        stv = [k.sb("stv%d" % i, [128, 4, 129], BF16) for i in range(2)]
        for i_ in range(2):
            k.op("dve", lambda e: e.memset(stv[i_][:], 1.0), w=[stv[i_]])

        w_in_v = w_in.t.rearrange("(c p) n -> p c n", p=128)
        w_bg_v = w_bg.t.rearrange("(c p) n -> p c n", p=128)
        k.dma("pool", wsm[:, :, 0:64], w_in_v[:, :, O_IK:O_IK + 64], r=[w_in], w=[wsm])
        k.dma("pool", wsm[:, :, 64:80], w_in_v[:, :, O_GLR:O_GLR + 16], r=[w_in], w=[wsm])
        k.dma("pool", wsm[:, :, 80:96], w_in_v[:, :, O_IW:O_IW + 16], r=[w_in], w=[wsm])

        def load_w(src_v, col0):
            t = wt[cnt["wt"] % 2]
            cnt["wt"] += 1
            k.dma("pool", t[:], src_v[:, :, col0:col0 + 512], w=[t])
            return t

        def stage():
            t = stg[cnt["stg"] % 4]
            cnt["stg"] += 1
            return t

        def fm_mm(ps, wtile, c0, ncol, hf):
            for c in range(16):
                k.op("pe", lambda e: e.matmul(ps[0:ncol, :], lhsT=wtile[:, c, c0:c0 + ncol],
                                              rhs=hT[:, c, hf * 512:(hf + 1) * 512],
                                              start=(c == 0), stop=(c == 15)),
                     r=[wtile, hT], w=[ps])

        def tm_mm(ps, wtile, c0, ncol, j):
            for c in range(16):
                k.op("pe", lambda e: e.matmul(ps[:, 0:ncol], lhsT=hT[:, c, j * 128:(j + 1) * 128],
                                              rhs=wtile[:, c, c0:c0 + ncol],
                                              start=(c == 0), stop=(c == 15)),
                     r=[wtile, hT], w=[ps])

        def headnorm(ps, gcol, scale, dst_ap, dst_t):
            s = sq[cnt["sq"] % 2]
            r_ = rr[cnt["sq"] % 2]
            cnt["sq"] += 1
            k.op("act", lambda e: e.activation(out=s[:], in_=ps[:], func=AF.Square), r=[ps], w=[s])
            ps2 = nps()
            k.op("pe", lambda e: e.matmul(ps2[:], lhsT=onesb[:], rhs=s[:], start=True, stop=True),
                 r=[onesb, s], w=[ps2])
            k.op("act", lambda e: e.activation(out=r_[:], in_=ps2[:], func=AF.Sqrt, bias=epsc[:, 0:1], scale=1.0),
                 r=[ps2, epsc], w=[r_])
            k.op("dve", lambda e: e.reciprocal(out=r_[:], in_=r_[:]), r=[r_], w=[r_])
            st = stage()
            if scale != 1.0:
                k.op("dve", lambda e: e.tensor_scalar(out=r_[:], in0=r_[:], scalar1=cst[:, gcol:gcol + 1], scalar2=float(scale),
                                                      op0=ALU.mult, op1=ALU.mult), r=[r_, cst], w=[r_])
            else:
                k.op("dve", lambda e: e.tensor_scalar(out=r_[:], in0=r_[:], scalar1=cst[:, gcol:gcol + 1], scalar2=None,
                                                      op0=ALU.mult), r=[r_, cst], w=[r_])
            k.op("dve", lambda e: e.tensor_tensor(out=st[:], in0=ps[:], in1=r_[:], op=ALU.mult), r=[ps, r_], w=[st])
            k.dma("sp", dst_ap, st[:], r=[st], w=[dst_t])

        for half in range(2):
            own = half == 1
            xsrc = x_own if own else x_pre
            for g in range(NG):
                tok_o = g * GT
                tok_v = half * TO + tok_o
                for j in range(G):
                    i = cnt["x"] % 2
                    cnt["x"] += 1
                    r0 = tok_o + j * 128
                    k.dma("sp", xt[i][:], xsrc[r0:r0 + 128, :], r=[xsrc], w=[xt[i]])
                    k.op("act", lambda e: e.activation(out=junk[:], in_=xt[i][:], func=AF.Square, accum_out=ss[i][:, 0:1]),
                         r=[xt[i]], w=[junk, ss[i]])
                    k.op("act", lambda e: e.activation(out=ss[i][:, 1:2], in_=ss[i][:, 0:1], func=AF.Sqrt, bias=epsc[:, 0:1],
                                                       scale=1.0 / D), r=[ss[i], epsc], w=[ss[i]])
                    k.op("dve", lambda e: e.reciprocal(out=ss[i][:, 2:3], in_=ss[i][:, 1:2]), r=[ss[i]], w=[ss[i]])
                    k.op("dve", lambda e: e.tensor_scalar(out=xn[i][:], in0=xt[i][:], scalar1=ss[i][:, 2:3], scalar2=None,
                                                          op0=ALU.mult), r=[xt[i], ss[i]], w=[xn[i]])
                    for hh in range(2):
                        ps = nps()
                        psb = ps.t[:].bitcast(BF16).rearrange("p (c n) -> p c n", n=128)
                        for c in range(8):
                            cc = hh * 8 + c
                            k.op("pe", lambda e: e.transpose(out=psb[:, c, :], in_=xn[i][:, cc * 128:(cc + 1) * 128],
                                                             identity=identb[:]), r=[xn[i], identb], w=[ps])
                        gb = cst[:, C_GMIX + hh * 8:C_GMIX + hh * 8 + 8].unsqueeze(2).to_broadcast([128, 8, 128])
                        k.op("dve", lambda e: e.tensor_tensor(out=hT[:, hh * 8:hh * 8 + 8, j * 128:(j + 1) * 128], in0=psb,
                                                              in1=gb, op=ALU.mult), r=[ps, cst], w=[hT])

                for hf in range(NH):
                    ps = nps()
                    fm_mm(ps, wsm, 0, 80, hf)
                    st = stage()
                    k.op("act", lambda e: e.activation(out=st[0:64, :], in_=ps[0:64, :], func=AF.Copy), r=[ps], w=[st])
                    c0 = tok_v + hf * 512
                    k.dma("sp", ikT_d[:, c0:c0 + 512], st[0:64, :], r=[st], w=[ikT_d])
                    sf = stgf[cnt["stgf"] % 2]
                    cnt["stgf"] += 1
                    k.op("dve", lambda e: e.tensor_copy(out=sf[64:80, :], in_=ps[64:80, :]), r=[ps], w=[sf])
                    k.dma("sp", glrT_d[:, c0:c0 + 512], sf[64:80, :], r=[sf], w=[glrT_d])
                if own:
                    for j in range(G):
                        ps = nps()
                        tm_mm(ps, wsm, 80, 16, j)
                        sf = stgf[cnt["stgf"] % 2]
                        cnt["stgf"] += 1
                        k.op("dve", lambda e: e.tensor_copy(out=sf[:, 0:16], in_=ps[:, 0:16]), r=[ps], w=[sf])
                        r0 = tok_o + j * 128
                        k.dma("sp", iw_d[r0:r0 + 128, :], sf[:, 0:16], r=[sf], w=[iw_d])

                blocks = []
                if own:
                    blocks.append(("tm", w_in_v, O_GQ, ("q", 0)))
                blocks.append(("tm", w_in_v, O_GK, ("k", 0)))
                blocks.append(("tm", w_in_v, O_GV, ("v", 0)))
                blocks.append(("tm", w_in_v, O_GV + 512, ("v", 512)))
                if own:
                    blocks.append(("fm", w_in_v, O_GR, ("gr", 0)))
                    blocks.append(("fm", w_in_v, O_GR + 512, ("gr", 4)))
                    blocks.append(("fm", w_in_v, O_DQ, ("dq", 0)))
                    blocks.append(("fm", w_in_v, O_DQ + 512, ("dq", 4)))
                blocks.append(("fm", w_in_v, O_DK, ("dk", 0)))
                blocks.append(("fm", w_in_v, O_DK + 512, ("dk", 4)))
                blocks.append(("tm", w_in_v, O_DV, ("dv", 0)))
                blocks.append(("tm", w_in_v, O_DV + 512, ("dv", 4)))
                if own:
                    blocks.append(("fm", w_in_v, O_IQ, ("iq", 0)))
                    blocks.append(("fm", w_in_v, O_IQ + 512, ("iq", 4)))
                    for bgi in range(8):
                        blocks.append(("fm", w_bg_v, bgi * 512, ("bg", bgi * 4)))

                nxt = load_w(blocks[0][1], blocks[0][2])
                for bi, (kind, src, col0, (nm, off)) in enumerate(blocks):
                    wtile = nxt
                    if bi + 1 < len(blocks):
                        nxt = load_w(blocks[bi + 1][1], blocks[bi + 1][2])
                    if kind == "tm":
                        for j in range(G):
                            ps = nps()
                            tm_mm(ps, wtile, 0, 512, j)
                            if nm == "dv":
                                sv = stv[cnt["stv"] % 2]
                                cnt["stv"] += 1
                                k.op("dve", lambda e: e.tensor_copy(out=sv[:, :, 0:128], in_=ps[:].rearrange("p (h d) -> p h d", d=128)),
                                     r=[ps], w=[sv])
                                vt = half * NT + g * G + j
                                dst = Vd[off:off + 4, :, vt, :].rearrange("h p d -> p h d")
                                k.dma("sp", dst, sv[:], r=[sv], w=[Vd])
                                continue
                            st = stage()
                            if (j % 2) == 0:
                                k.op("act", lambda e: e.activation(out=st[:], in_=ps[:], func=AF.Copy), r=[ps], w=[st])
                            else:
                                k.op("dve", lambda e: e.tensor_copy(out=st[:], in_=ps[:]), r=[ps], w=[st])
                            ro = tok_o + j * 128
                            rv = tok_v + j * 128
                            if nm == "q":
                                k.dma("sp", q_d[ro:ro + 128, :], st[:], r=[st], w=[q_d])
                            elif nm == "k":
                                k.dma("sp", k_d[rv:rv + 128, :], st[:], r=[st], w=[k_d])
                            elif nm == "v":
                                k.dma("sp", v_d[rv:rv + 128, off:off + 512], st[:], r=[st], w=[v_d])
                    else:
                        for sbi in range(4):
                            for hf in range(NH):
                                ps = nps()
                                fm_mm(ps, wtile, sbi * 128, 128, hf)
                                blk = off + sbi
                                co = tok_o + hf * 512
                                cv = tok_v + hf * 512
                                if nm == "gr":
                                    st = stage()
                                    k.op("act", lambda e: e.activation(out=st[:], in_=ps[:], func=AF.Silu), r=[ps], w=[st])
                                    k.dma("sp", grT_d[blk * 128:(blk + 1) * 128, co:co + 512], st[:], r=[st], w=[grT_d])
                                elif nm == "iq":
                                    st = stage()
                                    k.op("dve", lambda e: e.tensor_copy(out=st[:], in_=ps[:]), r=[ps], w=[st])
                                    k.dma("sp", iqT_d[blk * 128:(blk + 1) * 128, co:co + 512], st[:], r=[st], w=[iqT_d])
                                elif nm == "bg":
                                    st = stage()
                                    k.op("act", lambda e: e.activation(out=st[:], in_=ps[:], func=AF.Sigmoid,
                                                                       bias=cst[:, C_BBG + blk:C_BBG + blk + 1], scale=1.0),
                                         r=[ps, cst], w=[st])
                                    k.dma("sp", gateT_d[blk * 128:(blk + 1) * 128, co:co + 512], st[:], r=[st], w=[gateT_d])
                                elif nm == "dq":
                                    headnorm(ps, C_GQ, 128.0 ** -0.5, qT_d[blk, :, co:co + 512], qT_d)
                                elif nm == "dk":
                                    headnorm(ps, C_GK, 1.0, KT_d[blk, :, cv:cv + 512], KT_d)
                k.barrier()
        k.barrier()
        k.pop()
        ph.close()

    def phase2():
        ph = ExitStack()
        k.push(ph)
        wal = k.sb("wal", [16, 512], F32)
        bal = k.sb("bal", [1, 512], F32)
        onesrow = k.sb("onesrow", [1, 128], F32)
        onescol = k.sb("onescol", [128, 1], F32)
        onec = k.sb("onec", [128, 1], F32)
        trif = k.sb("trif", [128, 128], F32)
        trib = k.sb("trib", [128, 4, 128], BF16)
        S = k.sb("S", [64, 8, 128], F32)
        Sb = k.sb("Sb", [64, 8, 128], BF16)
        k.dma("sp", wal[:], w_alpha[:], w=[wal])
        k.dma("sp", bal[:], b_alpha[:], w=[bal])
        k.dma("sp", trif[:], trid[:], w=[trif])
        k.op("dve", lambda e: e.memset(onesrow[:], 1.0), w=[onesrow])
        k.op("dve", lambda e: e.memset(onescol[:], 1.0), w=[onescol])
        k.op("dve", lambda e: e.memset(onec[:], 1.0), w=[onec])
        k.op("dve", lambda e: e.memset(S[:], 0.0), w=[S])
        k.op("dve", lambda e: e.memset(Sb[:], 0.0), w=[Sb])
        for u in range(4):
            k.op("dve", lambda e: e.tensor_copy(out=trib[:, u, :], in_=trif[:]), r=[trif], w=[trib])
        NB = 2
        kt_ = [k.sb("kt%d" % i, [128, 512], BF16) for i in range(NB)]
        vt_ = [k.sb("vt%d" % i, [128, 1024], BF16) for i in range(NB)]
        qt_ = [k.sb("qt%d" % i, [128, 512], BF16) for i in range(NB)]
        gl_ = [k.sb("gl%d" % i, [16, 128], F32) for i in range(NB)]
        gr_ = [k.sb("gr%d" % i, [128, 8, 128], BF16) for i in range(NB)]
        e1 = k.sb("e1", [128, 512], F32)
        spt = k.sb("spt", [128, 512], F32)
        Ep = k.sb("Ep", [128, 512], F32)
        Em = k.sb("Em", [128, 512], F32)
        ktl = k.sb("ktl", [128, 512], BF16)
        qtl = k.sb("qtl", [128, 512], BF16)
        kTt = k.sb("kTt", [64, 8, 128], BF16)
        qTt = k.sb("qTt", [64, 8, 128], BF16)
        egl = k.sb("egl", [64, 8], F32)
        ATs = k.sb("ATs", [128, 8, 128], BF16)
        sq2 = k.sb("sq2", [128, 512], BF16)
        rr2 = k.sb("rr2", [128, 512], F32)
        ost = [k.sb("ost%d" % i, [128, 4, 128], BF16) for i in range(2)]
        grv = grT_d.t.rearrange("(h p) t -> p h t", p=128)
        oav = oaT_d.t.rearrange("(h p) t -> p h t", p=128)

        def loads(vt):
            i = vt % NB
            own = vt >= NT
            r0 = vt * 128
            k.dma("sp", kt_[i][:], k_d[r0:r0 + 128, :], r=[k_d], w=[kt_[i]])
            k.dma("sp", vt_[i][:], v_d[r0:r0 + 128, :], r=[v_d], w=[vt_[i]])
            k.dma("sp", gl_[i][:], glrT_d[:, r0:r0 + 128], r=[glrT_d], w=[gl_[i]])
            if own:
                ro = r0 - TO
                k.dma("sp", qt_[i][:], q_d[ro:ro + 128, :], r=[q_d], w=[qt_[i]])
                k.dma("sp", gr_[i][:], grv[:, :, ro:ro + 128], r=[grT_d], w=[gr_[i]])

        loads(0)
        for vt in range(2 * NT):
            i = vt % NB
            own = vt >= NT
            ro = vt * 128 - TO
            if vt + 1 < 2 * NT:
                loads(vt + 1)
            if vt == NT:
                k.op("dve", lambda e: e.tensor_scalar(out=S[:], in0=S[:], scalar1=cst[0:64, C_PREV:C_PREV + 1], scalar2=None,
                                                      op0=ALU.mult), r=[S, cst], w=[S])
                k.op("dve", lambda e: e.tensor_copy(out=Sb[:], in_=S[:]), r=[S], w=[Sb])
            ps = nps()
            k.op("pe", lambda e: e.matmul(ps[:], lhsT=gl_[i][:], rhs=wal[:], start=True, stop=False), r=[gl_[i], wal], w=[ps])
            k.op("pe", lambda e: e.matmul(ps[:], lhsT=onesrow[:], rhs=bal[:], start=False, stop=True), r=[onesrow, bal], w=[ps])
            k.op("act", lambda e: e.activation(out=e1[:], in_=ps[:], func=AF.Exp, scale=-1.0), r=[ps], w=[e1])
            k.op("act", lambda e: e.activation(out=spt[:], in_=e1[:], func=AF.Ln, bias=onec[:, 0:1], scale=1.0), r=[e1, onec], w=[spt])
            psG = nps()
            k.op("pe", lambda e: e.matmul(psG[:], lhsT=trif[:], rhs=spt[:], start=True, stop=True), r=[trif, spt], w=[psG])
            k.op("act", lambda e: e.activation(out=Em[:], in_=psG[:], func=AF.Exp, scale=1.0 / 16.0), r=[psG], w=[Em])
            if own:
                k.op("act", lambda e: e.activation(out=Ep[:], in_=psG[:], func=AF.Exp, scale=-1.0 / 16.0), r=[psG], w=[Ep])
            psE = nps()
            for h in range(8):
                k.op("pe", lambda e: e.matmul(psE[0:64, h:h + 1], lhsT=spt[:, h * 64:(h + 1) * 64], rhs=onescol[:], start=True, stop=True),
                     r=[spt, onescol], w=[psE])
            k.op("act", lambda e: e.activation(out=egl[:], in_=psE[0:64, 0:8], func=AF.Exp, scale=-1.0 / 16.0), r=[psE], w=[egl])
            k.op("dve", lambda e: e.tensor_tensor(out=ktl[:], in0=kt_[i][:], in1=Em[:], op=ALU.mult), r=[kt_[i], Em], w=[ktl])
            if own:
                k.op("dve", lambda e: e.scalar_tensor_tensor(out=qtl[:], in0=qt_[i][:], scalar=0.125, in1=Ep[:], op0=ALU.mult, op1=ALU.mult),
                     r=[qt_[i], Ep], w=[qtl])
                for (src, dst) in ((ktl, kTt), (qtl, qTt)):
                    pst = nps()
                    pstb = pst.t[:].bitcast(BF16).rearrange("p (c n) -> p c n", n=128)
                    for h in range(8):
                        k.op("pe", lambda e: e.transpose(out=pstb[0:64, h, :], in_=src[:, h * 64:(h + 1) * 64], identity=identb[:]),
                             r=[src, identb], w=[pst])
                    k.op("dve", lambda e: e.tensor_copy(out=dst[:], in_=pstb[0:64, :, :]), r=[pst], w=[dst])
                for hg in range(2):
                    psA = nps()
                    for u in range(4):
                        h = hg * 4 + u
                        k.op("pe", lambda e: e.matmul(psA[:, u * 128:(u + 1) * 128], lhsT=kTt[:, h, :], rhs=qTt[:, h, :], start=True, stop=True),
                             r=[kTt, qTt], w=[psA])
                    k.op("dve", lambda e: e.tensor_tensor(out=ATs[:, hg * 4:hg * 4 + 4, :], in0=psA[:].rearrange("p (u n) -> p u n", n=128),
                                                          in1=trib[:], op=ALU.mult), r=[psA, trib], w=[ATs])
                for hg in range(2):
                    psO = nps()
                    for u in range(4):
                        h = hg * 4 + u
                        k.op("pe", lambda e: e.matmul(psO[:, u * 128:(u + 1) * 128], lhsT=vt_[i][:, h * 128:(h + 1) * 128], rhs=ATs[:, h, :],
                                                      start=True, stop=False), r=[vt_[i], ATs], w=[psO])
                        k.op("pe", lambda e: e.matmul(psO[:, u * 128:(u + 1) * 128], lhsT=Sb[:, h, :], rhs=qTt[:, h, :],
                                                      start=False, stop=True), r=[Sb, qTt], w=[psO])
                    k.op("act", lambda e: e.activation(out=sq2[:], in_=psO[:], func=AF.Square), r=[psO], w=[sq2])
                    psM = nps()
                    k.op("pe", lambda e: e.matmul(psM[:], lhsT=onesb[:], rhs=sq2[:], start=True, stop=True), r=[onesb, sq2], w=[psM])
                    k.op("act", lambda e: e.activation(out=rr2[:], in_=psM[:], func=AF.Sqrt, bias=epsc[:, 0:1], scale=1.0), r=[psM, epsc], w=[rr2])
                    k.op("dve", lambda e: e.reciprocal(out=rr2[:], in_=rr2[:]), r=[rr2], w=[rr2])
                    k.op("dve", lambda e: e.scalar_tensor_tensor(out=rr2[:], in0=rr2[:], scalar=cst[:, C_GGLA:C_GGLA + 1],
                                                                 in1=gr_[i][:, hg * 4:hg * 4 + 4, :].rearrange("p u n -> p (u n)"),
                                                                 op0=ALU.mult, op1=ALU.mult), r=[rr2, cst, gr_[i]], w=[rr2])
                    o_ = ost[hg]
                    k.op("dve", lambda e: e.tensor_tensor(out=o_[:].rearrange("p u n -> p (u n)"), in0=psO[:], in1=rr2[:], op=ALU.mult),
                         r=[psO, rr2], w=[o_])
                    k.dma("sp", oav[:, hg * 4:hg * 4 + 4, ro:ro + 128], o_[:], r=[o_], w=[oaT_d])
            for hg in range(2):
                psU = nps()
                for u in range(4):
                    h = hg * 4 + u
                    k.op("pe", lambda e: e.matmul(psU[0:64, u * 128:(u + 1) * 128], lhsT=ktl[:, h * 64:(h + 1) * 64], rhs=vt_[i][:, h * 128:(h + 1) * 128],
                                                  start=True, stop=True), r=[ktl, vt_[i]], w=[psU])
                k.op("dve", lambda e: e.tensor_tensor(out=S[:, hg * 4:hg * 4 + 4, :], in0=psU[0:64, :].rearrange("p (u n) -> p u n", n=128),
                                                      in1=S[:, hg * 4:hg * 4 + 4, :], op=ALU.add), r=[psU, S], w=[S])
            k.op("dve", lambda e: e.tensor_tensor(out=S[:], in0=S[:], in1=egl[:].unsqueeze(2).to_broadcast([64, 8, 128]), op=ALU.mult),
                 r=[S, egl], w=[S])
            k.op("dve", lambda e: e.tensor_copy(out=Sb[:], in_=S[:]), r=[S], w=[Sb])
        k.barrier()
        k.pop()
        ph.close()

    def phase3():
        ph = ExitStack()
        k.push(ph)
        NKM = TV
        NKP = NKM + 128
        NIT = cfg.nit
        CK = 16
        ik2 = k.sb("ik2", [128, TV], BF16)
        k.dma("sp", ik2[0:64, :], ikT_d[:, :], r=[ikT_d], w=[ik2])
        k.dma("sp", ik2[64:128, :], ikT_d[:, :], r=[ikT_d], w=[ik2])
        cb = k.sb("cb", [128, 8], F32)
        tbb = k.sb("tbb", [128, 8, 2, 128], BF16)
        k.dma("pool", tbb[:], tbd[:], w=[tbb])
        k.dma("sp", cb[:], cbd[:], w=[cb])
        k.op("dve", lambda e: e.tensor_tensor(out=tbb[:].rearrange("p h a q -> p h (a q)"), in0=tbb[:].rearrange("p h a q -> p h (a q)"),
                                              in1=cb[:].unsqueeze(2).to_broadcast([128, 8, 256]), op=ALU.subtract), r=[tbb, cb], w=[tbb])
        sc = [k.sb("sc%d" % i, [128, NKP], F32) for i in range(2)]
        mk = k.sb("mk", [128, NKP], BF16)
        mkT = k.sb("mkT", [128, 2 * NT, 128], BF16)
        rl = [k.sb("rl%d" % i, [128, 512], BF16) for i in range(4)]
        iq = [k.sb("iq%d" % i, [128, 2, 8, 128], BF16) for i in range(3)]
        for i_ in range(3):
            k.op("pool", lambda e: e.memset(iq[i_][:], 0.0), w=[iq[i_]])
        iwt = [k.sb("iwt%d" % i, [128, 16], F32) for i in range(3)]
        qTt = [k.sb("qTd%d" % i, [128, 8, 128], BF16) for i in range(3)]
        aw = k.sb("aw", [128, 16], F32)
        sg = k.sb("sg", [128, 16], F32)
        dg = k.sb("dg", [128, 16, 128], BF16)
        gm = k.sb("gm", [128, 256], F32)
        bs = k.sb("bs", [128, 8], F32)
        NCB = 4
        kth = [k.sb("kth%d" % i, [128, CK * 128], BF16) for i in range(NCB)]
        vh = [k.sb("vh%d" % i, [128, CK, 129], BF16) for i in range(NCB)]
        pe_ = [k.sb("pe%d" % i, [128, 4, 128], BF16) for i in range(3)]
        pT = [k.sb("pT%d" % i, [128, 4, 128], BF16) for i in range(3)]
        obr = k.sb("obr", [128, 8, 129], F32)
        ob = k.sb("ob", [128, 8, 128], BF16)
        obT = k.sb("obT", [128, 8, 128], BF16)
        rden = k.sb("rden", [128, 8], F32)
        iqv = iqT_d.t.rearrange("(c two d) t -> two d c t", two=2, d=64)
        qTv = qT_d.t.rearrange("h p t -> p h t")
        obv = obT_d.t.rearrange("(h p) t -> p h t", p=128)
        p4c = [0]

        def nps4():
            p = psum[p4c[0] % 4]
            p4c[0] += 1
            return p
        cn = {"rl": 0, "pp": 0, "cb": 0, "ss": 0}

        def geo(t):
            vt = NT + t
            nkt = vt + 1
            return vt, nkt, nkt * 128

        def tloads(t):
            i = t % 3
            ro = t * 128
            k.dma("sp", iq[i][0:64, 0, :, :], iqv[0, :, :, ro:ro + 128], r=[iqT_d], w=[iq[i]])
            k.dma("sp", iq[i][64:128, 1, :, :], iqv[1, :, :, ro:ro + 128], r=[iqT_d], w=[iq[i]])
            k.dma("sp", iwt[i][:], iw_d[ro:ro + 128, :], r=[iw_d], w=[iwt[i]])
            k.dma("sp", qTt[i][:], qTv[:, :, ro:ro + 128], r=[qT_d], w=[qTt[i]])

        def stageA(t):
            i = t % 3
            vt, nkt, NK = geo(t)
            scb = sc[t % 2]
            nkb = (NK + 511) // 512
            k.op("act", lambda e: e.activation(out=aw[:], in_=iwt[i][:], func=AF.Abs, scale=1.0 / 32.0), r=[iwt[i]], w=[aw])
            k.op("act", lambda e: e.activation(out=sg[:], in_=iwt[i][:], func=AF.Sign), r=[iwt[i]], w=[sg])
            for j in range(16):
                k.op("pool", lambda e: e.tensor_scalar(out=dg[:, j, :], in0=identb[:], scalar1=sg[:, j:j + 1], scalar2=None, op0=ALU.mult),
                     r=[identb, sg], w=[dg])
            for kb in range(nkb):
                wd = min(512, NK - kb * 512)
                c0 = kb * 512
                psS = psum[4 + (cn["ss"] % 2)]
                cn["ss"] += 1
                pend = []

                def acc(jj, r__):
                    k.op("pe", lambda e: e.matmul(psS[:, 0:wd], lhsT=dg[:, jj, :], rhs=r__[:, 0:wd], start=(jj == 0), stop=(jj == 15)),
                         r=[dg, r__], w=[psS])
                for j in range(16):
                    c = j // 2
                    po = (j % 2) * 64
                    ps = nps4()
                    k.op("pe", lambda e: e.matmul(ps[:, 0:wd], lhsT=iq[i][:, j % 2, c, :], rhs=ik2[:, c0:c0 + wd],
                                                  start=True, stop=True), r=[iq[i], ik2], w=[ps])
                    r_ = rl[cn["rl"] % 4]
                    cn["rl"] += 1
                    k.op("act", lambda e: e.activation(out=r_[:, 0:wd], in_=ps[:, 0:wd], func=AF.Relu, scale=aw[:, j:j + 1]),
                         r=[ps, aw], w=[r_])
                    pend.append((j, r_))
                    if len(pend) > 2:
                        acc(*pend.pop(0))
                while pend:
                    acc(*pend.pop(0))
                if c0 < TO:
                    k.op("act", lambda e: e.activation(out=scb[:, c0:c0 + wd], in_=psS[:, 0:wd], func=AF.Identity,
                                                       bias=cst[:, C_PREB:C_PREB + 1], scale=1.0), r=[psS, cst], w=[scb])
                else:
                    k.op("act", lambda e: e.activation(out=scb[:, c0:c0 + wd], in_=psS[:, 0:wd], func=AF.Copy), r=[psS], w=[scb])

        def stageB(t):
            vt, nkt, NK = geo(t)
            scb = sc[t % 2]
            k.op("dve", lambda e: e.memset(scb[0:64, NK - 64:NK], -1e30), w=[scb])
            NKp = NK
            if nkt % 2 == 1:
                k.op("dve", lambda e: e.memset(scb[:, NK:NK + 128], -1e30), w=[scb])
                NKp = NK + 128
            k.op("dve", lambda e: e.tensor_reduce(out=gm[:], in_=scb[:, 0:NKp].rearrange("p (a g) -> p g a", g=256), axis=AX.X, op=ALU.max),
                 r=[scb], w=[gm])
            k.op("dve", lambda e: e.tensor_reduce(out=bs[:, 0:1], in_=gm[:], axis=AX.X, op=ALU.min), r=[gm], w=[bs])
            k.op("dve", lambda e: e.tensor_reduce(out=bs[:, 1:2], in_=gm[:], axis=AX.X, op=ALU.max), r=[gm], w=[bs])
            k.op("dve", lambda e: e.tensor_tensor(out=bs[:, 2:3], in0=bs[:, 1:2], in1=bs[:, 0:1], op=ALU.subtract), r=[bs], w=[bs])
            for it in range(NIT):
                ck = 2.0 ** -(it + 1)
                k.op("dve", lambda e: e.scalar_tensor_tensor(out=bs[:, 3:4], in0=bs[:, 2:3], scalar=ck, in1=bs[:, 0:1], op0=ALU.mult, op1=ALU.add),
                     r=[bs], w=[bs])
                k.op("dve", lambda e: e.tensor_scalar(out=mk[:, 0:NK], in0=scb[:, 0:NK], scalar1=bs[:, 3:4], scalar2=0.0, op0=ALU.is_ge, op1=ALU.add,
                                                      accum_out=bs[:, 4:5]), r=[scb, bs], w=[mk, bs])
                k.op("dve", lambda e: e.tensor_scalar(out=bs[:, 5:6], in0=bs[:, 4:5], scalar1=255.5, scalar2=ck, op0=ALU.is_ge, op1=ALU.mult),
                     r=[bs], w=[bs])
                k.op("dve", lambda e: e.scalar_tensor_tensor(out=bs[:, 0:1], in0=bs[:, 5:6], scalar=bs[:, 2:3], in1=bs[:, 0:1], op0=ALU.mult, op1=ALU.add),
                     r=[bs], w=[bs])
            k.op("dve", lambda e: e.tensor_scalar(out=bs[:, 6:7], in0=bs[:, 0:1], scalar1=-1e29, scalar2=None, op0=ALU.max), r=[bs], w=[bs])
            k.op("dve", lambda e: e.tensor_scalar(out=mk[:, 0:NK], in0=scb[:, 0:NK], scalar1=bs[:, 6:7], scalar2=None, op0=ALU.is_ge),
                 r=[scb, bs], w=[mk])

        def maskT(t):
            vt, nkt, NK = geo(t)
            for g8 in range((nkt + 7) // 8):
                n = min(8, nkt - g8 * 8)
                ps = nps4()
                psb = ps.t[:].bitcast(BF16).rearrange("p (c n) -> p c n", n=128)
                for u in range(n):
                    kt = g8 * 8 + u
                    k.op("pe", lambda e: e.transpose(out=psb[:, u, :], in_=mk[:, kt * 128:(kt + 1) * 128], identity=identb[:]),
                         r=[mk, identb], w=[ps])
                k.op("act", lambda e: e.activation(out=mkT[:, g8 * 8:g8 * 8 + n, :], in_=psb[:, 0:n, :], func=AF.Copy), r=[ps], w=[mkT])

        def attn(t):
            i = t % 3
            ro = t * 128
            vt, nkt, NK = geo(t)
            nck = (nkt + CK - 1) // CK
            seq = [(h, c_) for h in range(8) for c_ in range(nck)]
            bufs = {}

            def cload(idx):
                h, c_ = seq[idx]
                bi = cn["cb"] % NCB
                cn["cb"] += 1
                k0 = c_ * CK
                n_ = min(CK, nkt - k0)
                k.dma("sp", kth[bi][:, 0:n_ * 128], KT_d[h, :, k0 * 128:(k0 + n_) * 128], r=[KT_d], w=[kth[bi]])
                k.dma("sp", vh[bi][:, 0:n_, :], Vd[h, :, k0:k0 + n_, :], r=[Vd], w=[vh[bi]])
                bufs[idx] = bi
            for idx in range(min(2, len(seq))):
                cload(idx)
            prev = None
            state = {}

            def pv(st):
                hh, kts, p_, bi, psO = st
                for u, kt in enumerate(kts):
                    k.op("pe", lambda e: e.matmul(psO[:, 0:129], lhsT=p_[:, u, :], rhs=vh[bi][:, kt % CK, :],
                                                  start=(kt == 0), stop=(kt == nkt - 1)), r=[p_, vh[bi]], w=[psO])
                if kts[-1] == nkt - 1:
                    k.op("act", lambda e: e.activation(out=obr[:, hh, :], in_=psO[:, 0:129], func=AF.Copy), r=[psO], w=[obr])
            for idx, (h, c_) in enumerate(seq):
                if idx + 2 < len(seq):
                    cload(idx + 2)
                bi = bufs[idx]
                psO = psum[6 + (h % 2)]
                k0 = c_ * CK
                n_ = min(CK, nkt - k0)
                for kg in range((n_ + 3) // 4):
                    kts = [k0 + kg * 4 + u for u in range(min(4, n_ - kg * 4))]
                    n = len(kts)
                    ps = nps4()
                    for u, kt in enumerate(kts):
                        near = kt >= vt - 1
                        kl = kt - k0
                        k.op("pe", lambda e: e.matmul(ps[:, u * 128:(u + 1) * 128], lhsT=kth[bi][:, kl * 128:(kl + 1) * 128], rhs=qTt[i][:, h, :],
                                                      start=True, stop=not near), r=[kth[bi], qTt[i]], w=[ps])
                        if near:
                            a = vt - kt
                            k.op("pe", lambda e: e.matmul(ps[:, u * 128:(u + 1) * 128], lhsT=identb[:], rhs=tbb[:, h, a, :],
                                                          start=False, stop=True), r=[identb, tbb], w=[ps])
                    e_ = pe_[cn["pp"] % 3]
                    p_ = pT[cn["pp"] % 3]
                    cn["pp"] += 1
                    k.op("act", lambda e: e.activation(out=e_[:, 0:n, :].rearrange("p u n -> p (u n)"), in_=ps[:, 0:n * 128], func=AF.Exp,
                                                       bias=cb[:, h:h + 1], scale=1.0), r=[ps, cb], w=[e_])
                    k.op("pool", lambda e: e.tensor_tensor(out=p_[:, 0:n, :], in0=e_[:, 0:n, :], in1=mkT[:, kts[0]:kts[0] + n, :], op=ALU.mult),
                         r=[e_, mkT], w=[p_])
                    cur = (h, kts, p_, bi, psO)
                    if prev is not None:
                        pv(prev)
                    prev = cur
            pv(prev)
            k.op("dve", lambda e: e.reciprocal(out=rden[:], in_=obr[:, :, 128]), r=[obr], w=[rden])
            k.op("dve", lambda e: e.tensor_tensor(out=ob[:], in0=obr[:, :, 0:128], in1=rden[:].unsqueeze(2).to_broadcast([128, 8, 128]), op=ALU.mult),
                 r=[obr, rden], w=[ob])
            ps = nps4()
            psb = ps.t[:].bitcast(BF16).rearrange("p (c n) -> p c n", n=128)
            for h in range(8):
                k.op("pe", lambda e: e.transpose(out=psb[:, h, :], in_=ob[:, h, :], identity=identb[:]), r=[ob, identb], w=[ps])
            k.op("act", lambda e: e.activation(out=obT[:], in_=psb, func=AF.Copy), r=[ps], w=[obT])
            k.dma("sp", obv[:, :, ro:ro + 128], obT[:], r=[obT], w=[obT_d])

        tloads(0)
        if NT > 1:
            tloads(1)
        stageA(0)
        stageB(0)
        if NT > 1:
            stageA(1)
        for t in range(NT):
            if t + 2 < NT:
                tloads(t + 2)
            maskT(t)
            if t + 1 < NT:
                stageB(t + 1)
            attn(t)
            if t + 2 < NT:
                stageA(t + 2)
        k.barrier()
        k.pop()
        ph.close()

    def phase4a():
        ph = ExitStack()
        k.push(ph)
        wog = k.sb("wog", [128, 8, D], BF16)
        wod = k.sb("wod", [128, 8, D], BF16)
        for c in range(8):
            k.dma("pool", wog[:, c, :], w_oa[c * 128:(c + 1) * 128, :], r=[w_oa], w=[wog])
            k.dma("pool", wod[:, c, :], w_ob[c * 128:(c + 1) * 128, :], r=[w_ob], w=[wod])
        oag = [k.sb("oag%d" % i, [128, 8, 512], BF16) for i in range(2)]
        obg = [k.sb("obg%d" % i, [128, 8, 512], BF16) for i in range(2)]
        gt = [k.sb("gt%d" % i, [128, 2, 512], BF16) for i in range(3)]
        t1 = [k.sb("t1_%d" % i, [128, 512], F32) for i in range(2)]
        t2 = [k.sb("t2_%d" % i, [128, 512], F32) for i in range(2)]
        mst = [k.sb("mst%d" % i, [128, 512], BF16) for i in range(3)]
        oav = oaT_d.t.rearrange("(h p) t -> p h t", p=128)
        obv = obT_d.t.rearrange("(h p) t -> p h t", p=128)
        gtv = gateT_d.t.rearrange("(a c p) t -> p a c t", a=2, p=128)
        n = 0
        for g4 in range(NT // 4):
            t0 = g4 * 512
            i = g4 % 2
            k.dma("sp", oag[i][:], oav[:, :, t0:t0 + 512], r=[oaT_d], w=[oag[i]])
            k.dma("sp", obg[i][:], obv[:, :, t0:t0 + 512], r=[obT_d], w=[obg[i]])
            for cbk in range(16):
                g_ = gt[n % 3]
                k.dma("sp", g_[:], gtv[:, :, cbk, t0:t0 + 512], r=[gateT_d], w=[g_])
                psA = nps()
                for kc in range(8):
                    k.op("pe", lambda e: e.matmul(psA[:], lhsT=wog[:, kc, cbk * 128:(cbk + 1) * 128], rhs=oag[i][:, kc, :],
                                                  start=(kc == 0), stop=(kc == 7)), r=[wog, oag[i]], w=[psA])
                psB = nps()
                for kc in range(8):
                    k.op("pe", lambda e: e.matmul(psB[:], lhsT=wod[:, kc, cbk * 128:(cbk + 1) * 128], rhs=obg[i][:, kc, :],
                                                  start=(kc == 0), stop=(kc == 7)), r=[wod, obg[i]], w=[psB])
                a_ = t1[n % 2]
                b_ = t2[n % 2]
                m_ = mst[n % 3]
                k.op("dve", lambda e: e.tensor_tensor(out=a_[:], in0=psA[:], in1=g_[:, 0, :], op=ALU.mult), r=[psA, g_], w=[a_])
                k.op("dve", lambda e: e.tensor_tensor(out=b_[:], in0=psB[:], in1=g_[:, 1, :], op=ALU.mult), r=[psB, g_], w=[b_])
                k.op("pool", lambda e: e.tensor_tensor(out=m_[:], in0=a_[:], in1=b_[:], op=ALU.add), r=[a_, b_], w=[m_])
                k.dma("sp", mT_d[cbk * 128:(cbk + 1) * 128, t0:t0 + 512], m_[:], r=[m_], w=[mT_d])
                n += 1
        k.barrier()
        k.pop()
        ph.close()

    def phase4b():
        ph = ExitStack()
        k.push(ph)
        wo = k.sb("wo", [128, 16, D], BF16)
        for c in range(16):
            k.dma("pool", wo[:, c, :], w_o[c * 128:(c + 1) * 128, :], r=[w_o], w=[wo])
        wr = k.sb("wr", [128, 16, 72], BF16)
        k.dma("pool", wr[:], w_r.t.rearrange("(c p) n -> p c n", p=128), r=[w_r], w=[wr])
        brb = k.sb("brb", [1, 72], BF16)
        k.dma("pool", brb[:], b_r[:], r=[b_r], w=[brb])
        onesrb = k.sb("onesrb", [1, 128], BF16)
        k.op("dve", lambda e: e.memset(onesrb[:], 1.0), w=[onesrb])
        gffn = k.sb("gffn", [128, D], F32)
        k.dma("sp", gffn[:], gffn_row.t.partition_broadcast(128), r=[gffn_row], w=[gffn])
        trif = k.sb("trif4", [128, 128], F32)
        k.dma("sp", trif[:], trid[:], w=[trif])
        stri = k.sb("stri", [128, 128], BF16)
        k.op("dve", lambda e: e.tensor_tensor(out=stri[:], in0=trif[:], in1=identf[:], op=ALU.subtract), r=[trif, identf], w=[stri])
        ones1 = k.sb("ones1", [128, 128], BF16)
        k.op("dve", lambda e: e.memset(ones1[:], 1.0), w=[ones1])
        ebase = k.sb("ebase", [128, 64], F32)
        k.dma("sp", ebase[:], ebased[:], w=[ebase])
        run = k.sb("run", [128, 64], F32)
        k.op("dve", lambda e: e.memset(run[:], 0.0), w=[run])
        mt = [k.sb("mt%d" % i, [128, 16, 128], BF16) for i in range(2)]
        xt = [k.sb("x4_%d" % i, [128, D], F32) for i in range(2)]
        junk = k.sb("junk4", [128, D], BF16)
        hf = [k.sb("hf%d" % i, [128, D], BF16) for i in range(2)]
        hfT = k.sb("hfT", [128, 16, 128], BF16)
        rs = k.sb("rs4", [128, 16], F32)
        lgt = k.sb("lgt", [128, 72], F32)
        ohg = k.sb("ohg", [128, 8], F32)
        pen = k.sb("pen", [128, 8], F32)
        ml = k.sb("ml", [128, 64], F32)
        ml2 = k.sb("ml2", [128, 64], F32)
        s1 = k.sb("s1", [128, 64], F32)
        s2 = k.sb("s2", [128, 64], F32)
        selb = k.sb("selb", [128, 64], BF16)
        pos = k.sb("pos", [128, 64], F32)
        junk64 = k.sb("junk64", [128, 64], F32)
        slf = k.sb("slf", [128, 2], F32)
        mtv = mT_d.t.rearrange("(c p) t -> p c t", p=128)

        def loads(t):
            i = t % 2
            ro = t * 128
            k.dma("sp", mt[i][:], mtv[:, :, ro:ro + 128], r=[mT_d], w=[mt[i]])
            k.dma("sp", xt[i][:], x_own[ro:ro + 128, :], r=[x_own], w=[xt[i]])
        loads(0)
        for t in range(NT):
            i = t % 2
            ro = t * 128
            if t + 1 < NT:
                loads(t + 1)
            for nb in range(4):
                ps = nps()
                for c in range(16):
                    k.op("pe", lambda e: e.matmul(ps[:], lhsT=mt[i][:, c, :], rhs=wo[:, c, nb * 512:(nb + 1) * 512],
                                                  start=(c == 0), stop=(c == 15)), r=[mt[i], wo], w=[ps])
                k.op("dve", lambda e: e.tensor_tensor(out=xt[i][:, nb * 512:(nb + 1) * 512], in0=ps[:], in1=xt[i][:, nb * 512:(nb + 1) * 512],
                                                      op=ALU.add), r=[ps, xt[i]], w=[xt[i]])
            k.dma("sp", x1_d[ro:ro + 128, :], xt[i][:], r=[xt[i]], w=[x1_d])
            k.op("act", lambda e: e.activation(out=junk[:], in_=xt[i][:], func=AF.Square, accum_out=rs[:, 0:1]), r=[xt[i]], w=[junk, rs])
            k.op("act", lambda e: e.activation(out=rs[:, 1:2], in_=rs[:, 0:1], func=AF.Sqrt, bias=epsc[:, 0:1], scale=1.0 / D), r=[rs, epsc], w=[rs])
            k.op("dve", lambda e: e.reciprocal(out=rs[:, 2:3], in_=rs[:, 1:2]), r=[rs], w=[rs])
            k.op("dve", lambda e: e.scalar_tensor_tensor(out=hf[i][:], in0=xt[i][:], scalar=rs[:, 2:3], in1=gffn[:], op0=ALU.mult, op1=ALU.mult),
                 r=[xt[i], rs, gffn], w=[hf[i]])
            for hh in range(2):
                ps = nps()
                psb = ps.t[:].bitcast(BF16).rearrange("p (c n) -> p c n", n=128)
                for c in range(8):
                    cc = hh * 8 + c
                    k.op("pe", lambda e: e.transpose(out=psb[:, c, :], in_=hf[i][:, cc * 128:(cc + 1) * 128], identity=identb[:]),
                         r=[hf[i], identb], w=[ps])
                if hh == 0:
                    k.op("act", lambda e: e.activation(out=hfT[:, 0:8, :], in_=psb, func=AF.Copy), r=[ps], w=[hfT])
                else:
                    k.op("dve", lambda e: e.tensor_copy(out=hfT[:, 8:16, :], in_=psb), r=[ps], w=[hfT])
            ps = nps()
            for c in range(16):
                k.op("pe", lambda e: e.matmul(ps[:, 0:72], lhsT=hfT[:, c, :], rhs=wr[:, c, :], start=(c == 0), stop=False), r=[hfT, wr], w=[ps])
            k.op("pe", lambda e: e.matmul(ps[:, 0:72], lhsT=onesrb[:], rhs=brb[:], start=False, stop=True), r=[onesrb, brb], w=[ps])
            k.op("dve", lambda e: e.tensor_copy(out=lgt[:], in_=ps[:, 0:72]), r=[ps], w=[lgt])
            k.op("dve", lambda e: e.tensor_reduce(out=rs[:, 3:4], in_=lgt[:, 0:8], axis=AX.X, op=ALU.max), r=[lgt], w=[rs])
            k.op("dve", lambda e: e.tensor_scalar(out=ohg[:], in0=lgt[:, 0:8], scalar1=rs[:, 3:4], scalar2=None, op0=ALU.is_ge), r=[lgt, rs], w=[ohg])
            k.op("dve", lambda e: e.tensor_scalar(out=rs[:, 4:5], in0=rs[:, 3:4], scalar1=-1.0, scalar2=None, op0=ALU.mult), r=[rs], w=[rs])
            k.op("act", lambda e: e.activation(out=pen[:], in_=lgt[:, 0:8], func=AF.Exp, bias=rs[:, 4:5], scale=1.0, accum_out=rs[:, 5:6]),
                 r=[lgt, rs], w=[pen, rs])
            k.op("dve", lambda e: e.reciprocal(out=rs[:, 6:7], in_=rs[:, 5:6]), r=[rs], w=[rs])
            k.op("dve", lambda e: e.tensor_scalar(out=pen[:], in0=ohg[:], scalar1=1e9, scalar2=-1e9, op0=ALU.mult, op1=ALU.add), r=[ohg], w=[pen])
            k.op("dve", lambda e: e.tensor_tensor(out=ml[:].rearrange("p (g e) -> p g e", e=8), in0=lgt[:, 8:72].rearrange("p (g e) -> p g e", e=8),
                                                  in1=pen[:].unsqueeze(2).to_broadcast([128, 8, 8]), op=ALU.add), r=[lgt, pen], w=[ml])
            k.op("dve", lambda e: e.tensor_reduce(out=rs[:, 7:8], in_=ml[:], axis=AX.X, op=ALU.max), r=[ml], w=[rs])
            k.op("dve", lambda e: e.tensor_scalar(out=s1[:], in0=ml[:], scalar1=rs[:, 7:8], scalar2=None, op0=ALU.is_ge), r=[ml, rs], w=[s1])
            k.op("dve", lambda e: e.scalar_tensor_tensor(out=ml2[:], in0=s1[:], scalar=-1e9, in1=ml[:], op0=ALU.mult, op1=ALU.add), r=[s1, ml], w=[ml2])
            k.op("dve", lambda e: e.tensor_reduce(out=rs[:, 8:9], in_=ml2[:], axis=AX.X, op=ALU.max), r=[ml2], w=[rs])
            k.op("dve", lambda e: e.tensor_scalar(out=s2[:], in0=ml2[:], scalar1=rs[:, 8:9], scalar2=None, op0=ALU.is_ge), r=[ml2, rs], w=[s2])
            k.op("dve", lambda e: e.tensor_tensor(out=rs[:, 9:10], in0=rs[:, 8:9], in1=rs[:, 7:8], op=ALU.subtract), r=[rs], w=[rs])
            k.op("act", lambda e: e.activation(out=rs[:, 10:11], in_=rs[:, 9:10], func=AF.Exp), r=[rs], w=[rs])
            k.op("dve", lambda e: e.tensor_scalar(out=rs[:, 11:12], in0=rs[:, 10:11], scalar1=1.0, scalar2=None, op0=ALU.add), r=[rs], w=[rs])
            k.op("dve", lambda e: e.reciprocal(out=rs[:, 12:13], in_=rs[:, 11:12]), r=[rs], w=[rs])
            k.op("dve", lambda e: e.tensor_tensor(out=wts[:, t, 0:1], in0=rs[:, 6:7], in1=rs[:, 12:13], op=ALU.mult), r=[rs], w=[wts])
            k.op("dve", lambda e: e.tensor_tensor(out=wts[:, t, 1:2], in0=wts[:, t, 0:1], in1=rs[:, 10:11], op=ALU.mult), r=[rs, wts], w=[wts])
            k.op("dve", lambda e: e.tensor_tensor(out=selb[:], in0=s1[:], in1=s2[:], op=ALU.add), r=[s1, s2], w=[selb])
            psP = nps()
            k.op("pe", lambda e: e.matmul(psP[:, 0:64], lhsT=stri[:], rhs=selb[:], start=True, stop=True), r=[stri, selb], w=[psP])
            psC = nps()
            k.op("pe", lambda e: e.matmul(psC[:, 0:64], lhsT=ones1[:], rhs=selb[:], start=True, stop=True), r=[ones1, selb], w=[psC])
            k.op("dve", lambda e: e.tensor_tensor(out=pos[:], in0=psP[:, 0:64], in1=run[:], op=ALU.add), r=[psP, run], w=[pos])
            k.op("dve", lambda e: e.tensor_tensor(out=run[:], in0=psC[:, 0:64], in1=run[:], op=ALU.add), r=[psC, run], w=[run])
            k.op("dve", lambda e: e.scalar_tensor_tensor(out=pos[:], in0=pos[:], scalar=float(CAP - 1), in1=ebase[:], op0=ALU.min, op1=ALU.add),
                 r=[pos, ebase], w=[pos])
            for (sx, col) in ((s1, 0), (s2, 1)):
                k.op("dve", lambda e: e.tensor_tensor(out=junk64[:], in0=sx[:], in1=pos[:], op=ALU.mult), r=[sx, pos], w=[junk64])
                k.op("dve", lambda e: e.tensor_reduce(out=slf[:, col:col + 1], in_=junk64[:], axis=AX.X, op=ALU.add), r=[junk64], w=[slf])
            k.op("dve", lambda e: e.tensor_copy(out=slots_i[:, t, :], in_=slf[:]), r=[slf], w=[slots_i])
            for j in range(2):
                k.dma("pool", Xd[:, :], hf[i][:], r=[hf[i], slots_i], w=[Xd],
                      indirect=dict(out_offset=bass.IndirectOffsetOnAxis(ap=slots_i[:, t, j:j + 1], axis=0), in_offset=None))
        if cfg.debug:
            k.dma("sp", slots_o[:], slots_i[:], r=[slots_i], w=[slots_o])
            k.dma("sp", wts_o[:], wts[:], r=[wts], w=[wts_o])
        k.barrier()
        k.pop()
        ph.close()

    def phase5():
        ph = ExitStack()
        k.push(ph)
        NE = cfg.nexp
        NRB = CAP // 128
        wg = [k.sb("wg%d" % i, [128, 16, 512], BF16) for i in range(2)]
        wu = [k.sb("wu%d" % i, [128, 16, 512], BF16) for i in range(2)]
        wd = [k.sb("wd%d" % i, [128, 4, D], BF16) for i in range(2)]
        xr = [k.sb("xr%d" % i, [128, NRB, D], BF16) for i in range(2)]
        XT = k.sb("XT", [128, 16, CAP], BF16)
        sgt = [k.sb("sgt%d" % i, [128, CAP], F32) for i in range(2)]
        aT = k.sb("aT", [128, 4, CAP], BF16)
        yst = [k.sb("yst%d" % i, [128, D], F32) for i in range(2)]

        def loads(e):
            i = e % 2
            k.dma("pool", wg[i][:], w_eg.t[e].rearrange("(c p) f -> p c f", p=128), r=[w_eg], w=[wg[i]])
            k.dma("pool", wu[i][:], w_eu.t[e].rearrange("(c p) f -> p c f", p=128), r=[w_eu], w=[wu[i]])
            k.dma("pool", wd[i][:], w_ed.t[e].rearrange("(c p) n -> p c n", p=128), r=[w_ed], w=[wd[i]])
            k.dma("sp", xr[i][:], Xd.t[e * CAP:(e + 1) * CAP, :].rearrange("(b p) d -> p b d", p=128), r=[Xd], w=[xr[i]])
        loads(0)
        ny = 0
        for e_ in range(NE):
            i = e_ % 2
            if e_ + 1 < NE:
                loads(e_ + 1)
            n = 0
            for rb in range(NRB):
                for hh in range(2):
                    ps = nps()
                    psb = ps.t[:].bitcast(BF16).rearrange("p (c n) -> p c n", n=128)
                    for c in range(8):
                        cc = hh * 8 + c
                        k.op("pe", lambda e: e.transpose(out=psb[:, c, :], in_=xr[i][:, rb, cc * 128:(cc + 1) * 128], identity=identb[:]),
                             r=[xr[i], identb], w=[ps])
                    if n % 2 == 0:
                        k.op("act", lambda e: e.activation(out=XT[:, hh * 8:hh * 8 + 8, rb * 128:(rb + 1) * 128], in_=psb, func=AF.Copy), r=[ps], w=[XT])
                    else:
                        k.op("dve", lambda e: e.tensor_copy(out=XT[:, hh * 8:hh * 8 + 8, rb * 128:(rb + 1) * 128], in_=psb), r=[ps], w=[XT])
                    n += 1
            for fb in range(4):
                psg = nps()
                for c in range(16):
                    k.op("pe", lambda e: e.matmul(psg[:, 0:CAP], lhsT=wg[i][:, c, fb * 128:(fb + 1) * 128], rhs=XT[:, c, :],
                                                  start=(c == 0), stop=(c == 15)), r=[wg[i], XT], w=[psg])
                psu = nps()
                for c in range(16):
                    k.op("pe", lambda e: e.matmul(psu[:, 0:CAP], lhsT=wu[i][:, c, fb * 128:(fb + 1) * 128], rhs=XT[:, c, :],
                                                  start=(c == 0), stop=(c == 15)), r=[wu[i], XT], w=[psu])
                s_ = sgt[fb % 2]
                k.op("act", lambda e: e.activation(out=s_[:], in_=psg[:, 0:CAP], func=AF.Silu), r=[psg], w=[s_])
                k.op("dve", lambda e: e.tensor_tensor(out=aT[:, fb, :], in0=psu[:, 0:CAP], in1=s_[:], op=ALU.mult), r=[psu, s_], w=[aT])
            for rb in range(NRB):
                y_ = yst[ny % 2]
                ny += 1
                for nb in range(4):
                    ps = nps()
                    for fc in range(4):
                        k.op("pe", lambda e: e.matmul(ps[:], lhsT=aT[:, fc, rb * 128:(rb + 1) * 128], rhs=wd[i][:, fc, nb * 512:(nb + 1) * 512],
                                                      start=(fc == 0), stop=(fc == 3)), r=[aT, wd[i]], w=[ps])
                    if nb % 2 == 0:
                        k.op("act", lambda e: e.activation(out=y_[:, nb * 512:(nb + 1) * 512], in_=ps[:], func=AF.Copy), r=[ps], w=[y_])
                    else:
                        k.op("dve", lambda e: e.tensor_copy(out=y_[:, nb * 512:(nb + 1) * 512], in_=ps[:]), r=[ps], w=[y_])
                r0 = e_ * CAP + rb * 128
                k.dma("sp", Yd[r0:r0 + 128, :], y_[:], r=[y_], w=[Yd])
            if e_ % 16 == 15:
                k.barrier()
        k.barrier()
        k.pop()
        ph.close()

    def phase6():
        ph = ExitStack()
        k.push(ph)
        wpg = k.sb("wpg", [128, 16, D], BF16)
        for c in range(16):
            k.dma("pool", wpg[:, c, :], w_pg[c * 128:(c + 1) * 128, :], r=[w_pg], w=[wpg])
        wpp = k.sb("wpp", [128, 2, D], BF16)
        k.dma("pool", wpp[:], w_pp.t.rearrange("(c p) n -> p c n", p=128), r=[w_pp], w=[wpp])
        bpg = k.sb("bpg", [1, D], BF16)
        k.dma("pool", bpg[:], b_pg[:], r=[b_pg], w=[bpg])
        onesrb = k.sb("onesrb6", [1, 128], BF16)
        k.op("dve", lambda e: e.memset(onesrb[:], 1.0), w=[onesrb])
        gple = k.sb("gple", [128, D], F32)
        k.dma("sp", gple[:], gple_row.t.partition_broadcast(128), r=[gple_row], w=[gple])
        x1t = [k.sb("x1t%d" % i, [128, D], F32) for i in range(2)]
        y1t = [k.sb("y1t%d" % i, [128, D], F32) for i in range(2)]
        y2t = [k.sb("y2t%d" % i, [128, D], F32) for i in range(2)]
        pt = [k.sb("pt%d" % i, [128, 256], F32) for i in range(2)]
        ptb = k.sb("ptb", [128, 256], BF16)
        pTt = k.sb("pTt", [128, 2, 128], BF16)
        junk = k.sb("junk6", [128, D], BF16)
        hp = k.sb("hp", [128, D], BF16)
        hpT = k.sb("hpT", [128, 16, 128], BF16)
        rs = k.sb("rs6", [128, 4], F32)
        sgm = [k.sb("sgm%d" % i, [128, 512], F32) for i in range(2)]
        ot = [k.sb("ot%d" % i, [128, D], F32) for i in range(2)]

        def loads(t):
            i = t % 2
            ro = t * 128
            k.dma("sp", x1t[i][:], x1_d[ro:ro + 128, :], r=[x1_d], w=[x1t[i]])
            k.dma("sp", pt[i][:], p_own[ro:ro + 128, :], r=[p_own], w=[pt[i]])
            k.dma("pool", y1t[i][:], Yd[:, :], r=[Yd, slots_i], w=[y1t[i]],
                  indirect=dict(out_offset=None, in_offset=bass.IndirectOffsetOnAxis(ap=slots_i[:, t, 0:1], axis=0)))
            k.dma("pool", y2t[i][:], Yd[:, :], r=[Yd, slots_i], w=[y2t[i]],
                  indirect=dict(out_offset=None, in_offset=bass.IndirectOffsetOnAxis(ap=slots_i[:, t, 1:2], axis=0)))
        loads(0)
        for t in range(NT):
            i = t % 2
            ro = t * 128
            if t + 1 < NT:
                loads(t + 1)
            x2 = x1t[i]
            k.op("dve", lambda e: e.scalar_tensor_tensor(out=x2[:], in0=y1t[i][:], scalar=wts[:, t, 0:1], in1=x2[:], op0=ALU.mult, op1=ALU.add),
                 r=[y1t[i], wts, x2], w=[x2])
            k.op("dve", lambda e: e.scalar_tensor_tensor(out=x2[:], in0=y2t[i][:], scalar=wts[:, t, 1:2], in1=x2[:], op0=ALU.mult, op1=ALU.add),
                 r=[y2t[i], wts, x2], w=[x2])
            k.op("act", lambda e: e.activation(out=junk[:], in_=x2[:], func=AF.Square, accum_out=rs[:, 0:1]), r=[x2], w=[junk, rs])
            k.op("act", lambda e: e.activation(out=rs[:, 1:2], in_=rs[:, 0:1], func=AF.Sqrt, bias=epsc[:, 0:1], scale=1.0 / D), r=[rs, epsc], w=[rs])
            k.op("dve", lambda e: e.reciprocal(out=rs[:, 2:3], in_=rs[:, 1:2]), r=[rs], w=[rs])
            k.op("dve", lambda e: e.scalar_tensor_tensor(out=hp[:], in0=x2[:], scalar=rs[:, 2:3], in1=gple[:], op0=ALU.mult, op1=ALU.mult),
                 r=[x2, rs, gple], w=[hp])
            for hh in range(2):
                ps = nps()
                psb = ps.t[:].bitcast(BF16).rearrange("p (c n) -> p c n", n=128)
                for c in range(8):
                    cc = hh * 8 + c
                    k.op("pe", lambda e: e.transpose(out=psb[:, c, :], in_=hp[:, cc * 128:(cc + 1) * 128], identity=identb[:]),
                         r=[hp, identb], w=[ps])
                if hh == 0:
                    k.op("act", lambda e: e.activation(out=hpT[:, 0:8, :], in_=psb, func=AF.Copy), r=[ps], w=[hpT])
                else:
                    k.op("dve", lambda e: e.tensor_copy(out=hpT[:, 8:16, :], in_=psb), r=[ps], w=[hpT])
            k.op("dve", lambda e: e.tensor_copy(out=ptb[:], in_=pt[i][:]), r=[pt[i]], w=[ptb])
            ps = nps()
            psb = ps.t[:].bitcast(BF16).rearrange("p (c n) -> p c n", n=128)
            for c in range(2):
                k.op("pe", lambda e: e.transpose(out=psb[:, c, :], in_=ptb[:, c * 128:(c + 1) * 128], identity=identb[:]), r=[ptb, identb], w=[ps])
            k.op("dve", lambda e: e.tensor_copy(out=pTt[:], in_=psb[:, 0:2, :]), r=[ps], w=[pTt])
            o_ = ot[i]
            for nb in range(4):
                psg = nps()
                for c in range(16):
                    k.op("pe", lambda e: e.matmul(psg[:], lhsT=hpT[:, c, :], rhs=wpg[:, c, nb * 512:(nb + 1) * 512], start=(c == 0), stop=False),
                         r=[hpT, wpg], w=[psg])
                k.op("pe", lambda e: e.matmul(psg[:], lhsT=onesrb[:], rhs=bpg[:, nb * 512:(nb + 1) * 512], start=False, stop=True),
                     r=[onesrb, bpg], w=[psg])
                psp = nps()
                for c in range(2):
                    k.op("pe", lambda e: e.matmul(psp[:], lhsT=pTt[:, c, :], rhs=wpp[:, c, nb * 512:(nb + 1) * 512], start=(c == 0), stop=(c == 1)),
                         r=[pTt, wpp], w=[psp])
                s_ = sgm[nb % 2]
                k.op("act", lambda e: e.activation(out=s_[:], in_=psg[:], func=AF.Sigmoid), r=[psg], w=[s_])
                k.op("dve", lambda e: e.tensor_tensor(out=s_[:], in0=psp[:], in1=s_[:], op=ALU.mult), r=[psp, s_], w=[s_])
                k.op("pool", lambda e: e.tensor_tensor(out=o_[:, nb * 512:(nb + 1) * 512], in0=s_[:], in1=x2[:, nb * 512:(nb + 1) * 512], op=ALU.add),
                     r=[s_, x2], w=[o_])
            k.dma("sp", out_d[ro:ro + 128, :], o_[:], r=[o_], w=[out_d])
        k.barrier()
        k.pop()
        ph.close()

    phase1()
    if cfg.phases >= 2:
        phase2()
    if cfg.phases >= 3:
        phase3()
    if cfg.phases >= 4:
        phase4a()
        phase4b()
    if cfg.phases >= 5:
        phase5()
        phase6()

    k.barrier()
    return nc, es, k


def _pack_cst(inp, s):
    c = np.zeros((128, C_NCOL), np.float32)
    c[:, C_GMIX:C_GMIX + 16] = inp["norm_mix_g"][0].reshape(16, 128).T
    c[:, C_GFFN:C_GFFN + 16] = inp["norm_ffn_g"][0].reshape(16, 128).T
    c[:, C_GPLE:C_GPLE + 16] = inp["norm_ple_g"][0].reshape(16, 128).T
    c[:, C_BBG:C_BBG + 32] = inp["b_branch_gate"][0].reshape(32, 128).T
    c[:, C_GGLA] = inp["gla_norm_g"][0]
    c[:, C_GQ] = inp["q_norm_g"][0]
    c[:, C_GK] = inp["k_norm_g"][0]
    c[:, C_PREV] = 1.0 if s == 1 else 0.0
    c[:, C_PREB] = 0.0 if s == 1 else -1e30
    return c


def _t5_bucket(rel):
    half, exact = 16, 8
    sign = np.where(rel > 0, half, 0)
    n = np.abs(rel)
    nf = np.maximum(n, 1).astype(np.float32)
    large = exact + (np.log(nf / exact) / np.log(128 / exact) * (half - exact)).astype(np.int32)
    large = np.minimum(large, half - 1)
    return sign + np.where(n < exact, n, large)


def _bias_tables(rel_bias):
    s_ = np.arange(128)[:, None]
    q_ = np.arange(128)[None, :]
    tb = np.zeros((128, 8, 2, 128), np.float32)
    for a in range(2):
        bk = _t5_bucket((s_ - q_ - 128 * a).astype(np.int32))
        tb[:, :, a, :] = np.transpose(rel_bias[bk], (0, 2, 1))
    cb = np.ascontiguousarray(np.broadcast_to(rel_bias[15][None, :], (128, 8))).astype(np.float32)
    return tb, cb


def make_in_maps(inp, cfg):
    NT = cfg.NT
    TO = NT * 128
    x = inp["x"]
    p = inp["p"][0]
    ident = np.eye(128, dtype=np.float32)
    tb, cb = _bias_tables(inp["rel_bias"])
    w_r = np.ascontiguousarray(np.concatenate([inp["w_group_router"][0], inp["w_expert_router"][0]], axis=1))
    b_r = np.concatenate([inp["b_group_router"][0], inp["b_expert_router"][0]]).reshape(1, 72).astype(np.float32)
    ebase = np.ascontiguousarray(np.broadcast_to((np.arange(64, dtype=np.float32) * cfg.cap)[None, :], (128, 64)))
    maps = []
    for c in range(8):
        b, s = c // 2, c % 2
        m = {
            "x_pre": np.ascontiguousarray(x[b, 0:TO]),
            "x_own": np.ascontiguousarray(x[b, s * TO:(s + 1) * TO]),
            "p_own": np.ascontiguousarray(p[b, s * TO:(s + 1) * TO]),
            "cst": _pack_cst(inp, s),
            "ident": ident,
            "w_in": inp["w_in"][0],
            "w_bg": inp["w_branch_gate"][0],
            "w_alpha": inp["gla_w_alpha"][0],
            "b_alpha": inp["gla_b_alpha"][0].reshape(1, 512),
            "tri": np.triu(np.ones((128, 128), np.float32)),
            "tb": tb,
            "cb": cb,
            "w_oa": inp["w_out_gla"][0],
            "w_ob": inp["w_out_dsa"][0],
            "w_o": inp["w_out"][0],
            "w_r": w_r,
            "b_r": b_r,
            "gffn_row": inp["norm_ffn_g"][0].reshape(1, D),
            "ebase": ebase,
            "w_eg": inp["w_exp_gate"][0] if cfg.phases >= 5 else None,
            "w_eu": inp["w_exp_up"][0] if cfg.phases >= 5 else None,
            "w_ed": inp["w_exp_down"][0] if cfg.phases >= 5 else None,
            "w_pg": inp["w_ple_gate"][0],
            "w_pp": inp["w_ple_proj"][0],
            "b_pg": inp["b_ple_gate"][0].reshape(1, D),
            "gple_row": inp["norm_ple_g"][0].reshape(1, D),
        }
        maps.append({kk: vv for kk, vv in m.items() if vv is not None})
    return maps


def run(inp, cfg, trace=False):
    nc, es, k = build(cfg)
    maps = make_in_maps(inp, cfg)
    res = run_bass_kernel_spmd(nc, maps, core_ids=list(range(8)), trace=trace)
    es.close()
    return res


def kernel(**inputs):
    cfg = CFG(NT=32, G=8)
    inp = {k_: np.asarray(v) for k_, v in inputs.items()}
    res = run(inp, cfg)
    B, S = inp["x"].shape[:2]
    out = np.zeros((B, S, D), np.float32)
    TO = cfg.NT * 128
    for c in range(8):
        b, s = c // 2, c % 2
        out[b, s * TO:(s + 1) * TO] = res.results[c]["out"]
    return out
```

```python
import numpy as np
from contextlib import ExitStack
import concourse.bass as bass
import concourse.mybir as mybir
from concourse.bass_utils import run_bass_kernel_spmd

F32 = mybir.dt.float32
BF16 = mybir.dt.bfloat16
I32 = mybir.dt.int32
U32 = mybir.dt.uint32
AF = mybir.ActivationFunctionType
ALU = mybir.AluOpType
AX = mybir.AxisListType

D = 2048
EPS = 1e-6


class Buf:
    def __init__(self, name):
        self.name = name
        self.w = {}
        self.r = {}
        self.sem = None


class T:
    def __init__(self, t, b):
        self.t = t
        self.b = b

    def __getitem__(self, key):
        return self.t[key]


class KB:
    def __init__(self, nc, es):
        self.nc = nc
        self.es = es
        self.eng = {"pe": nc.tensor, "act": nc.scalar, "dve": nc.vector, "pool": nc.gpsimd, "sp": nc.sync}
        self.esem = {}
        for k in self.eng:
            self.esem[k] = es.enter_context(nc.semaphore("sem_" + k))
        self.ecnt = {k: 0 for k in self.eng}
        self.gen = {k: 0 for k in self.eng}
        self.rebase_at = 20000
        self.ptiles = [[]]
        self.waited = {}
        self.dsems = []
        self.free_dsems = []
        self.stack = [es]
        self.nops = 0
        self.nwaits = 0

    def push(self, es):
        self.stack.append(es)
        self.ptiles.append([])

    def pop(self):
        self.stack.pop()
        tl = self.ptiles.pop()
        for t in tl:
            if t.b.sem is not None:
                if t.b.sem[1] < 20000:
                    self.free_dsems.append(t.b.sem)
                t.b.sem = None

    def sb(self, name, shape, dtype):
        t = self.stack[-1].enter_context(self.nc.sbuf_tensor("sb_" + name, list(shape), dtype))
        tt = T(t, Buf(name))
        self.ptiles[-1].append(tt)
        return tt

    def dram(self, name, shape, dtype, kind="Internal"):
        t = self.nc.dram_tensor(name, list(shape), dtype, kind=kind).ap()
        return T(t, Buf(name))

    def _dsem(self, b):
        if b.sem is None:
            if self.free_dsems:
                b.sem = self.free_dsems.pop()
            else:
                s = self.es.enter_context(self.nc.semaphore("dsem%d" % len(self.dsems)))
                b.sem = [s, 0]
                self.dsems.append(b.sem)
        return b.sem

    def release(self, tiles):
        for t in tiles:
            if t.b.sem is not None:
                self.free_dsems.append(t.b.sem)
                t.b.sem = None

    def _wait(self, e, deps):
        for key, (sem, val) in deps.items():
            if e == "pe" and isinstance(key, tuple) and key[0] == "pe":
                continue
            wk = (e, key)
            if self.waited.get(wk, 0) >= val:
                continue
            self.waited[wk] = val
            self.eng[e].wait_ge(sem, val)
            self.nwaits += 1

    @staticmethod
    def _merge(d, key, sem, val):
        if key not in d or d[key][1] < val:
            d[key] = (sem, val)

    def _deps(self, r, w):
        deps = {}
        for t in r:
            for key, (sem, val) in t.b.w.items():
                self._merge(deps, key, sem, val)
        for t in w:
            for key, (sem, val) in t.b.w.items():
                self._merge(deps, key, sem, val)
            for key, (sem, val) in t.b.r.items():
                self._merge(deps, key, sem, val)
        return deps

    def op(self, e, fn, r=(), w=()):
        self._wait(e, self._deps(r, w))
        if self.ecnt[e] >= self.rebase_at:
            self.gen[e] += 1
            self.esem[e] = self.es.enter_context(self.nc.semaphore("sem_%s_g%d" % (e, self.gen[e])))
            self.ecnt[e] = 0
        inst = fn(self.eng[e])
        self.ecnt[e] += 1
        inst.then_inc(self.esem[e], 1)
        self.nops += 1
        key = (e, self.gen[e])
        for t in r:
            self._merge(t.b.r, key, self.esem[e], self.ecnt[e])
        for t in w:
            self._merge(t.b.w, key, self.esem[e], self.ecnt[e])
        return inst

    def dma(self, q, out, in_, r=(), w=(), indirect=None, **kw):
        self._wait(q, self._deps(r, w))
        ds = self._dsem(w[0].b)
        if indirect is not None:
            inst = self.eng[q].indirect_dma_start(out=out, in_=in_, **indirect)
        else:
            inst = self.eng[q].dma_start(out=out, in_=in_, **kw)
        ds[1] += 16
        inst.then_inc(ds[0], 16)
        self.nops += 1
        key = id(ds)
        for t in r:
            self._merge(t.b.r, key, ds[0], ds[1])
        for t in w:
            self._merge(t.b.w, key, ds[0], ds[1])
        return inst

    def barrier(self, engines=("pe", "act", "dve", "pool", "sp")):
        deps = {}
        for k in self.eng:
            if self.ecnt[k] > 0:
                deps[(k, self.gen[k])] = (self.esem[k], self.ecnt[k])
        for ds in self.dsems:
            if ds[1] > 0:
                deps[id(ds)] = (ds[0], ds[1])
        for e in engines:
            d2 = {kk: v for kk, v in deps.items() if not (isinstance(kk, tuple) and kk[0] == e)}
            self._wait(e, d2)
        for k in self.eng:
            if self.ecnt[k] > self.rebase_at:
                self.gen[k] += 1
                self.esem[k] = self.es.enter_context(self.nc.semaphore("sem_%s_g%d" % (k, self.gen[k])))
                self.ecnt[k] = 0


class CFG:
    def __init__(self, NT=32, G=8, debug=False, phases=99):
        self.NT = NT
        self.G = G
        self.debug = debug
        self.phases = phases
        self.nit = 22
        self.cap = 256
        self.nexp = 64
        self.rebase_at = 20000


O_GQ, O_GK, O_GV, O_GR, O_GLR, O_DQ, O_DK, O_DV, O_IQ, O_IK, O_IW = (
    0, 512, 1024, 2048, 3072, 3088, 4112, 5136, 6160, 7184, 7248)
IN_COLS = 7264

C_GMIX, C_GFFN, C_GPLE, C_BBG, C_GGLA, C_GQ, C_GK, C_PREV, C_PREB, C_NCOL = 0, 16, 32, 48, 80, 81, 82, 83, 84, 96


def build(cfg):
    NT, G = cfg.NT, cfg.G
    TO = NT * 128
    TV = 2 * TO
    GT = G * 128
    NG = NT // G
    NH = GT // 512
    nc = bass.Bass("TRN2", target_bir_lowering=False)
    es = ExitStack()
    k = KB(nc, es)
    k.rebase_at = cfg.rebase_at
    okind = "ExternalOutput" if cfg.debug else "Internal"

    def din(name, shape, dt=F32):
        return k.dram(name, shape, dt, kind="ExternalInput")

    x_pre = din("x_pre", [TO, D])
    x_own = din("x_own", [TO, D])
    p_own = din("p_own", [TO, 256])
    cstd = din("cst", [128, C_NCOL])
    identd = din("ident", [128, 128])
    w_in = din("w_in", [D, IN_COLS])
    w_bg = din("w_bg", [D, 2 * D])
    w_alpha = din("w_alpha", [16, 512])
    b_alpha = din("b_alpha", [1, 512])
    trid = din("tri", [128, 128])
    out_d = k.dram("out", [TO, D], F32, kind="ExternalOutput")
    oaT_d = k.dram("oaT_d", [1024, TO], BF16, okind)
    tbd = din("tb", [128, 8, 2, 128])
    cbd = din("cb", [128, 8])
    obT_d = k.dram("obT_d", [1024, TO], BF16, okind)
    w_oa = din("w_oa", [1024, D])
    w_ob = din("w_ob", [1024, D])
    w_o = din("w_o", [D, D])
    w_r = din("w_r", [D, 72])
    b_r = din("b_r", [1, 72])
    gffn_row = din("gffn_row", [1, D])
    ebased = din("ebase", [128, 64])
    CAP = cfg.cap
    mT_d = k.dram("mT_d", [D, TO], BF16, okind)
    x1_d = k.dram("x1_d", [TO, D], F32, okind)
    Xd = k.dram("Xd", [64 * CAP, D], BF16, "Internal")
    slots_o = k.dram("slots_o", [128, NT, 2], I32, okind)
    wts_o = k.dram("wts_o", [128, NT, 2], F32, okind)
    if cfg.phases >= 5:
        w_eg = din("w_eg", [64, D, 512])
        w_eu = din("w_eu", [64, D, 512])
        w_ed = din("w_ed", [64, 512, D])
    w_pg = din("w_pg", [D, D])
    w_pp = din("w_pp", [256, D])
    b_pg = din("b_pg", [1, D])
    gple_row = din("gple_row", [1, D])
    Yd = k.dram("Yd", [64 * CAP, D], F32, "Internal")
    slots_i = k.sb("slots_i", [128, NT, 2], I32)
    wts = k.sb("wts", [128, NT, 2], F32)

    grT_d = k.dram("grT_d", [1024, TO], BF16, okind)
    qT_d = k.dram("qT_d", [8, 128, TO], BF16, okind)
    KT_d = k.dram("KT_d", [8, 128, TV], BF16, okind)
    iqT_d = k.dram("iqT_d", [1024, TO], BF16, okind)
    gateT_d = k.dram("gateT_d", [4096, TO], BF16, okind)
    ikT_d = k.dram("ikT_d", [64, TV], BF16, okind)
    glrT_d = k.dram("glrT_d", [16, TV], F32, okind)
    q_d = k.dram("q_d", [TO, 512], BF16, okind)
    k_d = k.dram("k_d", [TV, 512], BF16, okind)
    v_d = k.dram("v_d", [TV, 1024], BF16, okind)
    iw_d = k.dram("iw_d", [TO, 16], F32, okind)
    Vd = k.dram("Vd", [8, 128, 2 * NT, 129], BF16, okind)

    cst = k.sb("cst", [128, C_NCOL], F32)
    identf = k.sb("identf", [128, 128], F32)
    identb = k.sb("identb", [128, 128], BF16)
    onesb = k.sb("onesb", [128, 128], BF16)
    epsc = k.sb("epsc", [128, 1], F32)
    k.dma("sp", cst[:], cstd[:], w=[cst])
    k.dma("sp", identf[:], identd[:], w=[identf])
    k.op("dve", lambda e: e.tensor_copy(out=identb[:], in_=identf[:]), r=[identf], w=[identb])
    k.op("dve", lambda e: e.memset(onesb[:], 1.0 / 128.0), w=[onesb])
    k.op("dve", lambda e: e.memset(epsc[:], EPS), w=[epsc])

    psum = []
    for i in range(8):
        t = es.enter_context(nc.psum_tensor("ps%d" % i, [128, 512], F32))
        psum.append(T(t, Buf("ps%d" % i)))
    psi = [0]

    def nps():
        p = psum[psi[0] % 8]
        psi[0] += 1
        return p

    def phase1():
        ph = ExitStack()
        k.push(ph)
        hT = k.sb("hT", [128, 16, GT], BF16)
        wt = [k.sb("wt%d" % i, [128, 16, 512], BF16) for i in range(2)]
        wsm = k.sb("wsm", [128, 16, 96], BF16)
        xt = [k.sb("xt%d" % i, [128, D], F32) for i in range(2)]
        xn = [k.sb("xn%d" % i, [128, D], BF16) for i in range(2)]
        junk = k.sb("junk", [128, D], BF16)
        ss = [k.sb("ss%d" % i, [128, 4], F32) for i in range(2)]
        stg = [k.sb("stg%d" % i, [128, 512], BF16) for i in range(4)]
        stgf = [k.sb("stgf%d" % i, [128, 512], F32) for i in range(2)]
        sq = [k.sb("sq%d" % i, [128, 512], BF16) for i in range(2)]
        rr = [k.sb("rr%d" % i, [128, 512], F32) for i in range(2)]
        cnt = {"stg": 0, "stgf": 0, "sq": 0, "wt": 0, "x": 0, "stv": 0}
        stv = [k.sb("stv%d" % i, [128, 4, 129], BF16) for i in range(2)]
        for i_ in range(2):
            k.op("dve", lambda e: e.memset(stv[i_][:], 1.0), w=[stv[i_]])

        w_in_v = w_in.t.rearrange("(c p) n -> p c n", p=128)
        w_bg_v = w_bg.t.rearrange("(c p) n -> p c n", p=128)
        k.dma("pool", wsm[:, :, 0:64], w_in_v[:, :, O_IK:O_IK + 64], r=[w_in], w=[wsm])
        k.dma("pool", wsm[:, :, 64:80], w_in_v[:, :, O_GLR:O_GLR + 16], r=[w_in], w=[wsm])
        k.dma("pool", wsm[:, :, 80:96], w_in_v[:, :, O_IW:O_IW + 16], r=[w_in], w=[wsm])

        def load_w(src_v, col0):
            t = wt[cnt["wt"] % 2]
            cnt["wt"] += 1
            k.dma("pool", t[:], src_v[:, :, col0:col0 + 512], w=[t])
            return t

        def stage():
            t = stg[cnt["stg"] % 4]
            cnt["stg"] += 1
            return t

        def fm_mm(ps, wtile, c0, ncol, hf):
            for c in range(16):
                k.op("pe", lambda e: e.matmul(ps[0:ncol, :], lhsT=wtile[:, c, c0:c0 + ncol],
                                              rhs=hT[:, c, hf * 512:(hf + 1) * 512],
                                              start=(c == 0), stop=(c == 15)),
                     r=[wtile, hT], w=[ps])

        def tm_mm(ps, wtile, c0, ncol, j):
            for c in range(16):
                k.op("pe", lambda e: e.matmul(ps[:, 0:ncol], lhsT=hT[:, c, j * 128:(j + 1) * 128],
                                              rhs=wtile[:, c, c0:c0 + ncol],
                                              start=(c == 0), stop=(c == 15)),
                     r=[wtile, hT], w=[ps])

        def headnorm(ps, gcol, scale, dst_ap, dst_t):
            s = sq[cnt["sq"] % 2]
            r_ = rr[cnt["sq"] % 2]
            cnt["sq"] += 1
            k.op("act", lambda e: e.activation(out=s[:], in_=ps[:], func=AF.Square), r=[ps], w=[s])
            ps2 = nps()
            k.op("pe", lambda e: e.matmul(ps2[:], lhsT=onesb[:], rhs=s[:], start=True, stop=True),
                 r=[onesb, s], w=[ps2])
            k.op("act", lambda e: e.activation(out=r_[:], in_=ps2[:], func=AF.Sqrt, bias=epsc[:, 0:1], scale=1.0),
                 r=[ps2, epsc], w=[r_])
            k.op("dve", lambda e: e.reciprocal(out=r_[:], in_=r_[:]), r=[r_], w=[r_])
            st = stage()
            if scale != 1.0:
                k.op("dve", lambda e: e.tensor_scalar(out=r_[:], in0=r_[:], scalar1=cst[:, gcol:gcol + 1], scalar2=float(scale),
                                                      op0=ALU.mult, op1=ALU.mult), r=[r_, cst], w=[r_])
            else:
                k.op("dve", lambda e: e.tensor_scalar(out=r_[:], in0=r_[:], scalar1=cst[:, gcol:gcol + 1], scalar2=None,
                                                      op0=ALU.mult), r=[r_, cst], w=[r_])
            k.op("dve", lambda e: e.tensor_tensor(out=st[:], in0=ps[:], in1=r_[:], op=ALU.mult), r=[ps, r_], w=[st])
            k.dma("sp", dst_ap, st[:], r=[st], w=[dst_t])

        for half in range(2):
            own = half == 1
            xsrc = x_own if own else x_pre
            for g in range(NG):
                tok_o = g * GT
                tok_v = half * TO + tok_o
                for j in range(G):
                    i = cnt["x"] % 2
                    cnt["x"] += 1
                    r0 = tok_o + j * 128
                    k.dma("sp", xt[i][:], xsrc[r0:r0 + 128, :], r=[xsrc], w=[xt[i]])
                    k.op("act", lambda e: e.activation(out=junk[:], in_=xt[i][:], func=AF.Square, accum_out=ss[i][:, 0:1]),
                         r=[xt[i]], w=[junk, ss[i]])
                    k.op("act", lambda e: e.activation(out=ss[i][:, 1:2], in_=ss[i][:, 0:1], func=AF.Sqrt, bias=epsc[:, 0:1],
                                                       scale=1.0 / D), r=[ss[i], epsc], w=[ss[i]])
                    k.op("dve", lambda e: e.reciprocal(out=ss[i][:, 2:3], in_=ss[i][:, 1:2]), r=[ss[i]], w=[ss[i]])
                    k.op("dve", lambda e: e.tensor_scalar(out=xn[i][:], in0=xt[i][:], scalar1=ss[i][:, 2:3], scalar2=None,
                                                          op0=ALU.mult), r=[xt[i], ss[i]], w=[xn[i]])
                    for hh in range(2):
                        ps = nps()
                        psb = ps.t[:].bitcast(BF16).rearrange("p (c n) -> p c n", n=128)
                        for c in range(8):
                            cc = hh * 8 + c
                            k.op("pe", lambda e: e.transpose(out=psb[:, c, :], in_=xn[i][:, cc * 128:(cc + 1) * 128],
                                                             identity=identb[:]), r=[xn[i], identb], w=[ps])
                        gb = cst[:, C_GMIX + hh * 8:C_GMIX + hh * 8 + 8].unsqueeze(2).to_broadcast([128, 8, 128])
                        k.op("dve", lambda e: e.tensor_tensor(out=hT[:, hh * 8:hh * 8 + 8, j * 128:(j + 1) * 128], in0=psb,
                                                              in1=gb, op=ALU.mult), r=[ps, cst], w=[hT])

                for hf in range(NH):
                    ps = nps()
                    fm_mm(ps, wsm, 0, 80, hf)
                    st = stage()
                    k.op("act", lambda e: e.activation(out=st[0:64, :], in_=ps[0:64, :], func=AF.Copy), r=[ps], w=[st])
                    c0 = tok_v + hf * 512
                    k.dma("sp", ikT_d[:, c0:c0 + 512], st[0:64, :], r=[st], w=[ikT_d])
                    sf = stgf[cnt["stgf"] % 2]
                    cnt["stgf"] += 1
                    k.op("dve", lambda e: e.tensor_copy(out=sf[64:80, :], in_=ps[64:80, :]), r=[ps], w=[sf])
                    k.dma("sp", glrT_d[:, c0:c0 + 512], sf[64:80, :], r=[sf], w=[glrT_d])
                if own:
                    for j in range(G):
                        ps = nps()
                        tm_mm(ps, wsm, 80, 16, j)
                        sf = stgf[cnt["stgf"] % 2]
                        cnt["stgf"] += 1
                        k.op("dve", lambda e: e.tensor_copy(out=sf[:, 0:16], in_=ps[:, 0:16]), r=[ps], w=[sf])
                        r0 = tok_o + j * 128
                        k.dma("sp", iw_d[r0:r0 + 128, :], sf[:, 0:16], r=[sf], w=[iw_d])

                blocks = []
                if own:
                    blocks.append(("tm", w_in_v, O_GQ, ("q", 0)))
                blocks.append(("tm", w_in_v, O_GK, ("k", 0)))
                blocks.append(("tm", w_in_v, O_GV, ("v", 0)))
                blocks.append(("tm", w_in_v, O_GV + 512, ("v", 512)))
                if own:
                    blocks.append(("fm", w_in_v, O_GR, ("gr", 0)))
                    blocks.append(("fm", w_in_v, O_GR + 512, ("gr", 4)))
                    blocks.append(("fm", w_in_v, O_DQ, ("dq", 0)))
                    blocks.append(("fm", w_in_v, O_DQ + 512, ("dq", 4)))
                blocks.append(("fm", w_in_v, O_DK, ("dk", 0)))
                blocks.append(("fm", w_in_v, O_DK + 512, ("dk", 4)))
                blocks.append(("tm", w_in_v, O_DV, ("dv", 0)))
                blocks.append(("tm", w_in_v, O_DV + 512, ("dv", 4)))
                if own:
                    blocks.append(("fm", w_in_v, O_IQ, ("iq", 0)))
                    blocks.append(("fm", w_in_v, O_IQ + 512, ("iq", 4)))
                    for bgi in range(8):
                        blocks.append(("fm", w_bg_v, bgi * 512, ("bg", bgi * 4)))

                nxt = load_w(blocks[0][1], blocks[0][2])
                for bi, (kind, src, col0, (nm, off)) in enumerate(blocks):
                    wtile = nxt
                    if bi + 1 < len(blocks):
                        nxt = load_w(blocks[bi + 1][1], blocks[bi + 1][2])
                    if kind == "tm":
                        for j in range(G):
                            ps = nps()
                            tm_mm(ps, wtile, 0, 512, j)
                            if nm == "dv":
                                sv = stv[cnt["stv"] % 2]
                                cnt["stv"] += 1
                                k.op("dve", lambda e: e.tensor_copy(out=sv[:, :, 0:128], in_=ps[:].rearrange("p (h d) -> p h d", d=128)),
                                     r=[ps], w=[sv])
                                vt = half * NT + g * G + j
                                dst = Vd[off:off + 4, :, vt, :].rearrange("h p d -> p h d")
                                k.dma("sp", dst, sv[:], r=[sv], w=[Vd])
                                continue
                            st = stage()
                            if (j % 2) == 0:
                                k.op("act", lambda e: e.activation(out=st[:], in_=ps[:], func=AF.Copy), r=[ps], w=[st])
                            else:
                                k.op("dve", lambda e: e.tensor_copy(out=st[:], in_=ps[:]), r=[ps], w=[st])
                            ro = tok_o + j * 128
                            rv = tok_v + j * 128
                            if nm == "q":
                                k.dma("sp", q_d[ro:ro + 128, :], st[:], r=[st], w=[q_d])
                            elif nm == "k":
                                k.dma("sp", k_d[rv:rv + 128, :], st[:], r=[st], w=[k_d])
                            elif nm == "v":
                                k.dma("sp", v_d[rv:rv + 128, off:off + 512], st[:], r=[st], w=[v_d])
                    else:
                        for sbi in range(4):
                            for hf in range(NH):
                                ps = nps()
                                fm_mm(ps, wtile, sbi * 128, 128, hf)
                                blk = off + sbi
                                co = tok_o + hf * 512
                                cv = tok_v + hf * 512
                                if nm == "gr":
                                    st = stage()
                                    k.op("act", lambda e: e.activation(out=st[:], in_=ps[:], func=AF.Silu), r=[ps], w=[st])
                                    k.dma("sp", grT_d[blk * 128:(blk + 1) * 128, co:co + 512], st[:], r=[st], w=[grT_d])
                                elif nm == "iq":
                                    st = stage()
                                    k.op("dve", lambda e: e.tensor_copy(out=st[:], in_=ps[:]), r=[ps], w=[st])
                                    k.dma("sp", iqT_d[blk * 128:(blk + 1) * 128, co:co + 512], st[:], r=[st], w=[iqT_d])
                                elif nm == "bg":
                                    st = stage()
                                    k.op("act", lambda e: e.activation(out=st[:], in_=ps[:], func=AF.Sigmoid,
                                                                       bias=cst[:, C_BBG + blk:C_BBG + blk + 1], scale=1.0),
                                         r=[ps, cst], w=[st])
                                    k.dma("sp", gateT_d[blk * 128:(blk + 1) * 128, co:co + 512], st[:], r=[st], w=[gateT_d])
                                elif nm == "dq":
                                    headnorm(ps, C_GQ, 128.0 ** -0.5, qT_d[blk, :, co:co + 512], qT_d)
                                elif nm == "dk":
                                    headnorm(ps, C_GK, 1.0, KT_d[blk, :, cv:cv + 512], KT_d)
                k.barrier()
        k.barrier()
        k.pop()
        ph.close()

    def phase2():
        ph = ExitStack()
        k.push(ph)
        wal = k.sb("wal", [16, 512], F32)
        bal = k.sb("bal", [1, 512], F32)
        onesrow = k.sb("onesrow", [1, 128], F32)
        onescol = k.sb("onescol", [128, 1], F32)
        onec = k.sb("onec", [128, 1], F32)
        trif = k.sb("trif", [128, 128], F32)
        trib = k.sb("trib", [128, 4, 128], BF16)
        S = k.sb("S", [64, 8, 128], F32)
        Sb = k.sb("Sb", [64, 8, 128], BF16)
        k.dma("sp", wal[:], w_alpha[:], w=[wal])
        k.dma("sp", bal[:], b_alpha[:], w=[bal])
        k.dma("sp", trif[:], trid[:], w=[trif])
        k.op("dve", lambda e: e.memset(onesrow[:], 1.0), w=[onesrow])
        k.op("dve", lambda e: e.memset(onescol[:], 1.0), w=[onescol])
        k.op("dve", lambda e: e.memset(onec[:], 1.0), w=[onec])
        k.op("dve", lambda e: e.memset(S[:], 0.0), w=[S])
        k.op("dve", lambda e: e.memset(Sb[:], 0.0), w=[Sb])
        for u in range(4):
            k.op("dve", lambda e: e.tensor_copy(out=trib[:, u, :], in_=trif[:]), r=[trif], w=[trib])
        NB = 2
        kt_ = [k.sb("kt%d" % i, [128, 512], BF16) for i in range(NB)]
        vt_ = [k.sb("vt%d" % i, [128, 1024], BF16) for i in range(NB)]
        qt_ = [k.sb("qt%d" % i, [128, 512], BF16) for i in range(NB)]
        gl_ = [k.sb("gl%d" % i, [16, 128], F32) for i in range(NB)]
        gr_ = [k.sb("gr%d" % i, [128, 8, 128], BF16) for i in range(NB)]
        e1 = k.sb("e1", [128, 512], F32)
        spt = k.sb("spt", [128, 512], F32)
        Ep = k.sb("Ep", [128, 512], F32)
        Em = k.sb("Em", [128, 512], F32)
        ktl = k.sb("ktl", [128, 512], BF16)
        qtl = k.sb("qtl", [128, 512], BF16)
        kTt = k.sb("kTt", [64, 8, 128], BF16)
        qTt = k.sb("qTt", [64, 8, 128], BF16)
        egl = k.sb("egl", [64, 8], F32)
        ATs = k.sb("ATs", [128, 8, 128], BF16)
        sq2 = k.sb("sq2", [128, 512], BF16)
        rr2 = k.sb("rr2", [128, 512], F32)
        ost = [k.sb("ost%d" % i, [128, 4, 128], BF16) for i in range(2)]
        grv = grT_d.t.rearrange("(h p) t -> p h t", p=128)
        oav = oaT_d.t.rearrange("(h p) t -> p h t", p=128)

        def loads(vt):
            i = vt % NB
            own = vt >= NT
            r0 = vt * 128
            k.dma("sp", kt_[i][:], k_d[r0:r0 + 128, :], r=[k_d], w=[kt_[i]])
            k.dma("sp", vt_[i][:], v_d[r0:r0 + 128, :], r=[v_d], w=[vt_[i]])
            k.dma("sp", gl_[i][:], glrT_d[:, r0:r0 + 128], r=[glrT_d], w=[gl_[i]])
            if own:
                ro = r0 - TO
                k.dma("sp", qt_[i][:], q_d[ro:ro + 128, :], r=[q_d], w=[qt_[i]])
                k.dma("sp", gr_[i][:], grv[:, :, ro:ro + 128], r=[grT_d], w=[gr_[i]])

        loads(0)
        for vt in range(2 * NT):
            i = vt % NB
            own = vt >= NT
            ro = vt * 128 - TO
            if vt + 1 < 2 * NT:
                loads(vt + 1)
            if vt == NT:
                k.op("dve", lambda e: e.tensor_scalar(out=S[:], in0=S[:], scalar1=cst[0:64, C_PREV:C_PREV + 1], scalar2=None,
                                                      op0=ALU.mult), r=[S, cst], w=[S])
                k.op("dve", lambda e: e.tensor_copy(out=Sb[:], in_=S[:]), r=[S], w=[Sb])
            ps = nps()
            k.op("pe", lambda e: e.matmul(ps[:], lhsT=gl_[i][:], rhs=wal[:], start=True, stop=False), r=[gl_[i], wal], w=[ps])
            k.op("pe", lambda e: e.matmul(ps[:], lhsT=onesrow[:], rhs=bal[:], start=False, stop=True), r=[onesrow, bal], w=[ps])
            k.op("act", lambda e: e.activation(out=e1[:], in_=ps[:], func=AF.Exp, scale=-1.0), r=[ps], w=[e1])
            k.op("act", lambda e: e.activation(out=spt[:], in_=e1[:], func=AF.Ln, bias=onec[:, 0:1], scale=1.0), r=[e1, onec], w=[spt])
            psG = nps()
            k.op("pe", lambda e: e.matmul(psG[:], lhsT=trif[:], rhs=spt[:], start=True, stop=True), r=[trif, spt], w=[psG])
            k.op("act", lambda e: e.activation(out=Em[:], in_=psG[:], func=AF.Exp, scale=1.0 / 16.0), r=[psG], w=[Em])
            if own:
                k.op("act", lambda e: e.activation(out=Ep[:], in_=psG[:], func=AF.Exp, scale=-1.0 / 16.0), r=[psG], w=[Ep])
            psE = nps()
            for h in range(8):
                k.op("pe", lambda e: e.matmul(psE[0:64, h:h + 1], lhsT=spt[:, h * 64:(h + 1) * 64], rhs=onescol[:], start=True, stop=True),
                     r=[spt, onescol], w=[psE])
            k.op("act", lambda e: e.activation(out=egl[:], in_=psE[0:64, 0:8], func=AF.Exp, scale=-1.0 / 16.0), r=[psE], w=[egl])
            k.op("dve", lambda e: e.tensor_tensor(out=ktl[:], in0=kt_[i][:], in1=Em[:], op=ALU.mult), r=[kt_[i], Em], w=[ktl])
            if own:
                k.op("dve", lambda e: e.scalar_tensor_tensor(out=qtl[:], in0=qt_[i][:], scalar=0.125, in1=Ep[:], op0=ALU.mult, op1=ALU.mult),
                     r=[qt_[i], Ep], w=[qtl])
                for (src, dst) in ((ktl, kTt), (qtl, qTt)):
                    pst = nps()
                    pstb = pst.t[:].bitcast(BF16).rearrange("p (c n) -> p c n", n=128)
                    for h in range(8):
                        k.op("pe", lambda e: e.transpose(out=pstb[0:64, h, :], in_=src[:, h * 64:(h + 1) * 64], identity=identb[:]),
                             r=[src, identb], w=[pst])
                    k.op("dve", lambda e: e.tensor_copy(out=dst[:], in_=pstb[0:64, :, :]), r=[pst], w=[dst])
                for hg in range(2):
                    psA = nps()
                    for u in range(4):
                        h = hg * 4 + u
                        k.op("pe", lambda e: e.matmul(psA[:, u * 128:(u + 1) * 128], lhsT=kTt[:, h, :], rhs=qTt[:, h, :], start=True, stop=True),
                             r=[kTt, qTt], w=[psA])
                    k.op("dve", lambda e: e.tensor_tensor(out=ATs[:, hg * 4:hg * 4 + 4, :], in0=psA[:].rearrange("p (u n) -> p u n", n=128),
                                                          in1=trib[:], op=ALU.mult), r=[psA, trib], w=[ATs])
                for hg in range(2):
                    psO = nps()
                    for u in range(4):
                        h = hg * 4 + u
                        k.op("pe", lambda e: e.matmul(psO[:, u * 128:(u + 1) * 128], lhsT=vt_[i][:, h * 128:(h + 1) * 128], rhs=ATs[:, h, :],
                                                      start=True, stop=False), r=[vt_[i], ATs], w=[psO])
                        k.op("pe", lambda e: e.matmul(psO[:, u * 128:(u + 1) * 128], lhsT=Sb[:, h, :], rhs=qTt[:, h, :],
                                                      start=False, stop=True), r=[Sb, qTt], w=[psO])
                    k.op("act", lambda e: e.activation(out=sq2[:], in_=psO[:], func=AF.Square), r=[psO], w=[sq2])
                    psM = nps()
                    k.op("pe", lambda e: e.matmul(psM[:], lhsT=onesb[:], rhs=sq2[:], start=True, stop=True), r=[onesb, sq2], w=[psM])
                    k.op("act", lambda e: e.activation(out=rr2[:], in_=psM[:], func=AF.Sqrt, bias=epsc[:, 0:1], scale=1.0), r=[psM, epsc], w=[rr2])
                    k.op("dve", lambda e: e.reciprocal(out=rr2[:], in_=rr2[:]), r=[rr2], w=[rr2])
                    k.op("dve", lambda e: e.scalar_tensor_tensor(out=rr2[:], in0=rr2[:], scalar=cst[:, C_GGLA:C_GGLA + 1],
                                                                 in1=gr_[i][:, hg * 4:hg * 4 + 4, :].rearrange("p u n -> p (u n)"),
                                                                 op0=ALU.mult, op1=ALU.mult), r=[rr2, cst, gr_[i]], w=[rr2])
                    o_ = ost[hg]
                    k.op("dve", lambda e: e.tensor_tensor(out=o_[:].rearrange("p u n -> p (u n)"), in0=psO[:], in1=rr2[:], op=ALU.mult),
                         r=[psO, rr2], w=[o_])
                    k.dma("sp", oav[:, hg * 4:hg * 4 + 4, ro:ro + 128], o_[:], r=[o_], w=[oaT_d])
            for hg in range(2):
                psU = nps()
                for u in range(4):
                    h = hg * 4 + u
                    k.op("pe", lambda e: e.matmul(psU[0:64, u * 128:(u + 1) * 128], lhsT=ktl[:, h * 64:(h + 1) * 64], rhs=vt_[i][:, h * 128:(h + 1) * 128],
                                                  start=True, stop=True), r=[ktl, vt_[i]], w=[psU])
                k.op("dve", lambda e: e.tensor_tensor(out=S[:, hg * 4:hg * 4 + 4, :], in0=psU[0:64, :].rearrange("p (u n) -> p u n", n=128),
                                                      in1=S[:, hg * 4:hg * 4 + 4, :], op=ALU.add), r=[psU, S], w=[S])
            k.op("dve", lambda e: e.tensor_tensor(out=S[:], in0=S[:], in1=egl[:].unsqueeze(2).to_broadcast([64, 8, 128]), op=ALU.mult),
                 r=[S, egl], w=[S])
            k.op("dve", lambda e: e.tensor_copy(out=Sb[:], in_=S[:]), r=[S], w=[Sb])
        k.barrier()
        k.pop()
        ph.close()

    def phase3():
        ph = ExitStack()
        k.push(ph)
        NKM = TV
        NKP = NKM + 128
        NIT = cfg.nit
        CK = 16
        ik2 = k.sb("ik2", [128, TV], BF16)
        k.dma("sp", ik2[0:64, :], ikT_d[:, :], r=[ikT_d], w=[ik2])
        k.dma("sp", ik2[64:128, :], ikT_d[:, :], r=[ikT_d], w=[ik2])
        cb = k.sb("cb", [128, 8], F32)
        tbb = k.sb("tbb", [128, 8, 2, 128], BF16)
        k.dma("pool", tbb[:], tbd[:], w=[tbb])
        k.dma("sp", cb[:], cbd[:], w=[cb])
        k.op("dve", lambda e: e.tensor_tensor(out=tbb[:].rearrange("p h a q -> p h (a q)"), in0=tbb[:].rearrange("p h a q -> p h (a q)"),
                                              in1=cb[:].unsqueeze(2).to_broadcast([128, 8, 256]), op=ALU.subtract), r=[tbb, cb], w=[tbb])
        sc = [k.sb("sc%d" % i, [128, NKP], F32) for i in range(2)]
        mk = k.sb("mk", [128, NKP], BF16)
        mkT = k.sb("mkT", [128, 2 * NT, 128], BF16)
        rl = [k.sb("rl%d" % i, [128, 512], BF16) for i in range(4)]
        iq = [k.sb("iq%d" % i, [128, 2, 8, 128], BF16) for i in range(3)]
        for i_ in range(3):
            k.op("pool", lambda e: e.memset(iq[i_][:], 0.0), w=[iq[i_]])
        iwt = [k.sb("iwt%d" % i, [128, 16], F32) for i in range(3)]
        qTt = [k.sb("qTd%d" % i, [128, 8, 128], BF16) for i in range(3)]
        aw = k.sb("aw", [128, 16], F32)
        sg = k.sb("sg", [128, 16], F32)
        dg = k.sb("dg", [128, 16, 128], BF16)
        gm = k.sb("gm", [128, 256], F32)
        bs = k.sb("bs", [128, 8], F32)
        NCB = 4
        kth = [k.sb("kth%d" % i, [128, CK * 128], BF16) for i in range(NCB)]
        vh = [k.sb("vh%d" % i, [128, CK, 129], BF16) for i in range(NCB)]
        pe_ = [k.sb("pe%d" % i, [128, 4, 128], BF16) for i in range(3)]
        pT = [k.sb("pT%d" % i, [128, 4, 128], BF16) for i in range(3)]
        obr = k.sb("obr", [128, 8, 129], F32)
        ob = k.sb("ob", [128, 8, 128], BF16)
        obT = k.sb("obT", [128, 8, 128], BF16)
        rden = k.sb("rden", [128, 8], F32)
        iqv = iqT_d.t.rearrange("(c two d) t -> two d c t", two=2, d=64)
        qTv = qT_d.t.rearrange("h p t -> p h t")
        obv = obT_d.t.rearrange("(h p) t -> p h t", p=128)
        p4c = [0]

        def nps4():
            p = psum[p4c[0] % 4]
            p4c[0] += 1
            return p
        cn = {"rl": 0, "pp": 0, "cb": 0, "ss": 0}

        def geo(t):
            vt = NT + t
            nkt = vt + 1
            return vt, nkt, nkt * 128

        def tloads(t):
            i = t % 3
            ro = t * 128
            k.dma("sp", iq[i][0:64, 0, :, :], iqv[0, :, :, ro:ro + 128], r=[iqT_d], w=[iq[i]])
            k.dma("sp", iq[i][64:128, 1, :, :], iqv[1, :, :, ro:ro + 128], r=[iqT_d], w=[iq[i]])
            k.dma("sp", iwt[i][:], iw_d[ro:ro + 128, :], r=[iw_d], w=[iwt[i]])
            k.dma("sp", qTt[i][:], qTv[:, :, ro:ro + 128], r=[qT_d], w=[qTt[i]])

        def stageA(t):
            i = t % 3
            vt, nkt, NK = geo(t)
            scb = sc[t % 2]
            nkb = (NK + 511) // 512
            k.op("act", lambda e: e.activation(out=aw[:], in_=iwt[i][:], func=AF.Abs, scale=1.0 / 32.0), r=[iwt[i]], w=[aw])
            k.op("act", lambda e: e.activation(out=sg[:], in_=iwt[i][:], func=AF.Sign), r=[iwt[i]], w=[sg])
            for j in range(16):
                k.op("pool", lambda e: e.tensor_scalar(out=dg[:, j, :], in0=identb[:], scalar1=sg[:, j:j + 1], scalar2=None, op0=ALU.mult),
                     r=[identb, sg], w=[dg])
            for kb in range(nkb):
                wd = min(512, NK - kb * 512)
                c0 = kb * 512
                psS = psum[4 + (cn["ss"] % 2)]
                cn["ss"] += 1
                pend = []

                def acc(jj, r__):
                    k.op("pe", lambda e: e.matmul(psS[:, 0:wd], lhsT=dg[:, jj, :], rhs=r__[:, 0:wd], start=(jj == 0), stop=(jj == 15)),
                         r=[dg, r__], w=[psS])
                for j in range(16):
                    c = j // 2
                    po = (j % 2) * 64
                    ps = nps4()
                    k.op("pe", lambda e: e.matmul(ps[:, 0:wd], lhsT=iq[i][:, j % 2, c, :], rhs=ik2[:, c0:c0 + wd],
                                                  start=True, stop=True), r=[iq[i], ik2], w=[ps])
                    r_ = rl[cn["rl"] % 4]
                    cn["rl"] += 1
                    k.op("act", lambda e: e.activation(out=r_[:, 0:wd], in_=ps[:, 0:wd], func=AF.Relu, scale=aw[:, j:j + 1]),
                         r=[ps, aw], w=[r_])
                    pend.append((j, r_))
                    if len(pend) > 2:
                        acc(*pend.pop(0))
                while pend:
                    acc(*pend.pop(0))
                if c0 < TO:
                    k.op("act", lambda e: e.activation(out=scb[:, c0:c0 + wd], in_=psS[:, 0:wd], func=AF.Identity,
                                                       bias=cst[:, C_PREB:C_PREB + 1], scale=1.0), r=[psS, cst], w=[scb])
                else:
                    k.op("act", lambda e: e.activation(out=scb[:, c0:c0 + wd], in_=psS[:, 0:wd], func=AF.Copy), r=[psS], w=[scb])

        def stageB(t):
            vt, nkt, NK = geo(t)
            scb = sc[t % 2]
            k.op("dve", lambda e: e.memset(scb[0:64, NK - 64:NK], -1e30), w=[scb])
            NKp = NK
            if nkt % 2 == 1:
                k.op("dve", lambda e: e.memset(scb[:, NK:NK + 128], -1e30), w=[scb])
                NKp = NK + 128
            k.op("dve", lambda e: e.tensor_reduce(out=gm[:], in_=scb[:, 0:NKp].rearrange("p (a g) -> p g a", g=256), axis=AX.X, op=ALU.max),
                 r=[scb], w=[gm])
            k.op("dve", lambda e: e.tensor_reduce(out=bs[:, 0:1], in_=gm[:], axis=AX.X, op=ALU.min), r=[gm], w=[bs])
            k.op("dve", lambda e: e.tensor_reduce(out=bs[:, 1:2], in_=gm[:], axis=AX.X, op=ALU.max), r=[gm], w=[bs])
            k.op("dve", lambda e: e.tensor_tensor(out=bs[:, 2:3], in0=bs[:, 1:2], in1=bs[:, 0:1], op=ALU.subtract), r=[bs], w=[bs])
            for it in range(NIT):
                ck = 2.0 ** -(it + 1)
                k.op("dve", lambda e: e.scalar_tensor_tensor(out=bs[:, 3:4], in0=bs[:, 2:3], scalar=ck, in1=bs[:, 0:1], op0=ALU.mult, op1=ALU.add),
                     r=[bs], w=[bs])
                k.op("dve", lambda e: e.tensor_scalar(out=mk[:, 0:NK], in0=scb[:, 0:NK], scalar1=bs[:, 3:4], scalar2=0.0, op0=ALU.is_ge, op1=ALU.add,
                                                      accum_out=bs[:, 4:5]), r=[scb, bs], w=[mk, bs])
                k.op("dve", lambda e: e.tensor_scalar(out=bs[:, 5:6], in0=bs[:, 4:5], scalar1=255.5, scalar2=ck, op0=ALU.is_ge, op1=ALU.mult),
                     r=[bs], w=[bs])
                k.op("dve", lambda e: e.scalar_tensor_tensor(out=bs[:, 0:1], in0=bs[:, 5:6], scalar=bs[:, 2:3], in1=bs[:, 0:1], op0=ALU.mult, op1=ALU.add),
                     r=[bs], w=[bs])
            k.op("dve", lambda e: e.tensor_scalar(out=bs[:, 6:7], in0=bs[:, 0:1], scalar1=-1e29, scalar2=None, op0=ALU.max), r=[bs], w=[bs])
            k.op("dve", lambda e: e.tensor_scalar(out=mk[:, 0:NK], in0=scb[:, 0:NK], scalar1=bs[:, 6:7], scalar2=-30000.0, op0=ALU.is_lt, op1=ALU.mult),
                 r=[scb, bs], w=[mk])

        def maskT(t):
            vt, nkt, NK = geo(t)
            for g8 in range((nkt + 7) // 8):
                n = min(8, nkt - g8 * 8)
                ps = nps4()
                psb = ps.t[:].bitcast(BF16).rearrange("p (c n) -> p c n", n=128)
                for u in range(n):
                    kt = g8 * 8 + u
                    k.op("pe", lambda e: e.transpose(out=psb[:, u, :], in_=mk[:, kt * 128:(kt + 1) * 128], identity=identb[:]),
                         r=[mk, identb], w=[ps])
                k.op("act", lambda e: e.activation(out=mkT[:, g8 * 8:g8 * 8 + n, :], in_=psb[:, 0:n, :], func=AF.Copy), r=[ps], w=[mkT])

        def attn(t):
            i = t % 3
            ro = t * 128
            vt, nkt, NK = geo(t)
            nck = (nkt + CK - 1) // CK
            seq = [(h, c_) for h in range(8) for c_ in range(nck)]
            bufs = {}

            def cload(idx):
                h, c_ = seq[idx]
                bi = cn["cb"] % NCB
                cn["cb"] += 1
                k0 = c_ * CK
                n_ = min(CK, nkt - k0)
                k.dma("sp", kth[bi][:, 0:n_ * 128], KT_d[h, :, k0 * 128:(k0 + n_) * 128], r=[KT_d], w=[kth[bi]])
                k.dma("sp", vh[bi][:, 0:n_, :], Vd[h, :, k0:k0 + n_, :], r=[Vd], w=[vh[bi]])
                bufs[idx] = bi
            for idx in range(min(2, len(seq))):
                cload(idx)
            prev = None
            state = {}

            def pv(st):
                hh, kts, p_, bi, psO = st
                for u, kt in enumerate(kts):
                    k.op("pe", lambda e: e.matmul(psO[:, 0:129], lhsT=p_[:, u, :], rhs=vh[bi][:, kt % CK, :],
                                                  start=(kt == 0), stop=(kt == nkt - 1)), r=[p_, vh[bi]], w=[psO])
                if kts[-1] == nkt - 1:
                    k.op("act", lambda e: e.activation(out=obr[:, hh, :], in_=psO[:, 0:129], func=AF.Copy), r=[psO], w=[obr])
            for idx, (h, c_) in enumerate(seq):
                if idx + 2 < len(seq):
                    cload(idx + 2)
                bi = bufs[idx]
                psO = psum[6 + (h % 2)]
                k0 = c_ * CK
                n_ = min(CK, nkt - k0)
                for kg in range((n_ + 3) // 4):
                    kts = [k0 + kg * 4 + u for u in range(min(4, n_ - kg * 4))]
                    n = len(kts)
                    ps = nps4()
                    for u, kt in enumerate(kts):
                        near = kt >= vt - 1
                        kl = kt - k0
                        k.op("pe", lambda e: e.matmul(ps[:, u * 128:(u + 1) * 128], lhsT=kth[bi][:, kl * 128:(kl + 1) * 128], rhs=qTt[i][:, h, :],
                                                      start=True, stop=False), r=[kth[bi], qTt[i]], w=[ps])
                        if near:
                            a = vt - kt
                            k.op("pe", lambda e: e.matmul(ps[:, u * 128:(u + 1) * 128], lhsT=identb[:], rhs=tbb[:, h, a, :],
                                                          start=False, stop=False), r=[identb, tbb], w=[ps])
                        k.op("pe", lambda e: e.matmul(ps[:, u * 128:(u + 1) * 128], lhsT=identb[:], rhs=mkT[:, kt, :],
                                                      start=False, stop=True), r=[identb, mkT], w=[ps])
                    p_ = pT[cn["pp"] % 3]
                    cn["pp"] += 1
                    k.op("act", lambda e: e.activation(out=p_[:, 0:n, :].rearrange("p u n -> p (u n)"), in_=ps[:, 0:n * 128], func=AF.Exp,
                                                       bias=cb[:, h:h + 1], scale=1.0), r=[ps, cb], w=[p_])
                    cur = (h, kts, p_, bi, psO)
                    if prev is not None:
                        pv(prev)
                    prev = cur
            pv(prev)
            k.op("dve", lambda e: e.reciprocal(out=rden[:], in_=obr[:, :, 128]), r=[obr], w=[rden])
            k.op("dve", lambda e: e.tensor_tensor(out=ob[:], in0=obr[:, :, 0:128], in1=rden[:].unsqueeze(2).to_broadcast([128, 8, 128]), op=ALU.mult),
                 r=[obr, rden], w=[ob])
            ps = nps4()
            psb = ps.t[:].bitcast(BF16).rearrange("p (c n) -> p c n", n=128)
            for h in range(8):
                k.op("pe", lambda e: e.transpose(out=psb[:, h, :], in_=ob[:, h, :], identity=identb[:]), r=[ob, identb], w=[ps])
            k.op("act", lambda e: e.activation(out=obT[:], in_=psb, func=AF.Copy), r=[ps], w=[obT])
            k.dma("sp", obv[:, :, ro:ro + 128], obT[:], r=[obT], w=[obT_d])

        tloads(0)
        if NT > 1:
            tloads(1)
        stageA(0)
        stageB(0)
        if NT > 1:
            stageA(1)
        for t in range(NT):
            if t + 2 < NT:
                tloads(t + 2)
            maskT(t)
            if t + 1 < NT:
                stageB(t + 1)
            attn(t)
            if t + 2 < NT:
                stageA(t + 2)
        k.barrier()
        k.pop()
        ph.close()

    def phase4a():
        ph = ExitStack()
        k.push(ph)
        wog = k.sb("wog", [128, 8, D], BF16)
        wod = k.sb("wod", [128, 8, D], BF16)
        for c in range(8):
            k.dma("pool", wog[:, c, :], w_oa[c * 128:(c + 1) * 128, :], r=[w_oa], w=[wog])
            k.dma("pool", wod[:, c, :], w_ob[c * 128:(c + 1) * 128, :], r=[w_ob], w=[wod])
        oag = [k.sb("oag%d" % i, [128, 8, 512], BF16) for i in range(2)]
        obg = [k.sb("obg%d" % i, [128, 8, 512], BF16) for i in range(2)]
        gt = [k.sb("gt%d" % i, [128, 2, 512], BF16) for i in range(3)]
        t1 = [k.sb("t1_%d" % i, [128, 512], F32) for i in range(2)]
        t2 = [k.sb("t2_%d" % i, [128, 512], F32) for i in range(2)]
        mst = [k.sb("mst%d" % i, [128, 512], BF16) for i in range(3)]
        oav = oaT_d.t.rearrange("(h p) t -> p h t", p=128)
        obv = obT_d.t.rearrange("(h p) t -> p h t", p=128)
        gtv = gateT_d.t.rearrange("(a c p) t -> p a c t", a=2, p=128)
        n = 0
        for g4 in range(NT // 4):
            t0 = g4 * 512
            i = g4 % 2
            k.dma("sp", oag[i][:], oav[:, :, t0:t0 + 512], r=[oaT_d], w=[oag[i]])
            k.dma("sp", obg[i][:], obv[:, :, t0:t0 + 512], r=[obT_d], w=[obg[i]])
            for cbk in range(16):
                g_ = gt[n % 3]
                k.dma("sp", g_[:], gtv[:, :, cbk, t0:t0 + 512], r=[gateT_d], w=[g_])
                psA = nps()
                for kc in range(8):
                    k.op("pe", lambda e: e.matmul(psA[:], lhsT=wog[:, kc, cbk * 128:(cbk + 1) * 128], rhs=oag[i][:, kc, :],
                                                  start=(kc == 0), stop=(kc == 7)), r=[wog, oag[i]], w=[psA])
                psB = nps()
                for kc in range(8):
                    k.op("pe", lambda e: e.matmul(psB[:], lhsT=wod[:, kc, cbk * 128:(cbk + 1) * 128], rhs=obg[i][:, kc, :],
                                                  start=(kc == 0), stop=(kc == 7)), r=[wod, obg[i]], w=[psB])
                a_ = t1[n % 2]
                b_ = t2[n % 2]
                m_ = mst[n % 3]
                k.op("dve", lambda e: e.tensor_tensor(out=a_[:], in0=psA[:], in1=g_[:, 0, :], op=ALU.mult), r=[psA, g_], w=[a_])
                k.op("dve", lambda e: e.tensor_tensor(out=b_[:], in0=psB[:], in1=g_[:, 1, :], op=ALU.mult), r=[psB, g_], w=[b_])
                k.op("pool", lambda e: e.tensor_tensor(out=m_[:], in0=a_[:], in1=b_[:], op=ALU.add), r=[a_, b_], w=[m_])
                k.dma("sp", mT_d[cbk * 128:(cbk + 1) * 128, t0:t0 + 512], m_[:], r=[m_], w=[mT_d])
                n += 1
        k.barrier()
        k.pop()
        ph.close()

    def phase4b():
        ph = ExitStack()
        k.push(ph)
        wo = k.sb("wo", [128, 16, D], BF16)
        for c in range(16):
            k.dma("pool", wo[:, c, :], w_o[c * 128:(c + 1) * 128, :], r=[w_o], w=[wo])
        wr = k.sb("wr", [128, 16, 72], BF16)
        k.dma("pool", wr[:], w_r.t.rearrange("(c p) n -> p c n", p=128), r=[w_r], w=[wr])
        brb = k.sb("brb", [1, 72], BF16)
        k.dma("pool", brb[:], b_r[:], r=[b_r], w=[brb])
        onesrb = k.sb("onesrb", [1, 128], BF16)
        k.op("dve", lambda e: e.memset(onesrb[:], 1.0), w=[onesrb])
        gffn = k.sb("gffn", [128, D], F32)
        k.dma("sp", gffn[:], gffn_row.t.partition_broadcast(128), r=[gffn_row], w=[gffn])
        trif = k.sb("trif4", [128, 128], F32)
        k.dma("sp", trif[:], trid[:], w=[trif])
        stri = k.sb("stri", [128, 128], BF16)
        k.op("dve", lambda e: e.tensor_tensor(out=stri[:], in0=trif[:], in1=identf[:], op=ALU.subtract), r=[trif, identf], w=[stri])
        ones1 = k.sb("ones1", [128, 128], BF16)
        k.op("dve", lambda e: e.memset(ones1[:], 1.0), w=[ones1])
        ebase = k.sb("ebase", [128, 64], F32)
        k.dma("sp", ebase[:], ebased[:], w=[ebase])
        run = k.sb("run", [128, 64], F32)
        k.op("dve", lambda e: e.memset(run[:], 0.0), w=[run])
        mt = [k.sb("mt%d" % i, [128, 16, 128], BF16) for i in range(2)]
        xt = [k.sb("x4_%d" % i, [128, D], F32) for i in range(2)]
        junk = k.sb("junk4", [128, D], BF16)
        hf = [k.sb("hf%d" % i, [128, D], BF16) for i in range(2)]
        hfT = k.sb("hfT", [128, 16, 128], BF16)
        rs = k.sb("rs4", [128, 16], F32)
        lgt = k.sb("lgt", [128, 72], F32)
        ohg = k.sb("ohg", [128, 8], F32)
        pen = k.sb("pen", [128, 8], F32)
        ml = k.sb("ml", [128, 64], F32)
        ml2 = k.sb("ml2", [128, 64], F32)
        s1 = k.sb("s1", [128, 64], F32)
        s2 = k.sb("s2", [128, 64], F32)
        selb = k.sb("selb", [128, 64], BF16)
        pos = k.sb("pos", [128, 64], F32)
        junk64 = k.sb("junk64", [128, 64], F32)
        slf = k.sb("slf", [128, 2], F32)
        mtv = mT_d.t.rearrange("(c p) t -> p c t", p=128)

        def loads(t):
            i = t % 2
            ro = t * 128
            k.dma("sp", mt[i][:], mtv[:, :, ro:ro + 128], r=[mT_d], w=[mt[i]])
            k.dma("sp", xt[i][:], x_own[ro:ro + 128, :], r=[x_own], w=[xt[i]])
        loads(0)
        for t in range(NT):
            i = t % 2
            ro = t * 128
            if t + 1 < NT:
                loads(t + 1)
            for nb in range(4):
                ps = nps()
                for c in range(16):
                    k.op("pe", lambda e: e.matmul(ps[:], lhsT=mt[i][:, c, :], rhs=wo[:, c, nb * 512:(nb + 1) * 512],
                                                  start=(c == 0), stop=(c == 15)), r=[mt[i], wo], w=[ps])
                k.op("dve", lambda e: e.tensor_tensor(out=xt[i][:, nb * 512:(nb + 1) * 512], in0=ps[:], in1=xt[i][:, nb * 512:(nb + 1) * 512],
                                                      op=ALU.add), r=[ps, xt[i]], w=[xt[i]])
            k.dma("sp", x1_d[ro:ro + 128, :], xt[i][:], r=[xt[i]], w=[x1_d])
            k.op("act", lambda e: e.activation(out=junk[:], in_=xt[i][:], func=AF.Square, accum_out=rs[:, 0:1]), r=[xt[i]], w=[junk, rs])
            k.op("act", lambda e: e.activation(out=rs[:, 1:2], in_=rs[:, 0:1], func=AF.Sqrt, bias=epsc[:, 0:1], scale=1.0 / D), r=[rs, epsc], w=[rs])
            k.op("dve", lambda e: e.reciprocal(out=rs[:, 2:3], in_=rs[:, 1:2]), r=[rs], w=[rs])
            k.op("dve", lambda e: e.scalar_tensor_tensor(out=hf[i][:], in0=xt[i][:], scalar=rs[:, 2:3], in1=gffn[:], op0=ALU.mult, op1=ALU.mult),
                 r=[xt[i], rs, gffn], w=[hf[i]])
            for hh in range(2):
                ps = nps()
                psb = ps.t[:].bitcast(BF16).rearrange("p (c n) -> p c n", n=128)
                for c in range(8):
                    cc = hh * 8 + c
                    k.op("pe", lambda e: e.transpose(out=psb[:, c, :], in_=hf[i][:, cc * 128:(cc + 1) * 128], identity=identb[:]),
                         r=[hf[i], identb], w=[ps])
                if hh == 0:
                    k.op("act", lambda e: e.activation(out=hfT[:, 0:8, :], in_=psb, func=AF.Copy), r=[ps], w=[hfT])
                else:
                    k.op("dve", lambda e: e.tensor_copy(out=hfT[:, 8:16, :], in_=psb), r=[ps], w=[hfT])
            ps = nps()
            for c in range(16):
                k.op("pe", lambda e: e.matmul(ps[:, 0:72], lhsT=hfT[:, c, :], rhs=wr[:, c, :], start=(c == 0), stop=False), r=[hfT, wr], w=[ps])
            k.op("pe", lambda e: e.matmul(ps[:, 0:72], lhsT=onesrb[:], rhs=brb[:], start=False, stop=True), r=[onesrb, brb], w=[ps])
            k.op("dve", lambda e: e.tensor_copy(out=lgt[:], in_=ps[:, 0:72]), r=[ps], w=[lgt])
            k.op("dve", lambda e: e.tensor_reduce(out=rs[:, 3:4], in_=lgt[:, 0:8], axis=AX.X, op=ALU.max), r=[lgt], w=[rs])
            k.op("dve", lambda e: e.tensor_scalar(out=ohg[:], in0=lgt[:, 0:8], scalar1=rs[:, 3:4], scalar2=None, op0=ALU.is_ge), r=[lgt, rs], w=[ohg])
            k.op("dve", lambda e: e.tensor_scalar(out=rs[:, 4:5], in0=rs[:, 3:4], scalar1=-1.0, scalar2=None, op0=ALU.mult), r=[rs], w=[rs])
            k.op("act", lambda e: e.activation(out=pen[:], in_=lgt[:, 0:8], func=AF.Exp, bias=rs[:, 4:5], scale=1.0, accum_out=rs[:, 5:6]),
                 r=[lgt, rs], w=[pen, rs])
            k.op("dve", lambda e: e.reciprocal(out=rs[:, 6:7], in_=rs[:, 5:6]), r=[rs], w=[rs])
            k.op("dve", lambda e: e.tensor_scalar(out=pen[:], in0=ohg[:], scalar1=1e9, scalar2=-1e9, op0=ALU.mult, op1=ALU.add), r=[ohg], w=[pen])
            k.op("dve", lambda e: e.tensor_tensor(out=ml[:].rearrange("p (g e) -> p g e", e=8), in0=lgt[:, 8:72].rearrange("p (g e) -> p g e", e=8),
                                                  in1=pen[:].unsqueeze(2).to_broadcast([128, 8, 8]), op=ALU.add), r=[lgt, pen], w=[ml])
            k.op("dve", lambda e: e.tensor_reduce(out=rs[:, 7:8], in_=ml[:], axis=AX.X, op=ALU.max), r=[ml], w=[rs])
            k.op("dve", lambda e: e.tensor_scalar(out=s1[:], in0=ml[:], scalar1=rs[:, 7:8], scalar2=None, op0=ALU.is_ge), r=[ml, rs], w=[s1])
            k.op("dve", lambda e: e.scalar_tensor_tensor(out=ml2[:], in0=s1[:], scalar=-1e9, in1=ml[:], op0=ALU.mult, op1=ALU.add), r=[s1, ml], w=[ml2])
            k.op("dve", lambda e: e.tensor_reduce(out=rs[:, 8:9], in_=ml2[:], axis=AX.X, op=ALU.max), r=[ml2], w=[rs])
            k.op("dve", lambda e: e.tensor_scalar(out=s2[:], in0=ml2[:], scalar1=rs[:, 8:9], scalar2=None, op0=ALU.is_ge), r=[ml2, rs], w=[s2])
            k.op("dve", lambda e: e.tensor_tensor(out=rs[:, 9:10], in0=rs[:, 8:9], in1=rs[:, 7:8], op=ALU.subtract), r=[rs], w=[rs])
            k.op("act", lambda e: e.activation(out=rs[:, 10:11], in_=rs[:, 9:10], func=AF.Exp), r=[rs], w=[rs])
            k.op("dve", lambda e: e.tensor_scalar(out=rs[:, 11:12], in0=rs[:, 10:11], scalar1=1.0, scalar2=None, op0=ALU.add), r=[rs], w=[rs])
            k.op("dve", lambda e: e.reciprocal(out=rs[:, 12:13], in_=rs[:, 11:12]), r=[rs], w=[rs])
            k.op("dve", lambda e: e.tensor_tensor(out=wts[:, t, 0:1], in0=rs[:, 6:7], in1=rs[:, 12:13], op=ALU.mult), r=[rs], w=[wts])
            k.op("dve", lambda e: e.tensor_tensor(out=wts[:, t, 1:2], in0=wts[:, t, 0:1], in1=rs[:, 10:11], op=ALU.mult), r=[rs, wts], w=[wts])
            k.op("dve", lambda e: e.tensor_tensor(out=selb[:], in0=s1[:], in1=s2[:], op=ALU.add), r=[s1, s2], w=[selb])
            psP = nps()
            k.op("pe", lambda e: e.matmul(psP[:, 0:64], lhsT=stri[:], rhs=selb[:], start=True, stop=True), r=[stri, selb], w=[psP])
            psC = nps()
            k.op("pe", lambda e: e.matmul(psC[:, 0:64], lhsT=ones1[:], rhs=selb[:], start=True, stop=True), r=[ones1, selb], w=[psC])
            k.op("dve", lambda e: e.tensor_tensor(out=pos[:], in0=psP[:, 0:64], in1=run[:], op=ALU.add), r=[psP, run], w=[pos])
            k.op("dve", lambda e: e.tensor_tensor(out=run[:], in0=psC[:, 0:64], in1=run[:], op=ALU.add), r=[psC, run], w=[run])
            k.op("dve", lambda e: e.scalar_tensor_tensor(out=pos[:], in0=pos[:], scalar=float(CAP - 1), in1=ebase[:], op0=ALU.min, op1=ALU.add),
                 r=[pos, ebase], w=[pos])
            for (sx, col) in ((s1, 0), (s2, 1)):
                k.op("dve", lambda e: e.tensor_tensor(out=junk64[:], in0=sx[:], in1=pos[:], op=ALU.mult), r=[sx, pos], w=[junk64])
                k.op("dve", lambda e: e.tensor_reduce(out=slf[:, col:col + 1], in_=junk64[:], axis=AX.X, op=ALU.add), r=[junk64], w=[slf])
            k.op("dve", lambda e: e.tensor_copy(out=slots_i[:, t, :], in_=slf[:]), r=[slf], w=[slots_i])
            for j in range(2):
                k.dma("pool", Xd[:, :], hf[i][:], r=[hf[i], slots_i], w=[Xd],
                      indirect=dict(out_offset=bass.IndirectOffsetOnAxis(ap=slots_i[:, t, j:j + 1], axis=0), in_offset=None))
        if cfg.debug:
            k.dma("sp", slots_o[:], slots_i[:], r=[slots_i], w=[slots_o])
            k.dma("sp", wts_o[:], wts[:], r=[wts], w=[wts_o])
        k.barrier()
        k.pop()
        ph.close()

    def phase5():
        ph = ExitStack()
        k.push(ph)
        NE = cfg.nexp
        NRB = CAP // 128
        wg = [k.sb("wg%d" % i, [128, 16, 512], BF16) for i in range(2)]
        wu = [k.sb("wu%d" % i, [128, 16, 512], BF16) for i in range(2)]
        wd = [k.sb("wd%d" % i, [128, 4, D], BF16) for i in range(2)]
        xr = [k.sb("xr%d" % i, [128, NRB, D], BF16) for i in range(2)]
        XT = k.sb("XT", [128, 16, CAP], BF16)
        sgt = [k.sb("sgt%d" % i, [128, CAP], F32) for i in range(2)]
        aT = k.sb("aT", [128, 4, CAP], BF16)
        yst = [k.sb("yst%d" % i, [128, D], F32) for i in range(2)]

        def loads(e):
            i = e % 2
            k.dma("pool", wg[i][:], w_eg.t[e].rearrange("(c p) f -> p c f", p=128), r=[w_eg], w=[wg[i]])
            k.dma("pool", wu[i][:], w_eu.t[e].rearrange("(c p) f -> p c f", p=128), r=[w_eu], w=[wu[i]])
            k.dma("pool", wd[i][:], w_ed.t[e].rearrange("(c p) n -> p c n", p=128), r=[w_ed], w=[wd[i]])
            k.dma("sp", xr[i][:], Xd.t[e * CAP:(e + 1) * CAP, :].rearrange("(b p) d -> p b d", p=128), r=[Xd], w=[xr[i]])
        loads(0)
        ny = 0
        for e_ in range(NE):
            i = e_ % 2
            if e_ + 1 < NE:
                loads(e_ + 1)
            n = 0
            for rb in range(NRB):
                for hh in range(2):
                    ps = nps()
                    psb = ps.t[:].bitcast(BF16).rearrange("p (c n) -> p c n", n=128)
                    for c in range(8):
                        cc = hh * 8 + c
                        k.op("pe", lambda e: e.transpose(out=psb[:, c, :], in_=xr[i][:, rb, cc * 128:(cc + 1) * 128], identity=identb[:]),
                             r=[xr[i], identb], w=[ps])
                    if n % 2 == 0:
                        k.op("act", lambda e: e.activation(out=XT[:, hh * 8:hh * 8 + 8, rb * 128:(rb + 1) * 128], in_=psb, func=AF.Copy), r=[ps], w=[XT])
                    else:
                        k.op("dve", lambda e: e.tensor_copy(out=XT[:, hh * 8:hh * 8 + 8, rb * 128:(rb + 1) * 128], in_=psb), r=[ps], w=[XT])
                    n += 1
            for fb in range(4):
                psg = nps()
                for c in range(16):
                    k.op("pe", lambda e: e.matmul(psg[:, 0:CAP], lhsT=wg[i][:, c, fb * 128:(fb + 1) * 128], rhs=XT[:, c, :],
                                                  start=(c == 0), stop=(c == 15)), r=[wg[i], XT], w=[psg])
                psu = nps()
                for c in range(16):
                    k.op("pe", lambda e: e.matmul(psu[:, 0:CAP], lhsT=wu[i][:, c, fb * 128:(fb + 1) * 128], rhs=XT[:, c, :],
                                                  start=(c == 0), stop=(c == 15)), r=[wu[i], XT], w=[psu])
                s_ = sgt[fb % 2]
                k.op("act", lambda e: e.activation(out=s_[:], in_=psg[:, 0:CAP], func=AF.Silu), r=[psg], w=[s_])
                k.op("dve", lambda e: e.tensor_tensor(out=aT[:, fb, :], in0=psu[:, 0:CAP], in1=s_[:], op=ALU.mult), r=[psu, s_], w=[aT])
            for rb in range(NRB):
                y_ = yst[ny % 2]
                ny += 1
                for nb in range(4):
                    ps = nps()
                    for fc in range(4):
                        k.op("pe", lambda e: e.matmul(ps[:], lhsT=aT[:, fc, rb * 128:(rb + 1) * 128], rhs=wd[i][:, fc, nb * 512:(nb + 1) * 512],
                                                      start=(fc == 0), stop=(fc == 3)), r=[aT, wd[i]], w=[ps])
                    if nb % 2 == 0:
                        k.op("act", lambda e: e.activation(out=y_[:, nb * 512:(nb + 1) * 512], in_=ps[:], func=AF.Copy), r=[ps], w=[y_])
                    else:
                        k.op("dve", lambda e: e.tensor_copy(out=y_[:, nb * 512:(nb + 1) * 512], in_=ps[:]), r=[ps], w=[y_])
                r0 = e_ * CAP + rb * 128
                k.dma("sp", Yd[r0:r0 + 128, :], y_[:], r=[y_], w=[Yd])
            if e_ % 16 == 15:
                k.barrier()
        k.barrier()
        k.pop()
        ph.close()

    def phase6():
        ph = ExitStack()
        k.push(ph)
        wpg = k.sb("wpg", [128, 16, D], BF16)
        for c in range(16):
            k.dma("pool", wpg[:, c, :], w_pg[c * 128:(c + 1) * 128, :], r=[w_pg], w=[wpg])
        wpp = k.sb("wpp", [128, 2, D], BF16)
        k.dma("pool", wpp[:], w_pp.t.rearrange("(c p) n -> p c n", p=128), r=[w_pp], w=[wpp])
        bpg = k.sb("bpg", [1, D], BF16)
        k.dma("pool", bpg[:], b_pg[:], r=[b_pg], w=[bpg])
        onesrb = k.sb("onesrb6", [1, 128], BF16)
        k.op("dve", lambda e: e.memset(onesrb[:], 1.0), w=[onesrb])
        gple = k.sb("gple", [128, D], F32)
        k.dma("sp", gple[:], gple_row.t.partition_broadcast(128), r=[gple_row], w=[gple])
        x1t = [k.sb("x1t%d" % i, [128, D], F32) for i in range(2)]
        y1t = [k.sb("y1t%d" % i, [128, D], F32) for i in range(2)]
        y2t = [k.sb("y2t%d" % i, [128, D], F32) for i in range(2)]
        pt = [k.sb("pt%d" % i, [128, 256], F32) for i in range(2)]
        ptb = k.sb("ptb", [128, 256], BF16)
        pTt = k.sb("pTt", [128, 2, 128], BF16)
        junk = k.sb("junk6", [128, D], BF16)
        hp = k.sb("hp", [128, D], BF16)
        hpT = k.sb("hpT", [128, 16, 128], BF16)
        rs = k.sb("rs6", [128, 4], F32)
        sgm = [k.sb("sgm%d" % i, [128, 512], F32) for i in range(2)]
        ot = [k.sb("ot%d" % i, [128, D], F32) for i in range(2)]

        def loads(t):
            i = t % 2
            ro = t * 128
            k.dma("sp", x1t[i][:], x1_d[ro:ro + 128, :], r=[x1_d], w=[x1t[i]])
            k.dma("sp", pt[i][:], p_own[ro:ro + 128, :], r=[p_own], w=[pt[i]])
            k.dma("pool", y1t[i][:], Yd[:, :], r=[Yd, slots_i], w=[y1t[i]],
                  indirect=dict(out_offset=None, in_offset=bass.IndirectOffsetOnAxis(ap=slots_i[:, t, 0:1], axis=0)))
            k.dma("pool", y2t[i][:], Yd[:, :], r=[Yd, slots_i], w=[y2t[i]],
                  indirect=dict(out_offset=None, in_offset=bass.IndirectOffsetOnAxis(ap=slots_i[:, t, 1:2], axis=0)))
        loads(0)
        for t in range(NT):
            i = t % 2
            ro = t * 128
            if t + 1 < NT:
                loads(t + 1)
            x2 = x1t[i]
            k.op("dve", lambda e: e.scalar_tensor_tensor(out=x2[:], in0=y1t[i][:], scalar=wts[:, t, 0:1], in1=x2[:], op0=ALU.mult, op1=ALU.add),
                 r=[y1t[i], wts, x2], w=[x2])
            k.op("dve", lambda e: e.scalar_tensor_tensor(out=x2[:], in0=y2t[i][:], scalar=wts[:, t, 1:2], in1=x2[:], op0=ALU.mult, op1=ALU.add),
                 r=[y2t[i], wts, x2], w=[x2])
            k.op("act", lambda e: e.activation(out=junk[:], in_=x2[:], func=AF.Square, accum_out=rs[:, 0:1]), r=[x2], w=[junk, rs])
            k.op("act", lambda e: e.activation(out=rs[:, 1:2], in_=rs[:, 0:1], func=AF.Sqrt, bias=epsc[:, 0:1], scale=1.0 / D), r=[rs, epsc], w=[rs])
            k.op("dve", lambda e: e.reciprocal(out=rs[:, 2:3], in_=rs[:, 1:2]), r=[rs], w=[rs])
            k.op("dve", lambda e: e.scalar_tensor_tensor(out=hp[:], in0=x2[:], scalar=rs[:, 2:3], in1=gple[:], op0=ALU.mult, op1=ALU.mult),
                 r=[x2, rs, gple], w=[hp])
            for hh in range(2):
                ps = nps()
                psb = ps.t[:].bitcast(BF16).rearrange("p (c n) -> p c n", n=128)
                for c in range(8):
                    cc = hh * 8 + c
                    k.op("pe", lambda e: e.transpose(out=psb[:, c, :], in_=hp[:, cc * 128:(cc + 1) * 128], identity=identb[:]),
                         r=[hp, identb], w=[ps])
                if hh == 0:
                    k.op("act", lambda e: e.activation(out=hpT[:, 0:8, :], in_=psb, func=AF.Copy), r=[ps], w=[hpT])
                else:
                    k.op("dve", lambda e: e.tensor_copy(out=hpT[:, 8:16, :], in_=psb), r=[ps], w=[hpT])
            k.op("dve", lambda e: e.tensor_copy(out=ptb[:], in_=pt[i][:]), r=[pt[i]], w=[ptb])
            ps = nps()
            psb = ps.t[:].bitcast(BF16).rearrange("p (c n) -> p c n", n=128)
            for c in range(2):
                k.op("pe", lambda e: e.transpose(out=psb[:, c, :], in_=ptb[:, c * 128:(c + 1) * 128], identity=identb[:]), r=[ptb, identb], w=[ps])
            k.op("dve", lambda e: e.tensor_copy(out=pTt[:], in_=psb[:, 0:2, :]), r=[ps], w=[pTt])
            o_ = ot[i]
            for nb in range(4):
                psg = nps()
                for c in range(16):
                    k.op("pe", lambda e: e.matmul(psg[:], lhsT=hpT[:, c, :], rhs=wpg[:, c, nb * 512:(nb + 1) * 512], start=(c == 0), stop=False),
                         r=[hpT, wpg], w=[psg])
                k.op("pe", lambda e: e.matmul(psg[:], lhsT=onesrb[:], rhs=bpg[:, nb * 512:(nb + 1) * 512], start=False, stop=True),
                     r=[onesrb, bpg], w=[psg])
                psp = nps()
                for c in range(2):
                    k.op("pe", lambda e: e.matmul(psp[:], lhsT=pTt[:, c, :], rhs=wpp[:, c, nb * 512:(nb + 1) * 512], start=(c == 0), stop=(c == 1)),
                         r=[pTt, wpp], w=[psp])
                s_ = sgm[nb % 2]
                k.op("act", lambda e: e.activation(out=s_[:], in_=psg[:], func=AF.Sigmoid), r=[psg], w=[s_])
                k.op("dve", lambda e: e.tensor_tensor(out=s_[:], in0=psp[:], in1=s_[:], op=ALU.mult), r=[psp, s_], w=[s_])
                k.op("pool", lambda e: e.tensor_tensor(out=o_[:, nb * 512:(nb + 1) * 512], in0=s_[:], in1=x2[:, nb * 512:(nb + 1) * 512], op=ALU.add),
                     r=[s_, x2], w=[o_])
            k.dma("sp", out_d[ro:ro + 128, :], o_[:], r=[o_], w=[out_d])
        k.barrier()
        k.pop()
        ph.close()

    phase1()
    if cfg.phases >= 2:
        phase2()
    if cfg.phases >= 3:
        phase3()
    if cfg.phases >= 4:
        phase4a()
        phase4b()
    if cfg.phases >= 5:
        phase5()
        phase6()

    k.barrier()
    return nc, es, k


def _pack_cst(inp, s):
    c = np.zeros((128, C_NCOL), np.float32)
    c[:, C_GMIX:C_GMIX + 16] = inp["norm_mix_g"][0].reshape(16, 128).T
    c[:, C_GFFN:C_GFFN + 16] = inp["norm_ffn_g"][0].reshape(16, 128).T
    c[:, C_GPLE:C_GPLE + 16] = inp["norm_ple_g"][0].reshape(16, 128).T
    c[:, C_BBG:C_BBG + 32] = inp["b_branch_gate"][0].reshape(32, 128).T
    c[:, C_GGLA] = inp["gla_norm_g"][0]
    c[:, C_GQ] = inp["q_norm_g"][0]
    c[:, C_GK] = inp["k_norm_g"][0]
    c[:, C_PREV] = 1.0 if s == 1 else 0.0
    c[:, C_PREB] = 0.0 if s == 1 else -1e30
    return c


def _t5_bucket(rel):
    half, exact = 16, 8
    sign = np.where(rel > 0, half, 0)
    n = np.abs(rel)
    nf = np.maximum(n, 1).astype(np.float32)
    large = exact + (np.log(nf / exact) / np.log(128 / exact) * (half - exact)).astype(np.int32)
    large = np.minimum(large, half - 1)
    return sign + np.where(n < exact, n, large)


def _bias_tables(rel_bias):
    s_ = np.arange(128)[:, None]
    q_ = np.arange(128)[None, :]
    tb = np.zeros((128, 8, 2, 128), np.float32)
    for a in range(2):
        bk = _t5_bucket((s_ - q_ - 128 * a).astype(np.int32))
        tb[:, :, a, :] = np.transpose(rel_bias[bk], (0, 2, 1))
    cb = np.ascontiguousarray(np.broadcast_to(rel_bias[15][None, :], (128, 8))).astype(np.float32)
    return tb, cb


def make_in_maps(inp, cfg):
    NT = cfg.NT
    TO = NT * 128
    x = inp["x"]
    p = inp["p"][0]
    ident = np.eye(128, dtype=np.float32)
    tb, cb = _bias_tables(inp["rel_bias"])
    w_r = np.ascontiguousarray(np.concatenate([inp["w_group_router"][0], inp["w_expert_router"][0]], axis=1))
    b_r = np.concatenate([inp["b_group_router"][0], inp["b_expert_router"][0]]).reshape(1, 72).astype(np.float32)
    ebase = np.ascontiguousarray(np.broadcast_to((np.arange(64, dtype=np.float32) * cfg.cap)[None, :], (128, 64)))
    maps = []
    for c in range(8):
        b, s = c // 2, c % 2
        m = {
            "x_pre": np.ascontiguousarray(x[b, 0:TO]),
            "x_own": np.ascontiguousarray(x[b, s * TO:(s + 1) * TO]),
            "p_own": np.ascontiguousarray(p[b, s * TO:(s + 1) * TO]),
            "cst": _pack_cst(inp, s),
            "ident": ident,
            "w_in": inp["w_in"][0],
            "w_bg": inp["w_branch_gate"][0],
            "w_alpha": inp["gla_w_alpha"][0],
            "b_alpha": inp["gla_b_alpha"][0].reshape(1, 512),
            "tri": np.triu(np.ones((128, 128), np.float32)),
            "tb": tb,
            "cb": cb,
            "w_oa": inp["w_out_gla"][0],
            "w_ob": inp["w_out_dsa"][0],
            "w_o": inp["w_out"][0],
            "w_r": w_r,
            "b_r": b_r,
            "gffn_row": inp["norm_ffn_g"][0].reshape(1, D),
            "ebase": ebase,
            "w_eg": inp["w_exp_gate"][0] if cfg.phases >= 5 else None,
            "w_eu": inp["w_exp_up"][0] if cfg.phases >= 5 else None,
            "w_ed": inp["w_exp_down"][0] if cfg.phases >= 5 else None,
            "w_pg": inp["w_ple_gate"][0],
            "w_pp": inp["w_ple_proj"][0],
            "b_pg": inp["b_ple_gate"][0].reshape(1, D),
            "gple_row": inp["norm_ple_g"][0].reshape(1, D),
        }
        maps.append({kk: vv for kk, vv in m.items() if vv is not None})
    return maps


def run(inp, cfg, trace=False):
    nc, es, k = build(cfg)
    maps = make_in_maps(inp, cfg)
    res = run_bass_kernel_spmd(nc, maps, core_ids=list(range(8)), trace=trace)
    es.close()
    return res


def kernel(**inputs):
    cfg = CFG(NT=32, G=8)
    inp = {k_: np.asarray(v) for k_, v in inputs.items()}
    res = run(inp, cfg)
    B, S = inp["x"].shape[:2]
    out = np.zeros((B, S, D), np.float32)
    TO = cfg.NT * 128
    for c in range(8):
        b, s = c // 2, c % 2
        out[b, s * TO:(s + 1) * TO] = res.results[c]["out"]
    return out
```

```python
import numpy as np
from contextlib import ExitStack
import concourse.bass as bass
import concourse.mybir as mybir
from concourse.bass_utils import run_bass_kernel_spmd

F32 = mybir.dt.float32
BF16 = mybir.dt.bfloat16
I32 = mybir.dt.int32
U32 = mybir.dt.uint32
AF = mybir.ActivationFunctionType
ALU = mybir.AluOpType
AX = mybir.AxisListType

D = 2048
EPS = 1e-6


class Buf:
    def __init__(self, name):
        self.name = name
        self.w = {}
        self.r = {}
        self.sem = None


class T:
    def __init__(self, t, b):
        self.t = t
        self.b = b

    def __getitem__(self, key):
        return self.t[key]


class KB:
    def __init__(self, nc, es):
        self.nc = nc
        self.es = es
        self.eng = {"pe": nc.tensor, "act": nc.scalar, "dve": nc.vector, "pool": nc.gpsimd, "sp": nc.sync}
        self.esem = {}
        for k in self.eng:
            self.esem[k] = es.enter_context(nc.semaphore("sem_" + k))
        self.ecnt = {k: 0 for k in self.eng}
        self.gen = {k: 0 for k in self.eng}
        self.rebase_at = 20000
        self.ptiles = [[]]
        self.waited = {}
        self.dsems = []
        self.free_dsems = []
        self.stack = [es]
        self.nops = 0
        self.nwaits = 0

    def push(self, es):
        self.stack.append(es)
        self.ptiles.append([])

    def pop(self):
        self.stack.pop()
        tl = self.ptiles.pop()
        for t in tl:
            if t.b.sem is not None:
                if t.b.sem[1] < 20000:
                    self.free_dsems.append(t.b.sem)
                t.b.sem = None

    def sb(self, name, shape, dtype):
        t = self.stack[-1].enter_context(self.nc.sbuf_tensor("sb_" + name, list(shape), dtype))
        tt = T(t, Buf(name))
        self.ptiles[-1].append(tt)
        return tt

    def dram(self, name, shape, dtype, kind="Internal"):
        t = self.nc.dram_tensor(name, list(shape), dtype, kind=kind).ap()
        return T(t, Buf(name))

    def _dsem(self, b):
        if b.sem is None:
            if self.free_dsems:
                b.sem = self.free_dsems.pop()
            else:
                s = self.es.enter_context(self.nc.semaphore("dsem%d" % len(self.dsems)))
                b.sem = [s, 0]
                self.dsems.append(b.sem)
        return b.sem

    def release(self, tiles):
        for t in tiles:
            if t.b.sem is not None:
                self.free_dsems.append(t.b.sem)
                t.b.sem = None

    def _wait(self, e, deps):
        for key, (sem, val) in deps.items():
            if e == "pe" and isinstance(key, tuple) and key[0] == "pe":
                continue
            wk = (e, key)
            if self.waited.get(wk, 0) >= val:
                continue
            self.waited[wk] = val
            self.eng[e].wait_ge(sem, val)
            self.nwaits += 1

    @staticmethod
    def _merge(d, key, sem, val):
        if key not in d or d[key][1] < val:
            d[key] = (sem, val)

    def _deps(self, r, w):
        deps = {}
        for t in r:
            for key, (sem, val) in t.b.w.items():
                self._merge(deps, key, sem, val)
        for t in w:
            for key, (sem, val) in t.b.w.items():
                self._merge(deps, key, sem, val)
            for key, (sem, val) in t.b.r.items():
                self._merge(deps, key, sem, val)
        return deps

    def op(self, e, fn, r=(), w=()):
        self._wait(e, self._deps(r, w))
        if self.ecnt[e] >= self.rebase_at:
            self.gen[e] += 1
            self.esem[e] = self.es.enter_context(self.nc.semaphore("sem_%s_g%d" % (e, self.gen[e])))
            self.ecnt[e] = 0
        inst = fn(self.eng[e])
        self.ecnt[e] += 1
        inst.then_inc(self.esem[e], 1)
        self.nops += 1
        key = (e, self.gen[e])
        for t in r:
            self._merge(t.b.r, key, self.esem[e], self.ecnt[e])
        for t in w:
            self._merge(t.b.w, key, self.esem[e], self.ecnt[e])
        return inst

    def dma(self, q, out, in_, r=(), w=(), indirect=None, **kw):
        self._wait(q, self._deps(r, w))
        ds = self._dsem(w[0].b)
        if indirect is not None:
            inst = self.eng[q].indirect_dma_start(out=out, in_=in_, **indirect)
        else:
            inst = self.eng[q].dma_start(out=out, in_=in_, **kw)
        ds[1] += 16
        inst.then_inc(ds[0], 16)
        self.nops += 1
        key = id(ds)
        for t in r:
            self._merge(t.b.r, key, ds[0], ds[1])
        for t in w:
            self._merge(t.b.w, key, ds[0], ds[1])
        return inst

    def barrier(self, engines=("pe", "act", "dve", "pool", "sp")):
        deps = {}
        for k in self.eng:
            if self.ecnt[k] > 0:
                deps[(k, self.gen[k])] = (self.esem[k], self.ecnt[k])
        for ds in self.dsems:
            if ds[1] > 0:
                deps[id(ds)] = (ds[0], ds[1])
        for e in engines:
            d2 = {kk: v for kk, v in deps.items() if not (isinstance(kk, tuple) and kk[0] == e)}
            self._wait(e, d2)
        for k in self.eng:
            if self.ecnt[k] > self.rebase_at:
                self.gen[k] += 1
                self.esem[k] = self.es.enter_context(self.nc.semaphore("sem_%s_g%d" % (k, self.gen[k])))
                self.ecnt[k] = 0


class CFG:
    def __init__(self, NT=32, G=8, debug=False, phases=99):
        self.NT = NT
        self.G = G
        self.debug = debug
        self.phases = phases
        self.nit = 22
        self.cap = 256
        self.nexp = 64
        self.rebase_at = 20000


O_GQ, O_GK, O_GV, O_GR, O_GLR, O_DQ, O_DK, O_DV, O_IQ, O_IK, O_IW = (
    0, 512, 1024, 2048, 3072, 3088, 4112, 5136, 6160, 7184, 7248)
IN_COLS = 7264

C_GMIX, C_GFFN, C_GPLE, C_BBG, C_GGLA, C_GQ, C_GK, C_PREV, C_PREB, C_NCOL = 0, 16, 32, 48, 80, 81, 82, 83, 84, 96


def build(cfg):
    NT, G = cfg.NT, cfg.G
    TO = NT * 128
    TV = 2 * TO
    GT = G * 128
    NG = NT // G
    NH = GT // 512
    nc = bass.Bass("TRN2", target_bir_lowering=False)
    es = ExitStack()
    k = KB(nc, es)
    k.rebase_at = cfg.rebase_at
    okind = "ExternalOutput" if cfg.debug else "Internal"

    def din(name, shape, dt=F32):
        return k.dram(name, shape, dt, kind="ExternalInput")

    x_pre = din("x_pre", [TO, D])
    x_own = din("x_own", [TO, D])
    p_own = din("p_own", [TO, 256])
    cstd = din("cst", [128, C_NCOL])
    identd = din("ident", [128, 128])
    w_in = din("w_in", [D, IN_COLS])
    w_bg = din("w_bg", [D, 2 * D])
    w_alpha = din("w_alpha", [16, 512])
    b_alpha = din("b_alpha", [1, 512])
    trid = din("tri", [128, 128])
    out_d = k.dram("out", [TO, D], F32, kind="ExternalOutput")
    oaT_d = k.dram("oaT_d", [1024, TO], BF16, okind)
    tbd = din("tb", [128, 8, 2, 128])
    cbd = din("cb", [128, 8])
    obT_d = k.dram("obT_d", [1024, TO], BF16, okind)
    w_oa = din("w_oa", [1024, D])
    w_ob = din("w_ob", [1024, D])
    w_o = din("w_o", [D, D])
    w_r = din("w_r", [D, 72])
    b_r = din("b_r", [1, 72])
    gffn_row = din("gffn_row", [1, D])
    ebased = din("ebase", [128, 64])
    CAP = cfg.cap
    mT_d = k.dram("mT_d", [D, TO], BF16, okind)
    x1_d = k.dram("x1_d", [TO, D], F32, okind)
    Xd = k.dram("Xd", [64 * CAP, D], BF16, "Internal")
    slots_o = k.dram("slots_o", [128, NT, 2], I32, okind)
    wts_o = k.dram("wts_o", [128, NT, 2], F32, okind)
    if cfg.phases >= 5:
        w_eg = din("w_eg", [64, D, 512])
        w_eu = din("w_eu", [64, D, 512])
        w_ed = din("w_ed", [64, 512, D])
    w_pg = din("w_pg", [D, D])
    w_pp = din("w_pp", [256, D])
    b_pg = din("b_pg", [1, D])
    gple_row = din("gple_row", [1, D])
    Yd = k.dram("Yd", [64 * CAP, D], F32, "Internal")
    slots_i = k.sb("slots_i", [128, NT, 2], I32)
    wts = k.sb("wts", [128, NT, 2], F32)

    grT_d = k.dram("grT_d", [1024, TO], BF16, okind)
    qT_d = k.dram("qT_d", [8, 128, TO], BF16, okind)
    KT_d = k.dram("KT_d", [8, 128, TV], BF16, okind)
    iqT_d = k.dram("iqT_d", [1024, TO], BF16, okind)
    gateT_d = k.dram("gateT_d", [4096, TO], BF16, okind)
    ikT_d = k.dram("ikT_d", [64, TV], BF16, okind)
    glrT_d = k.dram("glrT_d", [16, TV], F32, okind)
    q_d = k.dram("q_d", [TO, 512], BF16, okind)
    k_d = k.dram("k_d", [TV, 512], BF16, okind)
    v_d = k.dram("v_d", [TV, 1024], BF16, okind)
    iw_d = k.dram("iw_d", [TO, 16], F32, okind)
    Vd = k.dram("Vd", [8, 128, 2 * NT, 129], BF16, okind)

    cst = k.sb("cst", [128, C_NCOL], F32)
    identf = k.sb("identf", [128, 128], F32)
    identb = k.sb("identb", [128, 128], BF16)
    onesb = k.sb("onesb", [128, 128], BF16)
    epsc = k.sb("epsc", [128, 1], F32)
    k.dma("sp", cst[:], cstd[:], w=[cst])
    k.dma("sp", identf[:], identd[:], w=[identf])
    k.op("dve", lambda e: e.tensor_copy(out=identb[:], in_=identf[:]), r=[identf], w=[identb])
    k.op("dve", lambda e: e.memset(onesb[:], 1.0 / 128.0), w=[onesb])
    k.op("dve", lambda e: e.memset(epsc[:], EPS), w=[epsc])

    psum = []
    for i in range(8):
        t = es.enter_context(nc.psum_tensor("ps%d" % i, [128, 512], F32))
        psum.append(T(t, Buf("ps%d" % i)))
    psi = [0]

    def nps():
        p = psum[psi[0] % 8]
        psi[0] += 1
        return p

    def phase1():
        ph = ExitStack()
        k.push(ph)
        hT = k.sb("hT", [128, 16, GT], BF16)
        wt = [k.sb("wt%d" % i, [128, 16, 512], BF16) for i in range(2)]
        wsm = k.sb("wsm", [128, 16, 96], BF16)
        xt = [k.sb("xt%d" % i, [128, D], F32) for i in range(2)]
        xn = [k.sb("xn%d" % i, [128, D], BF16) for i in range(2)]
        junk = k.sb("junk", [128, D], BF16)
        ss = [k.sb("ss%d" % i, [128, 4], F32) for i in range(2)]
        stg = [k.sb("stg%d" % i, [128, 512], BF16) for i in range(4)]
        stgf = [k.sb("stgf%d" % i, [128, 512], F32) for i in range(2)]
        sq = [k.sb("sq%d" % i, [128, 512], BF16) for i in range(2)]
        rr = [k.sb("rr%d" % i, [128, 512], F32) for i in range(2)]
        cnt = {"stg": 0, "stgf": 0, "sq": 0, "wt": 0, "x": 0, "stv": 0}
        stv = [k.sb("stv%d" % i, [128, 4, 129], BF16) for i in range(2)]
        for i_ in range(2):
            k.op("dve", lambda e: e.memset(stv[i_][:], 1.0), w=[stv[i_]])

        w_in_v = w_in.t.rearrange("(c p) n -> p c n", p=128)
        w_bg_v = w_bg.t.rearrange("(c p) n -> p c n", p=128)
        k.dma("pool", wsm[:, :, 0:64], w_in_v[:, :, O_IK:O_IK + 64], r=[w_in], w=[wsm])
        k.dma("pool", wsm[:, :, 64:80], w_in_v[:, :, O_GLR:O_GLR + 16], r=[w_in], w=[wsm])
        k.dma("pool", wsm[:, :, 80:96], w_in_v[:, :, O_IW:O_IW + 16], r=[w_in], w=[wsm])

        def load_w(src_v, col0):
            t = wt[cnt["wt"] % 2]
            cnt["wt"] += 1
            k.dma("pool", t[:], src_v[:, :, col0:col0 + 512], w=[t])
            return t

        def stage():
            t = stg[cnt["stg"] % 4]
            cnt["stg"] += 1
            return t

        def fm_mm(ps, wtile, c0, ncol, hf):
            for c in range(16):
                k.op("pe", lambda e: e.matmul(ps[0:ncol, :], lhsT=wtile[:, c, c0:c0 + ncol],
                                              rhs=hT[:, c, hf * 512:(hf + 1) * 512],
                                              start=(c == 0), stop=(c == 15)),
                     r=[wtile, hT], w=[ps])

        def tm_mm(ps, wtile, c0, ncol, j):
            for c in range(16):
                k.op("pe", lambda e: e.matmul(ps[:, 0:ncol], lhsT=hT[:, c, j * 128:(j + 1) * 128],
                                              rhs=wtile[:, c, c0:c0 + ncol],
                                              start=(c == 0), stop=(c == 15)),
                     r=[wtile, hT], w=[ps])

        def headnorm(ps, gcol, scale, dst_ap, dst_t):
            s = sq[cnt["sq"] % 2]
            r_ = rr[cnt["sq"] % 2]
            cnt["sq"] += 1
            k.op("act", lambda e: e.activation(out=s[:], in_=ps[:], func=AF.Square), r=[ps], w=[s])
            ps2 = nps()
            k.op("pe", lambda e: e.matmul(ps2[:], lhsT=onesb[:], rhs=s[:], start=True, stop=True),
                 r=[onesb, s], w=[ps2])
            k.op("act", lambda e: e.activation(out=r_[:], in_=ps2[:], func=AF.Sqrt, bias=epsc[:, 0:1], scale=1.0),
                 r=[ps2, epsc], w=[r_])
            k.op("dve", lambda e: e.reciprocal(out=r_[:], in_=r_[:]), r=[r_], w=[r_])
            st = stage()
            if scale != 1.0:
                k.op("dve", lambda e: e.tensor_scalar(out=r_[:], in0=r_[:], scalar1=cst[:, gcol:gcol + 1], scalar2=float(scale),
                                                      op0=ALU.mult, op1=ALU.mult), r=[r_, cst], w=[r_])
            else:
                k.op("dve", lambda e: e.tensor_scalar(out=r_[:], in0=r_[:], scalar1=cst[:, gcol:gcol + 1], scalar2=None,
                                                      op0=ALU.mult), r=[r_, cst], w=[r_])
            k.op("dve", lambda e: e.tensor_tensor(out=st[:], in0=ps[:], in1=r_[:], op=ALU.mult), r=[ps, r_], w=[st])
            k.dma("sp", dst_ap, st[:], r=[st], w=[dst_t])

        for half in range(2):
            own = half == 1
            xsrc = x_own if own else x_pre
            for g in range(NG):
                tok_o = g * GT
                tok_v = half * TO + tok_o
                for j in range(G):
                    i = cnt["x"] % 2
                    cnt["x"] += 1
                    r0 = tok_o + j * 128
                    k.dma("sp", xt[i][:], xsrc[r0:r0 + 128, :], r=[xsrc], w=[xt[i]])
                    k.op("act", lambda e: e.activation(out=junk[:], in_=xt[i][:], func=AF.Square, accum_out=ss[i][:, 0:1]),
                         r=[xt[i]], w=[junk, ss[i]])
                    k.op("act", lambda e: e.activation(out=ss[i][:, 1:2], in_=ss[i][:, 0:1], func=AF.Sqrt, bias=epsc[:, 0:1],
                                                       scale=1.0 / D), r=[ss[i], epsc], w=[ss[i]])
                    k.op("dve", lambda e: e.reciprocal(out=ss[i][:, 2:3], in_=ss[i][:, 1:2]), r=[ss[i]], w=[ss[i]])
                    k.op("dve", lambda e: e.tensor_scalar(out=xn[i][:], in0=xt[i][:], scalar1=ss[i][:, 2:3], scalar2=None,
                                                          op0=ALU.mult), r=[xt[i], ss[i]], w=[xn[i]])
                    for hh in range(2):
                        ps = nps()
                        psb = ps.t[:].bitcast(BF16).rearrange("p (c n) -> p c n", n=128)
                        for c in range(8):
                            cc = hh * 8 + c
                            k.op("pe", lambda e: e.transpose(out=psb[:, c, :], in_=xn[i][:, cc * 128:(cc + 1) * 128],
                                                             identity=identb[:]), r=[xn[i], identb], w=[ps])
                        gb = cst[:, C_GMIX + hh * 8:C_GMIX + hh * 8 + 8].unsqueeze(2).to_broadcast([128, 8, 128])
                        k.op("dve", lambda e: e.tensor_tensor(out=hT[:, hh * 8:hh * 8 + 8, j * 128:(j + 1) * 128], in0=psb,
                                                              in1=gb, op=ALU.mult), r=[ps, cst], w=[hT])

                for hf in range(NH):
                    ps = nps()
                    fm_mm(ps, wsm, 0, 80, hf)
                    st = stage()
                    k.op("act", lambda e: e.activation(out=st[0:64, :], in_=ps[0:64, :], func=AF.Copy), r=[ps], w=[st])
                    c0 = tok_v + hf * 512
                    k.dma("sp", ikT_d[:, c0:c0 + 512], st[0:64, :], r=[st], w=[ikT_d])
                    sf = stgf[cnt["stgf"] % 2]
                    cnt["stgf"] += 1
                    k.op("dve", lambda e: e.tensor_copy(out=sf[64:80, :], in_=ps[64:80, :]), r=[ps], w=[sf])
                    k.dma("sp", glrT_d[:, c0:c0 + 512], sf[64:80, :], r=[sf], w=[glrT_d])
                if own:
                    for j in range(G):
                        ps = nps()
                        tm_mm(ps, wsm, 80, 16, j)
                        sf = stgf[cnt["stgf"] % 2]
                        cnt["stgf"] += 1
                        k.op("dve", lambda e: e.tensor_copy(out=sf[:, 0:16], in_=ps[:, 0:16]), r=[ps], w=[sf])
                        r0 = tok_o + j * 128
                        k.dma("sp", iw_d[r0:r0 + 128, :], sf[:, 0:16], r=[sf], w=[iw_d])

                blocks = []
                if own:
                    blocks.append(("tm", w_in_v, O_GQ, ("q", 0)))
                blocks.append(("tm", w_in_v, O_GK, ("k", 0)))
                blocks.append(("tm", w_in_v, O_GV, ("v", 0)))
                blocks.append(("tm", w_in_v, O_GV + 512, ("v", 512)))
                if own:
                    blocks.append(("fm", w_in_v, O_GR, ("gr", 0)))
                    blocks.append(("fm", w_in_v, O_GR + 512, ("gr", 4)))
                    blocks.append(("fm", w_in_v, O_DQ, ("dq", 0)))
                    blocks.append(("fm", w_in_v, O_DQ + 512, ("dq", 4)))
                blocks.append(("fm", w_in_v, O_DK, ("dk", 0)))
                blocks.append(("fm", w_in_v, O_DK + 512, ("dk", 4)))
                blocks.append(("tm", w_in_v, O_DV, ("dv", 0)))
                blocks.append(("tm", w_in_v, O_DV + 512, ("dv", 4)))
                if own:
                    blocks.append(("fm", w_in_v, O_IQ, ("iq", 0)))
                    blocks.append(("fm", w_in_v, O_IQ + 512, ("iq", 4)))
                    for bgi in range(8):
                        blocks.append(("fm", w_bg_v, bgi * 512, ("bg", bgi * 4)))

                nxt = load_w(blocks[0][1], blocks[0][2])
                for bi, (kind, src, col0, (nm, off)) in enumerate(blocks):
                    wtile = nxt
                    if bi + 1 < len(blocks):
                        nxt = load_w(blocks[bi + 1][1], blocks[bi + 1][2])
                    if kind == "tm":
                        for j in range(G):
                            ps = nps()
                            tm_mm(ps, wtile, 0, 512, j)
                            if nm == "dv":
                                sv = stv[cnt["stv"] % 2]
                                cnt["stv"] += 1
                                k.op("dve", lambda e: e.tensor_copy(out=sv[:, :, 0:128], in_=ps[:].rearrange("p (h d) -> p h d", d=128)),
                                     r=[ps], w=[sv])
                                vt = half * NT + g * G + j
                                dst = Vd[off:off + 4, :, vt, :].rearrange("h p d -> p h d")
                                k.dma("sp", dst, sv[:], r=[sv], w=[Vd])
                                continue
                            st = stage()
                            if (j % 2) == 0:
                                k.op("act", lambda e: e.activation(out=st[:], in_=ps[:], func=AF.Copy), r=[ps], w=[st])
                            else:
                                k.op("dve", lambda e: e.tensor_copy(out=st[:], in_=ps[:]), r=[ps], w=[st])
                            ro = tok_o + j * 128
                            rv = tok_v + j * 128
                            if nm == "q":
                                k.dma("sp", q_d[ro:ro + 128, :], st[:], r=[st], w=[q_d])
                            elif nm == "k":
                                k.dma("sp", k_d[rv:rv + 128, :], st[:], r=[st], w=[k_d])
                            elif nm == "v":
                                k.dma("sp", v_d[rv:rv + 128, off:off + 512], st[:], r=[st], w=[v_d])
                    else:
                        for sbi in range(4):
                            for hf in range(NH):
                                ps = nps()
                                fm_mm(ps, wtile, sbi * 128, 128, hf)
                                blk = off + sbi
                                co = tok_o + hf * 512
                                cv = tok_v + hf * 512
                                if nm == "gr":
                                    st = stage()
                                    k.op("act", lambda e: e.activation(out=st[:], in_=ps[:], func=AF.Silu), r=[ps], w=[st])
                                    k.dma("sp", grT_d[blk * 128:(blk + 1) * 128, co:co + 512], st[:], r=[st], w=[grT_d])
                                elif nm == "iq":
                                    st = stage()
                                    k.op("dve", lambda e: e.tensor_copy(out=st[:], in_=ps[:]), r=[ps], w=[st])
                                    k.dma("sp", iqT_d[blk * 128:(blk + 1) * 128, co:co + 512], st[:], r=[st], w=[iqT_d])
                                elif nm == "bg":
                                    st = stage()
                                    k.op("act", lambda e: e.activation(out=st[:], in_=ps[:], func=AF.Sigmoid,
                                                                       bias=cst[:, C_BBG + blk:C_BBG + blk + 1], scale=1.0),
                                         r=[ps, cst], w=[st])
                                    k.dma("sp", gateT_d[blk * 128:(blk + 1) * 128, co:co + 512], st[:], r=[st], w=[gateT_d])
                                elif nm == "dq":
                                    headnorm(ps, C_GQ, 128.0 ** -0.5, qT_d[blk, :, co:co + 512], qT_d)
                                elif nm == "dk":
                                    headnorm(ps, C_GK, 1.0, KT_d[blk, :, cv:cv + 512], KT_d)
                k.barrier()
        k.barrier()
        k.pop()
        ph.close()

    def phase2():
        ph = ExitStack()
        k.push(ph)
        wal = k.sb("wal", [16, 512], F32)
        bal = k.sb("bal", [1, 512], F32)
        onesrow = k.sb("onesrow", [1, 128], F32)
        onescol = k.sb("onescol", [128, 1], F32)
        onec = k.sb("onec", [128, 1], F32)
        trif = k.sb("trif", [128, 128], F32)
        trib = k.sb("trib", [128, 4, 128], BF16)
        S = k.sb("S", [64, 8, 128], F32)
        Sb = k.sb("Sb", [64, 8, 128], BF16)
        k.dma("sp", wal[:], w_alpha[:], w=[wal])
        k.dma("sp", bal[:], b_alpha[:], w=[bal])
        k.dma("sp", trif[:], trid[:], w=[trif])
        k.op("dve", lambda e: e.memset(onesrow[:], 1.0), w=[onesrow])
        k.op("dve", lambda e: e.memset(onescol[:], 1.0), w=[onescol])
        k.op("dve", lambda e: e.memset(onec[:], 1.0), w=[onec])
        k.op("dve", lambda e: e.memset(S[:], 0.0), w=[S])
        k.op("dve", lambda e: e.memset(Sb[:], 0.0), w=[Sb])
        for u in range(4):
            k.op("dve", lambda e: e.tensor_copy(out=trib[:, u, :], in_=trif[:]), r=[trif], w=[trib])
        NB = 2
        kt_ = [k.sb("kt%d" % i, [128, 512], BF16) for i in range(NB)]
        vt_ = [k.sb("vt%d" % i, [128, 1024], BF16) for i in range(NB)]
        qt_ = [k.sb("qt%d" % i, [128, 512], BF16) for i in range(NB)]
        gl_ = [k.sb("gl%d" % i, [16, 128], F32) for i in range(NB)]
        gr_ = [k.sb("gr%d" % i, [128, 8, 128], BF16) for i in range(NB)]
        e1 = k.sb("e1", [128, 512], F32)
        spt = k.sb("spt", [128, 512], F32)
        Ep = k.sb("Ep", [128, 512], F32)
        Em = k.sb("Em", [128, 512], F32)
        ktl = k.sb("ktl", [128, 512], BF16)
        qtl = k.sb("qtl", [128, 512], BF16)
        kTt = k.sb("kTt", [64, 8, 128], BF16)
        qTt = k.sb("qTt", [64, 8, 128], BF16)
        egl = k.sb("egl", [64, 8], F32)
        ATs = k.sb("ATs", [128, 8, 128], BF16)
        sq2 = k.sb("sq2", [128, 512], BF16)
        rr2 = k.sb("rr2", [128, 512], F32)
        ost = [k.sb("ost%d" % i, [128, 4, 128], BF16) for i in range(2)]
        grv = grT_d.t.rearrange("(h p) t -> p h t", p=128)
        oav = oaT_d.t.rearrange("(h p) t -> p h t", p=128)

        def loads(vt):
            i = vt % NB
            own = vt >= NT
            r0 = vt * 128
            k.dma("sp", kt_[i][:], k_d[r0:r0 + 128, :], r=[k_d], w=[kt_[i]])
            k.dma("sp", vt_[i][:], v_d[r0:r0 + 128, :], r=[v_d], w=[vt_[i]])
            k.dma("sp", gl_[i][:], glrT_d[:, r0:r0 + 128], r=[glrT_d], w=[gl_[i]])
            if own:
                ro = r0 - TO
                k.dma("sp", qt_[i][:], q_d[ro:ro + 128, :], r=[q_d], w=[qt_[i]])
                k.dma("sp", gr_[i][:], grv[:, :, ro:ro + 128], r=[grT_d], w=[gr_[i]])

        loads(0)
        for vt in range(2 * NT):
            i = vt % NB
            own = vt >= NT
            ro = vt * 128 - TO
            if vt + 1 < 2 * NT:
                loads(vt + 1)
            if vt == NT:
                k.op("dve", lambda e: e.tensor_scalar(out=S[:], in0=S[:], scalar1=cst[0:64, C_PREV:C_PREV + 1], scalar2=None,
                                                      op0=ALU.mult), r=[S, cst], w=[S])
                k.op("dve", lambda e: e.tensor_copy(out=Sb[:], in_=S[:]), r=[S], w=[Sb])
            ps = nps()
            k.op("pe", lambda e: e.matmul(ps[:], lhsT=gl_[i][:], rhs=wal[:], start=True, stop=False), r=[gl_[i], wal], w=[ps])
            k.op("pe", lambda e: e.matmul(ps[:], lhsT=onesrow[:], rhs=bal[:], start=False, stop=True), r=[onesrow, bal], w=[ps])
            k.op("act", lambda e: e.activation(out=e1[:], in_=ps[:], func=AF.Exp, scale=-1.0), r=[ps], w=[e1])
            k.op("act", lambda e: e.activation(out=spt[:], in_=e1[:], func=AF.Ln, bias=onec[:, 0:1], scale=1.0), r=[e1, onec], w=[spt])
            psG = nps()
            k.op("pe", lambda e: e.matmul(psG[:], lhsT=trif[:], rhs=spt[:], start=True, stop=True), r=[trif, spt], w=[psG])
            k.op("act", lambda e: e.activation(out=Em[:], in_=psG[:], func=AF.Exp, scale=1.0 / 16.0), r=[psG], w=[Em])
            if own:
                k.op("act", lambda e: e.activation(out=Ep[:], in_=psG[:], func=AF.Exp, scale=-1.0 / 16.0), r=[psG], w=[Ep])
            psE = nps()
            for h in range(8):
                k.op("pe", lambda e: e.matmul(psE[0:64, h:h + 1], lhsT=spt[:, h * 64:(h + 1) * 64], rhs=onescol[:], start=True, stop=True),
                     r=[spt, onescol], w=[psE])
            k.op("act", lambda e: e.activation(out=egl[:], in_=psE[0:64, 0:8], func=AF.Exp, scale=-1.0 / 16.0), r=[psE], w=[egl])
            k.op("dve", lambda e: e.tensor_tensor(out=ktl[:], in0=kt_[i][:], in1=Em[:], op=ALU.mult), r=[kt_[i], Em], w=[ktl])
            if own:
                k.op("dve", lambda e: e.scalar_tensor_tensor(out=qtl[:], in0=qt_[i][:], scalar=0.125, in1=Ep[:], op0=ALU.mult, op1=ALU.mult),
                     r=[qt_[i], Ep], w=[qtl])
                for (src, dst) in ((ktl, kTt), (qtl, qTt)):
                    pst = nps()
                    pstb = pst.t[:].bitcast(BF16).rearrange("p (c n) -> p c n", n=128)
                    for h in range(8):
                        k.op("pe", lambda e: e.transpose(out=pstb[0:64, h, :], in_=src[:, h * 64:(h + 1) * 64], identity=identb[:]),
                             r=[src, identb], w=[pst])
                    k.op("dve", lambda e: e.tensor_copy(out=dst[:], in_=pstb[0:64, :, :]), r=[pst], w=[dst])
                for hg in range(2):
                    psA = nps()
                    for u in range(4):
                        h = hg * 4 + u
                        k.op("pe", lambda e: e.matmul(psA[:, u * 128:(u + 1) * 128], lhsT=kTt[:, h, :], rhs=qTt[:, h, :], start=True, stop=True),
                             r=[kTt, qTt], w=[psA])
                    k.op("dve", lambda e: e.tensor_tensor(out=ATs[:, hg * 4:hg * 4 + 4, :], in0=psA[:].rearrange("p (u n) -> p u n", n=128),
                                                          in1=trib[:], op=ALU.mult), r=[psA, trib], w=[ATs])
                for hg in range(2):
                    psO = nps()
                    for u in range(4):
                        h = hg * 4 + u
                        k.op("pe", lambda e: e.matmul(psO[:, u * 128:(u + 1) * 128], lhsT=vt_[i][:, h * 128:(h + 1) * 128], rhs=ATs[:, h, :],
                                                      start=True, stop=False), r=[vt_[i], ATs], w=[psO])
                        k.op("pe", lambda e: e.matmul(psO[:, u * 128:(u + 1) * 128], lhsT=Sb[:, h, :], rhs=qTt[:, h, :],
                                                      start=False, stop=True), r=[Sb, qTt], w=[psO])
                    k.op("act", lambda e: e.activation(out=sq2[:], in_=psO[:], func=AF.Square), r=[psO], w=[sq2])
                    psM = nps()
                    k.op("pe", lambda e: e.matmul(psM[:], lhsT=onesb[:], rhs=sq2[:], start=True, stop=True), r=[onesb, sq2], w=[psM])
                    k.op("act", lambda e: e.activation(out=rr2[:], in_=psM[:], func=AF.Sqrt, bias=epsc[:, 0:1], scale=1.0), r=[psM, epsc], w=[rr2])
                    k.op("dve", lambda e: e.reciprocal(out=rr2[:], in_=rr2[:]), r=[rr2], w=[rr2])
                    k.op("dve", lambda e: e.scalar_tensor_tensor(out=rr2[:], in0=rr2[:], scalar=cst[:, C_GGLA:C_GGLA + 1],
                                                                 in1=gr_[i][:, hg * 4:hg * 4 + 4, :].rearrange("p u n -> p (u n)"),
                                                                 op0=ALU.mult, op1=ALU.mult), r=[rr2, cst, gr_[i]], w=[rr2])
                    o_ = ost[hg]
                    k.op("dve", lambda e: e.tensor_tensor(out=o_[:].rearrange("p u n -> p (u n)"), in0=psO[:], in1=rr2[:], op=ALU.mult),
                         r=[psO, rr2], w=[o_])
                    k.dma("sp", oav[:, hg * 4:hg * 4 + 4, ro:ro + 128], o_[:], r=[o_], w=[oaT_d])
            for hg in range(2):
                psU = nps()
                for u in range(4):
                    h = hg * 4 + u
                    k.op("pe", lambda e: e.matmul(psU[0:64, u * 128:(u + 1) * 128], lhsT=ktl[:, h * 64:(h + 1) * 64], rhs=vt_[i][:, h * 128:(h + 1) * 128],
                                                  start=True, stop=True), r=[ktl, vt_[i]], w=[psU])
                k.op("dve", lambda e: e.tensor_tensor(out=S[:, hg * 4:hg * 4 + 4, :], in0=psU[0:64, :].rearrange("p (u n) -> p u n", n=128),
                                                      in1=S[:, hg * 4:hg * 4 + 4, :], op=ALU.add), r=[psU, S], w=[S])
            k.op("dve", lambda e: e.tensor_tensor(out=S[:], in0=S[:], in1=egl[:].unsqueeze(2).to_broadcast([64, 8, 128]), op=ALU.mult),
                 r=[S, egl], w=[S])
            k.op("dve", lambda e: e.tensor_copy(out=Sb[:], in_=S[:]), r=[S], w=[Sb])
        k.barrier()
        k.pop()
        ph.close()

    def phase3():
        ph = ExitStack()
        k.push(ph)
        NKM = TV
        NKP = NKM + 128
        NIT = cfg.nit
        CK = 16
        ik2 = k.sb("ik2", [128, TV], BF16)
        k.dma("sp", ik2[0:64, :], ikT_d[:, :], r=[ikT_d], w=[ik2])
        k.dma("sp", ik2[64:128, :], ikT_d[:, :], r=[ikT_d], w=[ik2])
        cb = k.sb("cb", [128, 8], F32)
        tbb = k.sb("tbb", [128, 8, 2, 128], BF16)
        k.dma("pool", tbb[:], tbd[:], w=[tbb])
        k.dma("sp", cb[:], cbd[:], w=[cb])
        k.op("dve", lambda e: e.tensor_tensor(out=tbb[:].rearrange("p h a q -> p h (a q)"), in0=tbb[:].rearrange("p h a q -> p h (a q)"),
                                              in1=cb[:].unsqueeze(2).to_broadcast([128, 8, 256]), op=ALU.subtract), r=[tbb, cb], w=[tbb])
        sc = [k.sb("sc%d" % i, [128, NKP], F32) for i in range(2)]
        mk = k.sb("mk", [128, NKP], BF16)
        mkT = k.sb("mkT", [128, 2 * NT, 128], BF16)
        rl = [k.sb("rl%d" % i, [128, 512], BF16) for i in range(4)]
        iq = [k.sb("iq%d" % i, [128, 2, 8, 128], BF16) for i in range(3)]
        for i_ in range(3):
            k.op("pool", lambda e: e.memset(iq[i_][:], 0.0), w=[iq[i_]])
        iwt = [k.sb("iwt%d" % i, [128, 16], F32) for i in range(3)]
        qTt = [k.sb("qTd%d" % i, [128, 8, 128], BF16) for i in range(3)]
        aw = k.sb("aw", [128, 16], F32)
        sg = k.sb("sg", [128, 16], F32)
        dg = k.sb("dg", [128, 16, 128], BF16)
        gm = k.sb("gm", [128, 256], F32)
        bs = k.sb("bs", [128, 8], F32)
        NCB = 4
        kth = [k.sb("kth%d" % i, [128, CK * 128], BF16) for i in range(NCB)]
        vh = [k.sb("vh%d" % i, [128, CK, 129], BF16) for i in range(NCB)]
        pe_ = [k.sb("pe%d" % i, [128, 4, 128], BF16) for i in range(3)]
        pT = [k.sb("pT%d" % i, [128, 4, 128], BF16) for i in range(3)]
        obr = k.sb("obr", [128, 8, 129], F32)
        ob = k.sb("ob", [128, 8, 128], BF16)
        obT = k.sb("obT", [128, 8, 128], BF16)
        rden = k.sb("rden", [128, 8], F32)
        iqv = iqT_d.t.rearrange("(c two d) t -> two d c t", two=2, d=64)
        qTv = qT_d.t.rearrange("h p t -> p h t")
        obv = obT_d.t.rearrange("(h p) t -> p h t", p=128)
        p4c = [0]

        def nps4():
            p = psum[p4c[0] % 4]
            p4c[0] += 1
            return p
        cn = {"rl": 0, "pp": 0, "cb": 0, "ss": 0}

        def geo(t):
            vt = NT + t
            nkt = vt + 1
            return vt, nkt, nkt * 128

        def tloads(t):
            i = t % 3
            ro = t * 128
            k.dma("sp", iq[i][0:64, 0, :, :], iqv[0, :, :, ro:ro + 128], r=[iqT_d], w=[iq[i]])
            k.dma("sp", iq[i][64:128, 1, :, :], iqv[1, :, :, ro:ro + 128], r=[iqT_d], w=[iq[i]])
            k.dma("sp", iwt[i][:], iw_d[ro:ro + 128, :], r=[iw_d], w=[iwt[i]])
            k.dma("sp", qTt[i][:], qTv[:, :, ro:ro + 128], r=[qT_d], w=[qTt[i]])

        def prepA(t):
            i = t % 3
            k.op("act", lambda e: e.activation(out=aw[:], in_=iwt[i][:], func=AF.Abs, scale=1.0 / 32.0), r=[iwt[i]], w=[aw])
            k.op("act", lambda e: e.activation(out=sg[:], in_=iwt[i][:], func=AF.Sign), r=[iwt[i]], w=[sg])
            for j in range(16):
                k.op("pool", lambda e: e.tensor_scalar(out=dg[:, j, :], in0=identb[:], scalar1=sg[:, j:j + 1], scalar2=None, op0=ALU.mult),
                     r=[identb, sg], w=[dg])

        def stageA(t):
            i = t % 3
            vt, nkt, NK = geo(t)
            scb = sc[t % 2]
            nkb = (NK + 511) // 512
            for kb in range(nkb):
                wd = min(512, NK - kb * 512)
                c0 = kb * 512
                psS = psum[4 + (cn["ss"] % 2)]
                cn["ss"] += 1
                pend = []

                def acc(jj, r__):
                    k.op("pe", lambda e: e.matmul(psS[:, 0:wd], lhsT=dg[:, jj, :], rhs=r__[:, 0:wd], start=(jj == 0), stop=(jj == 15)),
                         r=[dg, r__], w=[psS])
                for j in range(16):
                    c = j // 2
                    po = (j % 2) * 64
                    ps = nps4()
                    k.op("pe", lambda e: e.matmul(ps[:, 0:wd], lhsT=iq[i][:, j % 2, c, :], rhs=ik2[:, c0:c0 + wd],
                                                  start=True, stop=True), r=[iq[i], ik2], w=[ps])
                    r_ = rl[cn["rl"] % 4]
                    cn["rl"] += 1
                    k.op("act", lambda e: e.activation(out=r_[:, 0:wd], in_=ps[:, 0:wd], func=AF.Relu, scale=aw[:, j:j + 1]),
                         r=[ps, aw], w=[r_])
                    pend.append((j, r_))
                    if len(pend) > 2:
                        acc(*pend.pop(0))
                while pend:
                    acc(*pend.pop(0))
                if c0 < TO:
                    k.op("act", lambda e: e.activation(out=scb[:, c0:c0 + wd], in_=psS[:, 0:wd], func=AF.Identity,
                                                       bias=cst[:, C_PREB:C_PREB + 1], scale=1.0), r=[psS, cst], w=[scb])
                else:
                    k.op("act", lambda e: e.activation(out=scb[:, c0:c0 + wd], in_=psS[:, 0:wd], func=AF.Copy), r=[psS], w=[scb])

        def stageB(t):
            vt, nkt, NK = geo(t)
            scb = sc[t % 2]
            k.op("dve", lambda e: e.memset(scb[0:64, NK - 64:NK], -1e30), w=[scb])
            NKp = NK
            if nkt % 2 == 1:
                k.op("dve", lambda e: e.memset(scb[:, NK:NK + 128], -1e30), w=[scb])
                NKp = NK + 128
            k.op("dve", lambda e: e.tensor_reduce(out=gm[:], in_=scb[:, 0:NKp].rearrange("p (a g) -> p g a", g=256), axis=AX.X, op=ALU.max),
                 r=[scb], w=[gm])
            k.op("dve", lambda e: e.tensor_reduce(out=bs[:, 0:1], in_=gm[:], axis=AX.X, op=ALU.min), r=[gm], w=[bs])
            k.op("dve", lambda e: e.tensor_reduce(out=bs[:, 1:2], in_=gm[:], axis=AX.X, op=ALU.max), r=[gm], w=[bs])
            k.op("dve", lambda e: e.tensor_tensor(out=bs[:, 2:3], in0=bs[:, 1:2], in1=bs[:, 0:1], op=ALU.subtract), r=[bs], w=[bs])
            for it in range(NIT):
                ck = 2.0 ** -(it + 1)
                k.op("dve", lambda e: e.scalar_tensor_tensor(out=bs[:, 3:4], in0=bs[:, 2:3], scalar=ck, in1=bs[:, 0:1], op0=ALU.mult, op1=ALU.add),
                     r=[bs], w=[bs])
                k.op("dve", lambda e: e.tensor_scalar(out=mk[:, 0:NK], in0=scb[:, 0:NK], scalar1=bs[:, 3:4], scalar2=0.0, op0=ALU.is_ge, op1=ALU.add,
                                                      accum_out=bs[:, 4:5]), r=[scb, bs], w=[mk, bs])
                k.op("dve", lambda e: e.tensor_scalar(out=bs[:, 5:6], in0=bs[:, 4:5], scalar1=255.5, scalar2=ck, op0=ALU.is_ge, op1=ALU.mult),
                     r=[bs], w=[bs])
                k.op("dve", lambda e: e.scalar_tensor_tensor(out=bs[:, 0:1], in0=bs[:, 5:6], scalar=bs[:, 2:3], in1=bs[:, 0:1], op0=ALU.mult, op1=ALU.add),
                     r=[bs], w=[bs])
            k.op("dve", lambda e: e.tensor_scalar(out=bs[:, 6:7], in0=bs[:, 0:1], scalar1=-1e29, scalar2=None, op0=ALU.max), r=[bs], w=[bs])
            k.op("dve", lambda e: e.tensor_scalar(out=mk[:, 0:NK], in0=scb[:, 0:NK], scalar1=bs[:, 6:7], scalar2=-30000.0, op0=ALU.is_lt, op1=ALU.mult),
                 r=[scb, bs], w=[mk])

        def maskT(t):
            vt, nkt, NK = geo(t)
            for g8 in range((nkt + 7) // 8):
                n = min(8, nkt - g8 * 8)
                ps = nps4()
                psb = ps.t[:].bitcast(BF16).rearrange("p (c n) -> p c n", n=128)
                for u in range(n):
                    kt = g8 * 8 + u
                    k.op("pe", lambda e: e.transpose(out=psb[:, u, :], in_=mk[:, kt * 128:(kt + 1) * 128], identity=identb[:]),
                         r=[mk, identb], w=[ps])
                k.op("act", lambda e: e.activation(out=mkT[:, g8 * 8:g8 * 8 + n, :], in_=psb[:, 0:n, :], func=AF.Copy), r=[ps], w=[mkT])

        def attn(t):
            i = t % 3
            ro = t * 128
            vt, nkt, NK = geo(t)
            nck = (nkt + CK - 1) // CK
            seq = [(h, c_) for h in range(8) for c_ in range(nck)]
            bufs = {}

            def cload(idx):
                h, c_ = seq[idx]
                bi = cn["cb"] % NCB
                cn["cb"] += 1
                k0 = c_ * CK
                n_ = min(CK, nkt - k0)
                k.dma("sp", kth[bi][:, 0:n_ * 128], KT_d[h, :, k0 * 128:(k0 + n_) * 128], r=[KT_d], w=[kth[bi]])
                k.dma("sp", vh[bi][:, 0:n_, :], Vd[h, :, k0:k0 + n_, :], r=[Vd], w=[vh[bi]])
                bufs[idx] = bi
            for idx in range(min(2, len(seq))):
                cload(idx)
            prev = None
            state = {}

            def pv(st):
                hh, kts, p_, bi, psO = st
                for u, kt in enumerate(kts):
                    k.op("pe", lambda e: e.matmul(psO[:, 0:129], lhsT=p_[:, u, :], rhs=vh[bi][:, kt % CK, :],
                                                  start=(kt == 0), stop=(kt == nkt - 1)), r=[p_, vh[bi]], w=[psO])
                if kts[-1] == nkt - 1:
                    k.op("act", lambda e: e.activation(out=obr[:, hh, :], in_=psO[:, 0:129], func=AF.Copy), r=[psO], w=[obr])
            for idx, (h, c_) in enumerate(seq):
                if idx + 2 < len(seq):
                    cload(idx + 2)
                bi = bufs[idx]
                psO = psum[6 + (h % 2)]
                k0 = c_ * CK
                n_ = min(CK, nkt - k0)
                for kg in range((n_ + 3) // 4):
                    kts = [k0 + kg * 4 + u for u in range(min(4, n_ - kg * 4))]
                    n = len(kts)
                    ps = nps4()
                    for u, kt in enumerate(kts):
                        near = kt >= vt - 1
                        kl = kt - k0
                        k.op("pe", lambda e: e.matmul(ps[:, u * 128:(u + 1) * 128], lhsT=kth[bi][:, kl * 128:(kl + 1) * 128], rhs=qTt[i][:, h, :],
                                                      start=True, stop=False), r=[kth[bi], qTt[i]], w=[ps])
                        if near:
                            a = vt - kt
                            k.op("pe", lambda e: e.matmul(ps[:, u * 128:(u + 1) * 128], lhsT=identb[:], rhs=tbb[:, h, a, :],
                                                          start=False, stop=False), r=[identb, tbb], w=[ps])
                        k.op("pe", lambda e: e.matmul(ps[:, u * 128:(u + 1) * 128], lhsT=identb[:], rhs=mkT[:, kt, :],
                                                      start=False, stop=True), r=[identb, mkT], w=[ps])
                    p_ = pT[cn["pp"] % 3]
                    cn["pp"] += 1
                    k.op("act", lambda e: e.activation(out=p_[:, 0:n, :].rearrange("p u n -> p (u n)"), in_=ps[:, 0:n * 128], func=AF.Exp,
                                                       bias=cb[:, h:h + 1], scale=1.0), r=[ps, cb], w=[p_])
                    cur = (h, kts, p_, bi, psO)
                    if prev is not None:
                        pv(prev)
                    prev = cur
            pv(prev)

        def attn_tail(t):
            ro = t * 128
            k.op("dve", lambda e: e.reciprocal(out=rden[:], in_=obr[:, :, 128]), r=[obr], w=[rden])
            k.op("dve", lambda e: e.tensor_tensor(out=ob[:], in0=obr[:, :, 0:128], in1=rden[:].unsqueeze(2).to_broadcast([128, 8, 128]), op=ALU.mult),
                 r=[obr, rden], w=[ob])
            ps = nps4()
            psb = ps.t[:].bitcast(BF16).rearrange("p (c n) -> p c n", n=128)
            for h in range(8):
                k.op("pe", lambda e: e.transpose(out=psb[:, h, :], in_=ob[:, h, :], identity=identb[:]), r=[ob, identb], w=[ps])
            k.op("act", lambda e: e.activation(out=obT[:], in_=psb, func=AF.Copy), r=[ps], w=[obT])
            k.dma("sp", obv[:, :, ro:ro + 128], obT[:], r=[obT], w=[obT_d])

        tloads(0)
        if NT > 1:
            tloads(1)
        prepA(0)
        stageA(0)
        stageB(0)
        if NT > 1:
            prepA(1)
            stageA(1)
        for t in range(NT):
            if t + 2 < NT:
                tloads(t + 2)
            maskT(t)
            if t + 1 < NT:
                stageB(t + 1)
            if t + 2 < NT:
                prepA(t + 2)
            attn(t)
            if t + 2 < NT:
                stageA(t + 2)
            attn_tail(t)
        k.barrier()
        k.pop()
        ph.close()

    def phase4a():
        ph = ExitStack()
        k.push(ph)
        wog = k.sb("wog", [128, 8, D], BF16)
        wod = k.sb("wod", [128, 8, D], BF16)
        for c in range(8):
            k.dma("pool", wog[:, c, :], w_oa[c * 128:(c + 1) * 128, :], r=[w_oa], w=[wog])
            k.dma("pool", wod[:, c, :], w_ob[c * 128:(c + 1) * 128, :], r=[w_ob], w=[wod])
        oag = [k.sb("oag%d" % i, [128, 8, 512], BF16) for i in range(2)]
        obg = [k.sb("obg%d" % i, [128, 8, 512], BF16) for i in range(2)]
        gt = [k.sb("gt%d" % i, [128, 2, 512], BF16) for i in range(3)]
        t1 = [k.sb("t1_%d" % i, [128, 512], F32) for i in range(2)]
        t2 = [k.sb("t2_%d" % i, [128, 512], F32) for i in range(2)]
        mst = [k.sb("mst%d" % i, [128, 512], BF16) for i in range(3)]
        oav = oaT_d.t.rearrange("(h p) t -> p h t", p=128)
        obv = obT_d.t.rearrange("(h p) t -> p h t", p=128)
        gtv = gateT_d.t.rearrange("(a c p) t -> p a c t", a=2, p=128)
        n = 0
        for g4 in range(NT // 4):
            t0 = g4 * 512
            i = g4 % 2
            k.dma("sp", oag[i][:], oav[:, :, t0:t0 + 512], r=[oaT_d], w=[oag[i]])
            k.dma("sp", obg[i][:], obv[:, :, t0:t0 + 512], r=[obT_d], w=[obg[i]])
            for cbk in range(16):
                g_ = gt[n % 3]
                k.dma("sp", g_[:], gtv[:, :, cbk, t0:t0 + 512], r=[gateT_d], w=[g_])
                psA = nps()
                for kc in range(8):
                    k.op("pe", lambda e: e.matmul(psA[:], lhsT=wog[:, kc, cbk * 128:(cbk + 1) * 128], rhs=oag[i][:, kc, :],
                                                  start=(kc == 0), stop=(kc == 7)), r=[wog, oag[i]], w=[psA])
                psB = nps()
                for kc in range(8):
                    k.op("pe", lambda e: e.matmul(psB[:], lhsT=wod[:, kc, cbk * 128:(cbk + 1) * 128], rhs=obg[i][:, kc, :],
                                                  start=(kc == 0), stop=(kc == 7)), r=[wod, obg[i]], w=[psB])
                a_ = t1[n % 2]
                b_ = t2[n % 2]
                m_ = mst[n % 3]
                k.op("dve", lambda e: e.tensor_tensor(out=a_[:], in0=psA[:], in1=g_[:, 0, :], op=ALU.mult), r=[psA, g_], w=[a_])
                k.op("dve", lambda e: e.tensor_tensor(out=b_[:], in0=psB[:], in1=g_[:, 1, :], op=ALU.mult), r=[psB, g_], w=[b_])
                k.op("pool", lambda e: e.tensor_tensor(out=m_[:], in0=a_[:], in1=b_[:], op=ALU.add), r=[a_, b_], w=[m_])
                k.dma("sp", mT_d[cbk * 128:(cbk + 1) * 128, t0:t0 + 512], m_[:], r=[m_], w=[mT_d])
                n += 1
        k.barrier()
        k.pop()
        ph.close()

    def phase4b():
        ph = ExitStack()
        k.push(ph)
        wo = k.sb("wo", [128, 16, D], BF16)
        for c in range(16):
            k.dma("pool", wo[:, c, :], w_o[c * 128:(c + 1) * 128, :], r=[w_o], w=[wo])
        wr = k.sb("wr", [128, 16, 72], BF16)
        k.dma("pool", wr[:], w_r.t.rearrange("(c p) n -> p c n", p=128), r=[w_r], w=[wr])
        brb = k.sb("brb", [1, 72], BF16)
        k.dma("pool", brb[:], b_r[:], r=[b_r], w=[brb])
        onesrb = k.sb("onesrb", [1, 128], BF16)
        k.op("dve", lambda e: e.memset(onesrb[:], 1.0), w=[onesrb])
        gffn = k.sb("gffn", [128, D], F32)
        k.dma("sp", gffn[:], gffn_row.t.partition_broadcast(128), r=[gffn_row], w=[gffn])
        trif = k.sb("trif4", [128, 128], F32)
        k.dma("sp", trif[:], trid[:], w=[trif])
        stri = k.sb("stri", [128, 128], BF16)
        k.op("dve", lambda e: e.tensor_tensor(out=stri[:], in0=trif[:], in1=identf[:], op=ALU.subtract), r=[trif, identf], w=[stri])
        ones1 = k.sb("ones1", [128, 128], BF16)
        k.op("dve", lambda e: e.memset(ones1[:], 1.0), w=[ones1])
        ebase = k.sb("ebase", [128, 64], F32)
        k.dma("sp", ebase[:], ebased[:], w=[ebase])
        run = k.sb("run", [128, 64], F32)
        k.op("dve", lambda e: e.memset(run[:], 0.0), w=[run])
        mt = [k.sb("mt%d" % i, [128, 16, 128], BF16) for i in range(2)]
        xt = [k.sb("x4_%d" % i, [128, D], F32) for i in range(2)]
        junk = k.sb("junk4", [128, D], BF16)
        hf = [k.sb("hf%d" % i, [128, D], BF16) for i in range(2)]
        hfT = k.sb("hfT", [128, 16, 128], BF16)
        rs = k.sb("rs4", [128, 16], F32)
        lgt = k.sb("lgt", [128, 72], F32)
        ohg = k.sb("ohg", [128, 8], F32)
        pen = k.sb("pen", [128, 8], F32)
        ml = k.sb("ml", [128, 64], F32)
        ml2 = k.sb("ml2", [128, 64], F32)
        s1 = k.sb("s1", [128, 64], F32)
        s2 = k.sb("s2", [128, 64], F32)
        selb = k.sb("selb", [128, 64], BF16)
        pos = k.sb("pos", [128, 64], F32)
        junk64 = k.sb("junk64", [128, 64], F32)
        slf = k.sb("slf", [128, 2], F32)
        mtv = mT_d.t.rearrange("(c p) t -> p c t", p=128)

        def loads(t):
            i = t % 2
            ro = t * 128
            k.dma("sp", mt[i][:], mtv[:, :, ro:ro + 128], r=[mT_d], w=[mt[i]])
            k.dma("sp", xt[i][:], x_own[ro:ro + 128, :], r=[x_own], w=[xt[i]])
        loads(0)
        for t in range(NT):
            i = t % 2
            ro = t * 128
            if t + 1 < NT:
                loads(t + 1)
            for nb in range(4):
                ps = nps()
                for c in range(16):
                    k.op("pe", lambda e: e.matmul(ps[:], lhsT=mt[i][:, c, :], rhs=wo[:, c, nb * 512:(nb + 1) * 512],
                                                  start=(c == 0), stop=(c == 15)), r=[mt[i], wo], w=[ps])
                k.op("dve", lambda e: e.tensor_tensor(out=xt[i][:, nb * 512:(nb + 1) * 512], in0=ps[:], in1=xt[i][:, nb * 512:(nb + 1) * 512],
                                                      op=ALU.add), r=[ps, xt[i]], w=[xt[i]])
            k.dma("sp", x1_d[ro:ro + 128, :], xt[i][:], r=[xt[i]], w=[x1_d])
            k.op("act", lambda e: e.activation(out=junk[:], in_=xt[i][:], func=AF.Square, accum_out=rs[:, 0:1]), r=[xt[i]], w=[junk, rs])
            k.op("act", lambda e: e.activation(out=rs[:, 1:2], in_=rs[:, 0:1], func=AF.Sqrt, bias=epsc[:, 0:1], scale=1.0 / D), r=[rs, epsc], w=[rs])
            k.op("dve", lambda e: e.reciprocal(out=rs[:, 2:3], in_=rs[:, 1:2]), r=[rs], w=[rs])
            k.op("dve", lambda e: e.scalar_tensor_tensor(out=hf[i][:], in0=xt[i][:], scalar=rs[:, 2:3], in1=gffn[:], op0=ALU.mult, op1=ALU.mult),
                 r=[xt[i], rs, gffn], w=[hf[i]])
            for hh in range(2):
                ps = nps()
                psb = ps.t[:].bitcast(BF16).rearrange("p (c n) -> p c n", n=128)
                for c in range(8):
                    cc = hh * 8 + c
                    k.op("pe", lambda e: e.transpose(out=psb[:, c, :], in_=hf[i][:, cc * 128:(cc + 1) * 128], identity=identb[:]),
                         r=[hf[i], identb], w=[ps])
                if hh == 0:
                    k.op("act", lambda e: e.activation(out=hfT[:, 0:8, :], in_=psb, func=AF.Copy), r=[ps], w=[hfT])
                else:
                    k.op("dve", lambda e: e.tensor_copy(out=hfT[:, 8:16, :], in_=psb), r=[ps], w=[hfT])
            ps = nps()
            for c in range(16):
                k.op("pe", lambda e: e.matmul(ps[:, 0:72], lhsT=hfT[:, c, :], rhs=wr[:, c, :], start=(c == 0), stop=False), r=[hfT, wr], w=[ps])
            k.op("pe", lambda e: e.matmul(ps[:, 0:72], lhsT=onesrb[:], rhs=brb[:], start=False, stop=True), r=[onesrb, brb], w=[ps])
            k.op("dve", lambda e: e.tensor_copy(out=lgt[:], in_=ps[:, 0:72]), r=[ps], w=[lgt])
            k.op("dve", lambda e: e.tensor_reduce(out=rs[:, 3:4], in_=lgt[:, 0:8], axis=AX.X, op=ALU.max), r=[lgt], w=[rs])
            k.op("dve", lambda e: e.tensor_scalar(out=ohg[:], in0=lgt[:, 0:8], scalar1=rs[:, 3:4], scalar2=None, op0=ALU.is_ge), r=[lgt, rs], w=[ohg])
            k.op("dve", lambda e: e.tensor_scalar(out=rs[:, 4:5], in0=rs[:, 3:4], scalar1=-1.0, scalar2=None, op0=ALU.mult), r=[rs], w=[rs])
            k.op("act", lambda e: e.activation(out=pen[:], in_=lgt[:, 0:8], func=AF.Exp, bias=rs[:, 4:5], scale=1.0, accum_out=rs[:, 5:6]),
                 r=[lgt, rs], w=[pen, rs])
            k.op("dve", lambda e: e.reciprocal(out=rs[:, 6:7], in_=rs[:, 5:6]), r=[rs], w=[rs])
            k.op("dve", lambda e: e.tensor_scalar(out=pen[:], in0=ohg[:], scalar1=1e9, scalar2=-1e9, op0=ALU.mult, op1=ALU.add), r=[ohg], w=[pen])
            k.op("dve", lambda e: e.tensor_tensor(out=ml[:].rearrange("p (g e) -> p g e", e=8), in0=lgt[:, 8:72].rearrange("p (g e) -> p g e", e=8),
                                                  in1=pen[:].unsqueeze(2).to_broadcast([128, 8, 8]), op=ALU.add), r=[lgt, pen], w=[ml])
            k.op("dve", lambda e: e.tensor_reduce(out=rs[:, 7:8], in_=ml[:], axis=AX.X, op=ALU.max), r=[ml], w=[rs])
            k.op("dve", lambda e: e.tensor_scalar(out=s1[:], in0=ml[:], scalar1=rs[:, 7:8], scalar2=None, op0=ALU.is_ge), r=[ml, rs], w=[s1])
            k.op("dve", lambda e: e.scalar_tensor_tensor(out=ml2[:], in0=s1[:], scalar=-1e9, in1=ml[:], op0=ALU.mult, op1=ALU.add), r=[s1, ml], w=[ml2])
            k.op("dve", lambda e: e.tensor_reduce(out=rs[:, 8:9], in_=ml2[:], axis=AX.X, op=ALU.max), r=[ml2], w=[rs])
            k.op("dve", lambda e: e.tensor_scalar(out=s2[:], in0=ml2[:], scalar1=rs[:, 8:9], scalar2=None, op0=ALU.is_ge), r=[ml2, rs], w=[s2])
            k.op("dve", lambda e: e.tensor_tensor(out=rs[:, 9:10], in0=rs[:, 8:9], in1=rs[:, 7:8], op=ALU.subtract), r=[rs], w=[rs])
            k.op("act", lambda e: e.activation(out=rs[:, 10:11], in_=rs[:, 9:10], func=AF.Exp), r=[rs], w=[rs])
            k.op("dve", lambda e: e.tensor_scalar(out=rs[:, 11:12], in0=rs[:, 10:11], scalar1=1.0, scalar2=None, op0=ALU.add), r=[rs], w=[rs])
            k.op("dve", lambda e: e.reciprocal(out=rs[:, 12:13], in_=rs[:, 11:12]), r=[rs], w=[rs])
            k.op("dve", lambda e: e.tensor_tensor(out=wts[:, t, 0:1], in0=rs[:, 6:7], in1=rs[:, 12:13], op=ALU.mult), r=[rs], w=[wts])
            k.op("dve", lambda e: e.tensor_tensor(out=wts[:, t, 1:2], in0=wts[:, t, 0:1], in1=rs[:, 10:11], op=ALU.mult), r=[rs, wts], w=[wts])
            k.op("dve", lambda e: e.tensor_tensor(out=selb[:], in0=s1[:], in1=s2[:], op=ALU.add), r=[s1, s2], w=[selb])
            psP = nps()
            k.op("pe", lambda e: e.matmul(psP[:, 0:64], lhsT=stri[:], rhs=selb[:], start=True, stop=True), r=[stri, selb], w=[psP])
            psC = nps()
            k.op("pe", lambda e: e.matmul(psC[:, 0:64], lhsT=ones1[:], rhs=selb[:], start=True, stop=True), r=[ones1, selb], w=[psC])
            k.op("dve", lambda e: e.tensor_tensor(out=pos[:], in0=psP[:, 0:64], in1=run[:], op=ALU.add), r=[psP, run], w=[pos])
            k.op("dve", lambda e: e.tensor_tensor(out=run[:], in0=psC[:, 0:64], in1=run[:], op=ALU.add), r=[psC, run], w=[run])
            k.op("dve", lambda e: e.scalar_tensor_tensor(out=pos[:], in0=pos[:], scalar=float(CAP - 1), in1=ebase[:], op0=ALU.min, op1=ALU.add),
                 r=[pos, ebase], w=[pos])
            for (sx, col) in ((s1, 0), (s2, 1)):
                k.op("dve", lambda e: e.tensor_tensor(out=junk64[:], in0=sx[:], in1=pos[:], op=ALU.mult), r=[sx, pos], w=[junk64])
                k.op("dve", lambda e: e.tensor_reduce(out=slf[:, col:col + 1], in_=junk64[:], axis=AX.X, op=ALU.add), r=[junk64], w=[slf])
            k.op("dve", lambda e: e.tensor_copy(out=slots_i[:, t, :], in_=slf[:]), r=[slf], w=[slots_i])
            for j in range(2):
                k.dma("pool", Xd[:, :], hf[i][:], r=[hf[i], slots_i], w=[Xd],
                      indirect=dict(out_offset=bass.IndirectOffsetOnAxis(ap=slots_i[:, t, j:j + 1], axis=0), in_offset=None))
        if cfg.debug:
            k.dma("sp", slots_o[:], slots_i[:], r=[slots_i], w=[slots_o])
            k.dma("sp", wts_o[:], wts[:], r=[wts], w=[wts_o])
        k.barrier()
        k.pop()
        ph.close()

    def phase5():
        ph = ExitStack()
        k.push(ph)
        NE = cfg.nexp
        NRB = CAP // 128
        wg = [k.sb("wg%d" % i, [128, 16, 512], BF16) for i in range(2)]
        wu = [k.sb("wu%d" % i, [128, 16, 512], BF16) for i in range(2)]
        wd = [k.sb("wd%d" % i, [128, 4, D], BF16) for i in range(2)]
        xr = [k.sb("xr%d" % i, [128, NRB, D], BF16) for i in range(2)]
        XT = k.sb("XT", [128, 16, CAP], BF16)
        sgt = [k.sb("sgt%d" % i, [128, CAP], F32) for i in range(2)]
        aT = k.sb("aT", [128, 4, CAP], BF16)
        yst = [k.sb("yst%d" % i, [128, D], F32) for i in range(2)]

        def loads(e):
            i = e % 2
            k.dma("pool", wg[i][:], w_eg.t[e].rearrange("(c p) f -> p c f", p=128), r=[w_eg], w=[wg[i]])
            k.dma("pool", wu[i][:], w_eu.t[e].rearrange("(c p) f -> p c f", p=128), r=[w_eu], w=[wu[i]])
            k.dma("pool", wd[i][:], w_ed.t[e].rearrange("(c p) n -> p c n", p=128), r=[w_ed], w=[wd[i]])
            k.dma("sp", xr[i][:], Xd.t[e * CAP:(e + 1) * CAP, :].rearrange("(b p) d -> p b d", p=128), r=[Xd], w=[xr[i]])
        loads(0)
        ny = 0
        for e_ in range(NE):
            i = e_ % 2
            if e_ + 1 < NE:
                loads(e_ + 1)
            n = 0
            for rb in range(NRB):
                for hh in range(2):
                    ps = nps()
                    psb = ps.t[:].bitcast(BF16).rearrange("p (c n) -> p c n", n=128)
                    for c in range(8):
                        cc = hh * 8 + c
                        k.op("pe", lambda e: e.transpose(out=psb[:, c, :], in_=xr[i][:, rb, cc * 128:(cc + 1) * 128], identity=identb[:]),
                             r=[xr[i], identb], w=[ps])
                    if n % 2 == 0:
                        k.op("act", lambda e: e.activation(out=XT[:, hh * 8:hh * 8 + 8, rb * 128:(rb + 1) * 128], in_=psb, func=AF.Copy), r=[ps], w=[XT])
                    else:
                        k.op("dve", lambda e: e.tensor_copy(out=XT[:, hh * 8:hh * 8 + 8, rb * 128:(rb + 1) * 128], in_=psb), r=[ps], w=[XT])
                    n += 1
            for fb in range(4):
                psg = nps()
                for c in range(16):
                    k.op("pe", lambda e: e.matmul(psg[:, 0:CAP], lhsT=wg[i][:, c, fb * 128:(fb + 1) * 128], rhs=XT[:, c, :],
                                                  start=(c == 0), stop=(c == 15)), r=[wg[i], XT], w=[psg])
                psu = nps()
                for c in range(16):
                    k.op("pe", lambda e: e.matmul(psu[:, 0:CAP], lhsT=wu[i][:, c, fb * 128:(fb + 1) * 128], rhs=XT[:, c, :],
                                                  start=(c == 0), stop=(c == 15)), r=[wu[i], XT], w=[psu])
                s_ = sgt[fb % 2]
                k.op("act", lambda e: e.activation(out=s_[:], in_=psg[:, 0:CAP], func=AF.Silu), r=[psg], w=[s_])
                k.op("dve", lambda e: e.tensor_tensor(out=aT[:, fb, :], in0=psu[:, 0:CAP], in1=s_[:], op=ALU.mult), r=[psu, s_], w=[aT])
            for rb in range(NRB):
                y_ = yst[ny % 2]
                ny += 1
                for nb in range(4):
                    ps = nps()
                    for fc in range(4):
                        k.op("pe", lambda e: e.matmul(ps[:], lhsT=aT[:, fc, rb * 128:(rb + 1) * 128], rhs=wd[i][:, fc, nb * 512:(nb + 1) * 512],
                                                      start=(fc == 0), stop=(fc == 3)), r=[aT, wd[i]], w=[ps])
                    if nb % 2 == 0:
                        k.op("act", lambda e: e.activation(out=y_[:, nb * 512:(nb + 1) * 512], in_=ps[:], func=AF.Copy), r=[ps], w=[y_])
                    else:
                        k.op("dve", lambda e: e.tensor_copy(out=y_[:, nb * 512:(nb + 1) * 512], in_=ps[:]), r=[ps], w=[y_])
                r0 = e_ * CAP + rb * 128
                k.dma("sp", Yd[r0:r0 + 128, :], y_[:], r=[y_], w=[Yd])
            if e_ % 16 == 15:
                k.barrier()
        k.barrier()
        k.pop()
        ph.close()

    def phase6():
        ph = ExitStack()
        k.push(ph)
        wpg = k.sb("wpg", [128, 16, D], BF16)
        for c in range(16):
            k.dma("pool", wpg[:, c, :], w_pg[c * 128:(c + 1) * 128, :], r=[w_pg], w=[wpg])
        wpp = k.sb("wpp", [128, 2, D], BF16)
        k.dma("pool", wpp[:], w_pp.t.rearrange("(c p) n -> p c n", p=128), r=[w_pp], w=[wpp])
        bpg = k.sb("bpg", [1, D], BF16)
        k.dma("pool", bpg[:], b_pg[:], r=[b_pg], w=[bpg])
        onesrb = k.sb("onesrb6", [1, 128], BF16)
        k.op("dve", lambda e: e.memset(onesrb[:], 1.0), w=[onesrb])
        gple = k.sb("gple", [128, D], F32)
        k.dma("sp", gple[:], gple_row.t.partition_broadcast(128), r=[gple_row], w=[gple])
        x1t = [k.sb("x1t%d" % i, [128, D], F32) for i in range(2)]
        y1t = [k.sb("y1t%d" % i, [128, D], F32) for i in range(2)]
        y2t = [k.sb("y2t%d" % i, [128, D], F32) for i in range(2)]
        pt = [k.sb("pt%d" % i, [128, 256], F32) for i in range(2)]
        ptb = k.sb("ptb", [128, 256], BF16)
        pTt = k.sb("pTt", [128, 2, 128], BF16)
        junk = k.sb("junk6", [128, D], BF16)
        hp = k.sb("hp", [128, D], BF16)
        hpT = k.sb("hpT", [128, 16, 128], BF16)
        rs = k.sb("rs6", [128, 4], F32)
        sgm = [k.sb("sgm%d" % i, [128, 512], F32) for i in range(2)]
        ot = [k.sb("ot%d" % i, [128, D], F32) for i in range(2)]

        def loads(t):
            i = t % 2
            ro = t * 128
            k.dma("sp", x1t[i][:], x1_d[ro:ro + 128, :], r=[x1_d], w=[x1t[i]])
            k.dma("sp", pt[i][:], p_own[ro:ro + 128, :], r=[p_own], w=[pt[i]])
            k.dma("pool", y1t[i][:], Yd[:, :], r=[Yd, slots_i], w=[y1t[i]],
                  indirect=dict(out_offset=None, in_offset=bass.IndirectOffsetOnAxis(ap=slots_i[:, t, 0:1], axis=0)))
            k.dma("pool", y2t[i][:], Yd[:, :], r=[Yd, slots_i], w=[y2t[i]],
                  indirect=dict(out_offset=None, in_offset=bass.IndirectOffsetOnAxis(ap=slots_i[:, t, 1:2], axis=0)))
        loads(0)
        for t in range(NT):
            i = t % 2
            ro = t * 128
            if t + 1 < NT:
                loads(t + 1)
            x2 = x1t[i]
            k.op("dve", lambda e: e.scalar_tensor_tensor(out=x2[:], in0=y1t[i][:], scalar=wts[:, t, 0:1], in1=x2[:], op0=ALU.mult, op1=ALU.add),
                 r=[y1t[i], wts, x2], w=[x2])
            k.op("dve", lambda e: e.scalar_tensor_tensor(out=x2[:], in0=y2t[i][:], scalar=wts[:, t, 1:2], in1=x2[:], op0=ALU.mult, op1=ALU.add),
                 r=[y2t[i], wts, x2], w=[x2])
            k.op("act", lambda e: e.activation(out=junk[:], in_=x2[:], func=AF.Square, accum_out=rs[:, 0:1]), r=[x2], w=[junk, rs])
            k.op("act", lambda e: e.activation(out=rs[:, 1:2], in_=rs[:, 0:1], func=AF.Sqrt, bias=epsc[:, 0:1], scale=1.0 / D), r=[rs, epsc], w=[rs])
            k.op("dve", lambda e: e.reciprocal(out=rs[:, 2:3], in_=rs[:, 1:2]), r=[rs], w=[rs])
            k.op("dve", lambda e: e.scalar_tensor_tensor(out=hp[:], in0=x2[:], scalar=rs[:, 2:3], in1=gple[:], op0=ALU.mult, op1=ALU.mult),
                 r=[x2, rs, gple], w=[hp])
            for hh in range(2):
                ps = nps()
                psb = ps.t[:].bitcast(BF16).rearrange("p (c n) -> p c n", n=128)
                for c in range(8):
                    cc = hh * 8 + c
                    k.op("pe", lambda e: e.transpose(out=psb[:, c, :], in_=hp[:, cc * 128:(cc + 1) * 128], identity=identb[:]),
                         r=[hp, identb], w=[ps])
                if hh == 0:
                    k.op("act", lambda e: e.activation(out=hpT[:, 0:8, :], in_=psb, func=AF.Copy), r=[ps], w=[hpT])
                else:
                    k.op("dve", lambda e: e.tensor_copy(out=hpT[:, 8:16, :], in_=psb), r=[ps], w=[hpT])
            k.op("dve", lambda e: e.tensor_copy(out=ptb[:], in_=pt[i][:]), r=[pt[i]], w=[ptb])
            ps = nps()
            psb = ps.t[:].bitcast(BF16).rearrange("p (c n) -> p c n", n=128)
            for c in range(2):
                k.op("pe", lambda e: e.transpose(out=psb[:, c, :], in_=ptb[:, c * 128:(c + 1) * 128], identity=identb[:]), r=[ptb, identb], w=[ps])
            k.op("dve", lambda e: e.tensor_copy(out=pTt[:], in_=psb[:, 0:2, :]), r=[ps], w=[pTt])
            o_ = ot[i]
            for nb in range(4):
                psg = nps()
                for c in range(16):
                    k.op("pe", lambda e: e.matmul(psg[:], lhsT=hpT[:, c, :], rhs=wpg[:, c, nb * 512:(nb + 1) * 512], start=(c == 0), stop=False),
                         r=[hpT, wpg], w=[psg])
                k.op("pe", lambda e: e.matmul(psg[:], lhsT=onesrb[:], rhs=bpg[:, nb * 512:(nb + 1) * 512], start=False, stop=True),
                     r=[onesrb, bpg], w=[psg])
                psp = nps()
                for c in range(2):
                    k.op("pe", lambda e: e.matmul(psp[:], lhsT=pTt[:, c, :], rhs=wpp[:, c, nb * 512:(nb + 1) * 512], start=(c == 0), stop=(c == 1)),
                         r=[pTt, wpp], w=[psp])
                s_ = sgm[nb % 2]
                k.op("act", lambda e: e.activation(out=s_[:], in_=psg[:], func=AF.Sigmoid), r=[psg], w=[s_])
                k.op("dve", lambda e: e.tensor_tensor(out=s_[:], in0=psp[:], in1=s_[:], op=ALU.mult), r=[psp, s_], w=[s_])
                k.op("pool", lambda e: e.tensor_tensor(out=o_[:, nb * 512:(nb + 1) * 512], in0=s_[:], in1=x2[:, nb * 512:(nb + 1) * 512], op=ALU.add),
                     r=[s_, x2], w=[o_])
            k.dma("sp", out_d[ro:ro + 128, :], o_[:], r=[o_], w=[out_d])
        k.barrier()
        k.pop()
        ph.close()

    phase1()
    if cfg.phases >= 2:
        phase2()
    if cfg.phases >= 3:
        phase3()
    if cfg.phases >= 4:
        phase4a()
        phase4b()
    if cfg.phases >= 5:
        phase5()
        phase6()

    k.barrier()
    return nc, es, k


def _pack_cst(inp, s):
    c = np.zeros((128, C_NCOL), np.float32)
    c[:, C_GMIX:C_GMIX + 16] = inp["norm_mix_g"][0].reshape(16, 128).T
    c[:, C_GFFN:C_GFFN + 16] = inp["norm_ffn_g"][0].reshape(16, 128).T
    c[:, C_GPLE:C_GPLE + 16] = inp["norm_ple_g"][0].reshape(16, 128).T
    c[:, C_BBG:C_BBG + 32] = inp["b_branch_gate"][0].reshape(32, 128).T
    c[:, C_GGLA] = inp["gla_norm_g"][0]
    c[:, C_GQ] = inp["q_norm_g"][0]
    c[:, C_GK] = inp["k_norm_g"][0]
    c[:, C_PREV] = 1.0 if s == 1 else 0.0
    c[:, C_PREB] = 0.0 if s == 1 else -1e30
    return c


def _t5_bucket(rel):
    half, exact = 16, 8
    sign = np.where(rel > 0, half, 0)
    n = np.abs(rel)
    nf = np.maximum(n, 1).astype(np.float32)
    large = exact + (np.log(nf / exact) / np.log(128 / exact) * (half - exact)).astype(np.int32)
    large = np.minimum(large, half - 1)
    return sign + np.where(n < exact, n, large)


def _bias_tables(rel_bias):
    s_ = np.arange(128)[:, None]
    q_ = np.arange(128)[None, :]
    tb = np.zeros((128, 8, 2, 128), np.float32)
    for a in range(2):
        bk = _t5_bucket((s_ - q_ - 128 * a).astype(np.int32))
        tb[:, :, a, :] = np.transpose(rel_bias[bk], (0, 2, 1))
    cb = np.ascontiguousarray(np.broadcast_to(rel_bias[15][None, :], (128, 8))).astype(np.float32)
    return tb, cb


def make_in_maps(inp, cfg):
    NT = cfg.NT
    TO = NT * 128
    x = inp["x"]
    p = inp["p"][0]
    ident = np.eye(128, dtype=np.float32)
    tb, cb = _bias_tables(inp["rel_bias"])
    w_r = np.ascontiguousarray(np.concatenate([inp["w_group_router"][0], inp["w_expert_router"][0]], axis=1))
    b_r = np.concatenate([inp["b_group_router"][0], inp["b_expert_router"][0]]).reshape(1, 72).astype(np.float32)
    ebase = np.ascontiguousarray(np.broadcast_to((np.arange(64, dtype=np.float32) * cfg.cap)[None, :], (128, 64)))
    maps = []
    for c in range(8):
        b, s = c // 2, c % 2
        m = {
            "x_pre": np.ascontiguousarray(x[b, 0:TO]),
            "x_own": np.ascontiguousarray(x[b, s * TO:(s + 1) * TO]),
            "p_own": np.ascontiguousarray(p[b, s * TO:(s + 1) * TO]),
            "cst": _pack_cst(inp, s),
            "ident": ident,
            "w_in": inp["w_in"][0],
            "w_bg": inp["w_branch_gate"][0],
            "w_alpha": inp["gla_w_alpha"][0],
            "b_alpha": inp["gla_b_alpha"][0].reshape(1, 512),
            "tri": np.triu(np.ones((128, 128), np.float32)),
            "tb": tb,
            "cb": cb,
            "w_oa": inp["w_out_gla"][0],
            "w_ob": inp["w_out_dsa"][0],
            "w_o": inp["w_out"][0],
            "w_r": w_r,
            "b_r": b_r,
            "gffn_row": inp["norm_ffn_g"][0].reshape(1, D),
            "ebase": ebase,
            "w_eg": inp["w_exp_gate"][0] if cfg.phases >= 5 else None,
            "w_eu": inp["w_exp_up"][0] if cfg.phases >= 5 else None,
            "w_ed": inp["w_exp_down"][0] if cfg.phases >= 5 else None,
            "w_pg": inp["w_ple_gate"][0],
            "w_pp": inp["w_ple_proj"][0],
            "b_pg": inp["b_ple_gate"][0].reshape(1, D),
            "gple_row": inp["norm_ple_g"][0].reshape(1, D),
        }
        maps.append({kk: vv for kk, vv in m.items() if vv is not None})
    return maps


def run(inp, cfg, trace=False):
    nc, es, k = build(cfg)
    maps = make_in_maps(inp, cfg)
    res = run_bass_kernel_spmd(nc, maps, core_ids=list(range(8)), trace=trace)
    es.close()
    return res


def kernel(**inputs):
    cfg = CFG(NT=32, G=8)
    inp = {k_: np.asarray(v) for k_, v in inputs.items()}
    res = run(inp, cfg)
    B, S = inp["x"].shape[:2]
    out = np.zeros((B, S, D), np.float32)
    TO = cfg.NT * 128
    for c in range(8):
        b, s = c // 2, c % 2
        out[b, s * TO:(s + 1) * TO] = res.results[c]["out"]
    return out
```
